# Optimizing a Trainium2 kernel written in Bass

```python
import math
import jax, jax.numpy as jnp
from jax import lax
import numpy as np

D_MODEL = 1024
BATCH = 16
SEQ = 4096
DEPTH = 1

HEAD_DIM = 64
SWA_Q_HEADS = 8
SWA_KV_HEADS = 2
SWA_GROUP = SWA_Q_HEADS // SWA_KV_HEADS
SWA_WINDOW = 128
SWA_BLOCK = 128
MOBA_HEADS = 8
MOBA_BLOCK = 256
MOBA_TOPK = 3
MOBA_QCHUNK = 32
N_BRANCHES = 2
REL_BUCKETS = 32
REL_MAX_DIST = 128
N_REL_HEADS = SWA_Q_HEADS + MOBA_HEADS
N_GROUPS = 4
EXPERTS_PER_GROUP = 8
N_EXPERTS = N_GROUPS * EXPERTS_PER_GROUP
EXPERT_TOPK = 2
D_EXPERT = 512
MOE_BLOCK = 256
LN_EPS = 1e-5
DEEPNORM_ALPHA = (2.0 * DEPTH) ** 0.25
DEEPNORM_BETA = (8.0 * DEPTH) ** -0.25
NEG_INF = -1e30
ATTN_SCALE = HEAD_DIM ** -0.5

SWA_Q_W = SWA_Q_HEADS * HEAD_DIM
SWA_KV_W = SWA_KV_HEADS * HEAD_DIM
MOBA_W = MOBA_HEADS * HEAD_DIM
GATE_W = N_BRANCHES * D_MODEL
OFF_SWA_Q = 0
OFF_SWA_K = OFF_SWA_Q + SWA_Q_W
OFF_SWA_V = OFF_SWA_K + SWA_KV_W
OFF_MOBA_Q = OFF_SWA_V + SWA_KV_W
OFF_MOBA_K = OFF_MOBA_Q + MOBA_W
OFF_MOBA_V = OFF_MOBA_K + MOBA_W
OFF_GATE = OFF_MOBA_V + MOBA_W
D_IN_PROJ = OFF_GATE + GATE_W

kernel_name = "hybrid_swa_sinks_moba_gated_deepnorm_hmoe"


def layer_norm(x, gain, bias):
    xf = x.astype(jnp.float32)
    mu = jnp.mean(xf, axis=-1, keepdims=True)
    var = jnp.mean(jnp.square(xf - mu), axis=-1, keepdims=True)
    y = (xf - mu) * lax.rsqrt(var + LN_EPS) * gain.astype(jnp.float32) + bias.astype(jnp.float32)
    return y.astype(x.dtype)


def rel_bucket(dist):
    n = jnp.maximum(dist, 0)
    max_exact = REL_BUCKETS // 2
    nf = jnp.maximum(n, 1).astype(jnp.float32)
    large = max_exact + (jnp.log(nf / max_exact) / math.log(REL_MAX_DIST / max_exact)
                         * (REL_BUCKETS - max_exact)).astype(jnp.int32)
    large = jnp.minimum(large, REL_BUCKETS - 1)
    return jnp.where(n < max_exact, n, large)


def sliding_window_attention(q, k, v, sinks, rel_table_a):
    B, S, _ = q.shape
    nb = S // SWA_BLOCK
    q = q.reshape(B, nb, SWA_BLOCK, SWA_KV_HEADS, SWA_GROUP, HEAD_DIM)
    k = k.reshape(B, nb, SWA_BLOCK, SWA_KV_HEADS, HEAD_DIM)
    v = v.reshape(B, nb, SWA_BLOCK, SWA_KV_HEADS, HEAD_DIM)
    pad = jnp.zeros_like(k[:, :1])
    k_cat = jnp.concatenate([jnp.concatenate([pad, k[:, :-1]], axis=1), k], axis=2)
    v_cat = jnp.concatenate([jnp.concatenate([pad, v[:, :-1]], axis=1), v], axis=2)
    logits = jnp.einsum('bnqkgd,bnskd->bnkgqs', q, k_cat).astype(jnp.float32) * ATTN_SCALE
    qi = jnp.arange(SWA_BLOCK)
    kj = jnp.arange(2 * SWA_BLOCK)
    dist = SWA_BLOCK + qi[:, None] - kj[None, :]
    bias = rel_table_a.astype(jnp.float32)[rel_bucket(dist)]
    bias = bias.reshape(SWA_BLOCK, 2 * SWA_BLOCK, SWA_KV_HEADS, SWA_GROUP).transpose(2, 3, 0, 1)
    k_abs = jnp.arange(nb)[:, None] * SWA_BLOCK - SWA_BLOCK + kj[None, :]
    visible = ((dist >= 0) & (dist < SWA_WINDOW))[None] & (k_abs >= 0)[:, None, :]
    logits = jnp.where(visible[None, :, None, None], logits + bias[None, None], NEG_INF)
    sink = jnp.broadcast_to(
        sinks.astype(jnp.float32).reshape(SWA_KV_HEADS, SWA_GROUP)[None, None, :, :, None, None],
        logits.shape[:-1] + (1,))
    probs = jax.nn.softmax(jnp.concatenate([logits, sink], axis=-1), axis=-1)[..., :-1]
    out = jnp.einsum('bnkgqs,bnskd->bnqkgd', probs.astype(v.dtype), v_cat)
    return out.reshape(B, S, SWA_Q_W)


def moba_attention(q, k, v, rel_table_b):
    B, S, _ = q.shape
    nb = -(-S // MOBA_BLOCK)
    s_pad = nb * MOBA_BLOCK

    def heads(t):
        t = jnp.pad(t, ((0, 0), (0, s_pad - S), (0, 0)))
        return t.reshape(B, s_pad, MOBA_HEADS, HEAD_DIM).transpose(0, 2, 1, 3)

    q, k, v = heads(q), heads(k), heads(v)
    kb = k.reshape(B, MOBA_HEADS, nb, MOBA_BLOCK, HEAD_DIM)
    vb = v.reshape(B, MOBA_HEADS, nb, MOBA_BLOCK, HEAD_DIM)
    k_mean = jnp.mean(kb, axis=3)
    q_blk = jnp.arange(s_pad) // MOBA_BLOCK
    gate = jnp.einsum('bhsd,bhnd->bhsn', q, k_mean).astype(jnp.float32)
    past = jnp.arange(nb)[None, :] < q_blk[:, None]
    gate = jnp.where(past, gate, NEG_INF)
    k_sel = min(MOBA_TOPK, nb)
    _, top_idx = lax.top_k(gate, k_sel)
    sel_valid = jnp.arange(k_sel)[None, :] < q_blk[:, None]
    b_ix = jnp.arange(B)[:, None, None, None]
    h_ix = jnp.arange(MOBA_HEADS)[None, :, None, None]
    table_h = rel_table_b.astype(jnp.float32).T

    def one_chunk(c):
        start = c * MOBA_QCHUNK
        qc = lax.dynamic_slice_in_dim(q, start, MOBA_QCHUNK, axis=2)
        idx = lax.dynamic_slice_in_dim(top_idx, start, MOBA_QCHUNK, axis=2)
        valid = lax.dynamic_slice_in_dim(sel_valid, start, MOBA_QCHUNK, axis=0)
        blk = start // MOBA_BLOCK
        k_own = lax.dynamic_index_in_dim(kb, blk, axis=2, keepdims=False)
        v_own = lax.dynamic_index_in_dim(vb, blk, axis=2, keepdims=False)
        k_g = kb[b_ix, h_ix, idx]
        v_g = vb[b_ix, h_ix, idx]
        q_pos = start + jnp.arange(MOBA_QCHUNK)
        d_own = q_pos[:, None] - (blk * MOBA_BLOCK + jnp.arange(MOBA_BLOCK))[None, :]
        l_own = jnp.einsum('bhcd,bhtd->bhct', qc, k_own).astype(jnp.float32) * ATTN_SCALE
        l_own = jnp.where(d_own >= 0, l_own + table_h[:, rel_bucket(d_own)][None], NEG_INF)
        sel_pos = idx[..., None] * MOBA_BLOCK + jnp.arange(MOBA_BLOCK)
        d_sel = q_pos[None, None, :, None, None] - sel_pos
        l_sel = jnp.einsum('bhcd,bhcktd->bhckt', qc, k_g).astype(jnp.float32) * ATTN_SCALE
        l_sel = l_sel + table_h[h_ix[..., None], rel_bucket(d_sel)]
        l_sel = jnp.where(valid[None, None, :, :, None], l_sel, NEG_INF)
        logits = jnp.concatenate([l_own, l_sel.reshape(B, MOBA_HEADS, MOBA_QCHUNK, k_sel * MOBA_BLOCK)], axis=-1)
        p = jax.nn.softmax(logits, axis=-1).astype(v.dtype)
        p_own = p[..., :MOBA_BLOCK]
        p_sel = p[..., MOBA_BLOCK:].reshape(B, MOBA_HEADS, MOBA_QCHUNK, k_sel, MOBA_BLOCK)
        return (jnp.einsum('bhct,bhtd->bhcd', p_own, v_own)
                + jnp.einsum('bhckt,bhcktd->bhcd', p_sel, v_g))

    out = lax.map(one_chunk, jnp.arange(s_pad // MOBA_QCHUNK))
    out = out.transpose(1, 0, 3, 2, 4).reshape(B, s_pad, MOBA_W)
    return out[:, :S]


def hierarchical_moe(h, w_group, b_group, w_expert, b_expert, w_gate, w_up, w_down):
    B, S, D = h.shape
    T = B * S
    hf = h.reshape(T, D)
    g_logits = (hf @ w_group + b_group).astype(jnp.float32)
    g_idx = jnp.argmax(g_logits, axis=-1)
    g_prob = jnp.take_along_axis(jax.nn.softmax(g_logits, axis=-1), g_idx[:, None], axis=1)
    e_logits = (hf @ w_expert + b_expert).astype(jnp.float32).reshape(T, N_GROUPS, EXPERTS_PER_GROUP)
    e_logits = jnp.take_along_axis(e_logits, g_idx[:, None, None], axis=1)[:, 0]
    e_top, e_loc = lax.top_k(e_logits, EXPERT_TOPK)
    weights = g_prob * jax.nn.softmax(e_top, axis=-1)
    flat_e = (g_idx[:, None] * EXPERTS_PER_GROUP + e_loc).reshape(-1)
    flat_w = weights.reshape(-1)
    n_assign = T * EXPERT_TOPK
    order = jnp.argsort(flat_e)
    sorted_e = flat_e[order]
    tok = order // EXPERT_TOPK
    sizes = jnp.bincount(flat_e, length=N_EXPERTS)
    start_unpadded = jnp.cumsum(sizes) - sizes
    padded = ((sizes + MOE_BLOCK - 1) // MOE_BLOCK) * MOE_BLOCK
    padded_end = jnp.cumsum(padded)
    padded_start = padded_end - padded
    dest = padded_start[sorted_e] + jnp.arange(n_assign) - start_unpadded[sorted_e]
    cap = -(-n_assign // MOE_BLOCK) * MOE_BLOCK + N_EXPERTS * MOE_BLOCK
    n_blocks = cap // MOE_BLOCK
    x_buf = jnp.zeros((cap, D), h.dtype).at[dest].set(hf[tok])
    blk_expert = jnp.minimum(
        jnp.searchsorted(padded_end, jnp.arange(n_blocks) * MOE_BLOCK, side='right'), N_EXPERTS - 1)

    def expert_block(args):
        xb, e = args
        act = jax.nn.silu(xb @ w_gate[e]) * (xb @ w_up[e])
        return act @ w_down[e]

    y_buf = lax.map(expert_block, (x_buf.reshape(n_blocks, MOE_BLOCK, D), blk_expert)).reshape(cap, D)
    contrib = y_buf[dest] * flat_w[order][:, None].astype(h.dtype)
    y = jnp.zeros_like(hf).at[tok].add(contrib)
    return y.reshape(B, S, D)


def setup_inputs(seed: int = 0) -> dict:
    key = jax.random.key(seed)
    ks = jax.random.split(key, 24)
    f32 = jnp.float32

    def nrm(k, shape, scale):
        return jax.random.normal(k, shape, f32) * scale

    s_d = D_MODEL ** -0.5
    beta = DEEPNORM_BETA
    x = jax.random.normal(ks[0], (BATCH, SEQ, D_MODEL), f32)
    w_in = jnp.concatenate([
        nrm(ks[1], (DEPTH, D_MODEL, SWA_Q_W), s_d),
        nrm(ks[2], (DEPTH, D_MODEL, SWA_KV_W), s_d),
        nrm(ks[3], (DEPTH, D_MODEL, SWA_KV_W), s_d * beta),
        nrm(ks[4], (DEPTH, D_MODEL, MOBA_W), s_d),
        nrm(ks[5], (DEPTH, D_MODEL, MOBA_W), s_d),
        nrm(ks[6], (DEPTH, D_MODEL, MOBA_W), s_d * beta),
        nrm(ks[7], (DEPTH, D_MODEL, GATE_W), s_d),
    ], axis=-1)
    b_in = nrm(ks[8], (DEPTH, D_IN_PROJ), 0.02)
    attn_sinks = nrm(ks[9], (DEPTH, SWA_Q_HEADS), 0.5)
    rel_bias_table = nrm(ks[10], (REL_BUCKETS, N_REL_HEADS), 0.2)
    w_branch_swa = nrm(ks[11], (DEPTH, SWA_Q_W, D_MODEL), SWA_Q_W ** -0.5 * beta)
    w_branch_moba = nrm(ks[12], (DEPTH, MOBA_W, D_MODEL), MOBA_W ** -0.5 * beta)
    w_out = nrm(ks[13], (DEPTH, D_MODEL, D_MODEL), s_d * beta)
    ln1_gain = 1.0 + nrm(ks[14], (DEPTH, D_MODEL), 0.02)
    ln1_bias = nrm(ks[15], (DEPTH, D_MODEL), 0.02)
    w_group_router = nrm(ks[16], (DEPTH, D_MODEL, N_GROUPS), s_d)
    b_group_router = nrm(ks[17], (DEPTH, N_GROUPS), 0.01)
    w_expert_router = nrm(ks[18], (DEPTH, D_MODEL, N_EXPERTS), s_d)
    b_expert_router = nrm(ks[19], (DEPTH, N_EXPERTS), 0.01)
    w_expert_gate = nrm(ks[20], (DEPTH, N_EXPERTS, D_MODEL, D_EXPERT), s_d * beta)
    w_expert_up = nrm(ks[21], (DEPTH, N_EXPERTS, D_MODEL, D_EXPERT), s_d * beta)
    w_expert_down = nrm(ks[22], (DEPTH, N_EXPERTS, D_EXPERT, D_MODEL), D_EXPERT ** -0.5 * beta)
    k_ln = jax.random.split(ks[23], 2)
    ln2_gain = 1.0 + nrm(k_ln[0], (DEPTH, D_MODEL), 0.02)
    ln2_bias = nrm(k_ln[1], (DEPTH, D_MODEL), 0.02)
    return {"x": x, "w_in": w_in, "b_in": b_in, "attn_sinks": attn_sinks,
            "rel_bias_table": rel_bias_table, "w_branch_swa": w_branch_swa,
            "w_branch_moba": w_branch_moba, "w_out": w_out, "ln1_gain": ln1_gain,
            "ln1_bias": ln1_bias, "w_group_router": w_group_router,
            "b_group_router": b_group_router, "w_expert_router": w_expert_router,
            "b_expert_router": b_expert_router, "w_expert_gate": w_expert_gate,
            "w_expert_up": w_expert_up, "w_expert_down": w_expert_down,
            "ln2_gain": ln2_gain, "ln2_bias": ln2_bias}


def reference(x, w_in, b_in, attn_sinks, rel_bias_table, w_branch_swa, w_branch_moba, w_out,
              ln1_gain, ln1_bias, w_group_router, b_group_router, w_expert_router, b_expert_router,
              w_expert_gate, w_expert_up, w_expert_down, ln2_gain, ln2_bias):
    rel_a = rel_bias_table[:, :SWA_Q_HEADS]
    rel_b = rel_bias_table[:, SWA_Q_HEADS:]
    for layer in range(DEPTH):
        proj = x @ w_in[layer] + b_in[layer]
        y_a = sliding_window_attention(proj[..., OFF_SWA_Q:OFF_SWA_K], proj[..., OFF_SWA_K:OFF_SWA_V],
                                       proj[..., OFF_SWA_V:OFF_MOBA_Q], attn_sinks[layer], rel_a)
        y_b = moba_attention(proj[..., OFF_MOBA_Q:OFF_MOBA_K], proj[..., OFF_MOBA_K:OFF_MOBA_V],
                             proj[..., OFF_MOBA_V:OFF_GATE], rel_b)
        gates = jax.nn.sigmoid(proj[..., OFF_GATE:])
        merged = (gates[..., :D_MODEL] * (y_a @ w_branch_swa[layer])
                  + gates[..., D_MODEL:] * (y_b @ w_branch_moba[layer]))
        mixed = merged @ w_out[layer]
        x = layer_norm(DEEPNORM_ALPHA * x + mixed, ln1_gain[layer], ln1_bias[layer])
        moe = hierarchical_moe(x, w_group_router[layer], b_group_router[layer], w_expert_router[layer],
                               b_expert_router[layer], w_expert_gate[layer], w_expert_up[layer],
                               w_expert_down[layer])
        x = layer_norm(DEEPNORM_ALPHA * x + moe, ln2_gain[layer], ln2_bias[layer])
    return x
```

```python
from contextlib import ExitStack
import os


class StopBuild(Exception):
    pass


def chk(n):
    if int(os.environ.get('KSTOP', '99')) == n:
        raise StopBuild()

import numpy as np
import concourse.bass as bass
import concourse.mybir as mybir
from concourse.bass_utils import run_bass_kernel_spmd

F32 = mybir.dt.float32
BF16 = mybir.dt.bfloat16
ALU = mybir.AluOpType
AF = mybir.ActivationFunctionType
AX = mybir.AxisListType

D = 1024
NCORES = 8
BIG = 30000.0
ALPHA = 2.0 ** 0.25
EPS = 1e-5
NEXP = 32
DE = 512


class Res:
    __slots__ = ("name", "w", "r", "dsem", "dcount", "excl")

    def __init__(self, name, excl=False):
        self.name = name
        self.w = None
        self.r = {}
        self.dsem = None
        self.dcount = 0
        self.excl = excl


class Sched:
    ENGS = ("pe", "act", "dve", "pool", "sp")

    def __init__(self, nc, stack):
        self.nc = nc
        self.ops = {e: [] for e in self.ENGS}
        self.cnt = {e: 0 for e in self.ENGS}
        self.seen = {e: {} for e in self.ENGS}
        self.sems = {}
        self.dma_keys = {}
        self._stack = stack

    def _sem(self, key):
        s = self.sems.get(key)
        if s is None:
            s = self._stack.enter_context(self.nc.semaphore("s_" + str(key)))
            self.sems[key] = s
        return s

    def _collect(self, eng, reads, writes):
        need = {}

        def add(tok):
            if tok is None:
                return
            k, v = tok
            if k == eng and eng == "pe":
                return
            if need.get(k, 0) < v:
                need[k] = v
        for r in reads:
            add(r.w)
            if r.excl:
                for k, v in r.r.items():
                    add((k, v))
        for w in writes:
            add(w.w)
            for k, v in w.r.items():
                add((k, v))
        waits = []
        seen = self.seen[eng]
        for k, v in need.items():
            if seen.get(k, 0) < v:
                seen[k] = v
                waits.append((k, v))
        return waits

    def _update(self, tok, reads, writes):
        for r in reads:
            if r.excl:
                r.w = tok
                r.r = {}
            elif r.r.get(tok[0], 0) < tok[1]:
                r.r[tok[0]] = tok[1]
        for w in writes:
            w.w = tok
            w.r = {}

    def op(self, eng, fn, reads=(), writes=()):
        waits = self._collect(eng, reads, writes)
        self.cnt[eng] += 1
        tok = (eng, self.cnt[eng])
        self._sem(eng)
        for k, _ in waits:
            self._sem(k)
        self.ops[eng].append((waits, fn, (eng, 1)))
        self._update(tok, reads, writes)
        return tok

    def raw(self, eng, fn, reads=()):
        waits = self._collect(eng, reads, ())
        for k, _ in waits:
            self._sem(k)
        self.ops[eng].append((waits, fn, "raw"))

    def dma(self, q, fn, reads=(), writes=(), sem_res=None):
        waits = self._collect(q, reads, writes)
        sr = sem_res if sem_res is not None else writes[0]
        if sr.dsem is None:
            sr.dsem = "d_" + sr.name
        sr.dcount += 16
        self.dma_keys[sr.dsem] = sr.dcount
        tok = (sr.dsem, sr.dcount)
        self._sem(sr.dsem)
        for k, _ in waits:
            self._sem(k)
        self.ops[q].append((waits, fn, (sr.dsem, 16)))
        self._update(tok, reads, writes)
        return tok

    def barrier(self):
        for e in self.ENGS:
            waits = []
            seen = self.seen[e]
            for k in self.ENGS:
                v = self.cnt[k]
                if k != e and v > 0 and seen.get(k, 0) < v:
                    seen[k] = v
                    waits.append((k, v))
            for k, v in self.dma_keys.items():
                if seen.get(k, 0) < v:
                    seen[k] = v
                    waits.append((k, v))
            self.ops[e].append((waits, None, None))

    def emit(self, block):
        emap = {"pe": block.tensor, "act": block.scalar, "dve": block.vector,
                "pool": block.gpsimd, "sp": block.sync}
        for ename in self.ENGS:
            ops = self.ops[ename]
            if not ops:
                continue

            def body(e, ops=ops):
                for waits, fn, inc in ops:
                    for k, v in waits:
                        e.wait_ge(self.sems[k], v)
                    if fn is None:
                        continue
                    if inc == "raw":
                        fn(e)
                    else:
                        fn(e).then_inc(self.sems[inc[0]], inc[1])
            emap[ename](body)
            self.ops[ename] = []


def rel_bucket_np(dist):
    n = np.maximum(dist, 0)
    nf = np.maximum(n, 1).astype(np.float32)
    large = 16 + (np.log(nf / np.float32(16)) / np.float32(np.log(8.0)) * 16).astype(np.int32)
    large = np.minimum(large, 31)
    return np.where(n < 16, n, large)


def build(NSEQ, S):
    NT = NSEQ * S
    NCH = S // 512
    NTT = S // 128
    nc = bass.Bass("TRN2", target_bir_lowering=False)
    dt = lambda name, shape, ty, kind="ExternalInput": nc.dram_tensor(name, shape, ty, kind=kind).ap()
    x_tok = dt("x_tok", [NT, D], F32)
    x_T = dt("x_T", [D, NT], F32)
    w_in = dt("w_in", [D, 4352], F32)
    bqk = dt("bqk", [128, 13], F32)
    bg = dt("bg", [128, 16], F32)
    bv = dt("bv", [1, 640], F32)
    sinks = dt("sinks", [1, 8], F32)
    t31 = dt("t31", [1, 8], F32)
    ga_raw = dt("ga_raw", [128, 2 * 8 * 128], F32)
    gb_raw = dt("gb_raw", [128, 8 * 1024], F32)
    wa_d = dt("wa", [512, D], F32)
    wb_d = dt("wb", [512, D], F32)
    wo_d = dt("wo", [D, D], F32)
    ln_d = dt("ln", [4, D], F32)
    wr_d = dt("wr", [D, 36], F32)
    br_d = dt("br", [1, 36], F32)
    wg_d = dt("wg", [NEXP, D, DE], F32)
    wu_d = dt("wu", [NEXP, D, DE], F32)
    wd_d = dt("wd", [NEXP, DE, D], F32)
    out_d = dt("out", [NT, D], F32, kind="ExternalOutput")
    x1_d = dt("x1_scr", [NT, D], F32, kind="Internal")
    winb_d = dt("winb_scr", [128, 8 * (4352 + D)], BF16, kind="Internal")
    x1T_d = dt("x1T_scr", [D, NT], BF16, kind="Internal")
    wgb_d = dt("wgb_scr", [NEXP, 128, 8 * DE], BF16, kind="Internal")
    wub_d = dt("wub_scr", [NEXP, 128, 8 * DE], BF16, kind="Internal")
    wdb_d = dt("wdb_scr", [NEXP, 128, 4 * D], BF16, kind="Internal")
    xbuf_d = dt("xbuf_scr", [(2 * NT) // 256 * 256 + NEXP * 256, D], BF16, kind="Internal")
    ybuf_d = dt("ybuf_scr", [(2 * NT) // 256 * 256 + NEXP * 256, D], F32, kind="Internal")

    with ExitStack() as top:
        S_ = Sched(nc, top)
        op, dma = S_.op, S_.dma
        psS = [top.enter_context(nc.psum_tensor(f"psS{i}", [128, 512], F32)) for i in range(4)]
        rS = [Res(f"psS{i}", excl=True) for i in range(4)]
        psO = [top.enter_context(nc.psum_tensor(f"psO{i}", [128, 512], F32)) for i in range(2)]
        rO = [Res(f"psO{i}", excl=True) for i in range(2)]
        psT = [top.enter_context(nc.psum_tensor(f"psT{i}", [128, 1024], BF16)) for i in range(2)]
        rT = [Res(f"psT{i}", excl=True) for i in range(2)]
        ctr = {"S": 0, "O": 0, "T": 0}

        def nxt(kind):
            lst, rl = {"S": (psS, rS), "O": (psO, rO), "T": (psT, rT)}[kind]
            i = ctr[kind] % len(lst)
            ctr[kind] += 1
            return lst[i], rl[i]

        r_x1 = Res("x1_scr")
        r_x1s = [Res("x1s0"), Res("x1s1")]
        r_x1T = Res("x1T_scr")
        r_cw = [Res("cw0"), Res("cw1")]
        r_out = Res("out")

        with ExitStack() as st:
            sb = lambda name, shape, ty: st.enter_context(nc.sbuf_tensor(name, shape, ty))
            ident = sb("ident", [128, 128], BF16); r_ident = Res("ident")
            identf = sb("identf", [128, 128], F32); r_identf = Res("identf")
            op("pool", lambda e: e.memset(identf[:], 0.0), writes=[r_identf])
            op("pool", lambda e: e.affine_select(out=identf[:], in_=identf[:], pattern=[[-1, 128]],
                                                 compare_op=ALU.not_equal, fill=1.0, base=0,
                                                 channel_multiplier=1), reads=[r_identf], writes=[r_identf])
            op("dve", lambda e: e.tensor_copy(out=ident[:], in_=identf[:]), reads=[r_identf], writes=[r_ident])
            bqk_t = sb("bqk_t", [128, 13], F32); r_bqk = Res("bqk")
            bg_t = sb("bg_t", [128, 16], F32); r_bg = Res("bg")
            bv_t = sb("bv_t", [128, 640], F32); r_bv = Res("bv")
            es_t = sb("es_t", [128, 8], F32); r_es = Res("es")
            t31_t = sb("t31_t", [128, 8], F32); r_t31 = Res("t31")
            ln_t = sb("ln_t", [128, 2, D], F32); r_ln = Res("ln")
            dma("sp", lambda e: e.dma_start(out=bqk_t[:], in_=bqk), writes=[r_bqk])
            dma("sp", lambda e: e.dma_start(out=bg_t[:], in_=bg), writes=[r_bg])
            dma("sp", lambda e: e.dma_start(out=bv_t[:], in_=bv.partition_broadcast(128)), writes=[r_bv])
            dma("sp", lambda e: e.dma_start(out=es_t[:], in_=sinks.partition_broadcast(128)), writes=[r_es])
            dma("sp", lambda e: e.dma_start(out=t31_t[:], in_=t31.partition_broadcast(128)), writes=[r_t31])
            for i in range(2):
                dma("sp", lambda e, i=i: e.dma_start(out=ln_t[:, i, :], in_=ln_d[i:i + 1, :].partition_broadcast(128)), writes=[r_ln])
            op("act", lambda e: e.activation(out=es_t[:], in_=es_t[:], func=AF.Exp), reads=[r_es], writes=[r_es])
            op("dve", lambda e: e.tensor_scalar(out=bqk_t[:, 0:4], in0=bqk_t[:, 0:4], scalar1=0.125, scalar2=None, op0=ALU.mult), reads=[r_bqk], writes=[r_bqk])
            op("dve", lambda e: e.tensor_scalar(out=bqk_t[:, 5:9], in0=bqk_t[:, 5:9], scalar1=0.125, scalar2=None, op0=ALU.mult), reads=[r_bqk], writes=[r_bqk])
            PMr = sb("PMr", [128, 8, 32], F32); r_PMr = Res("PMr")
            OWr = sb("OWr", [128, 8, 32], F32); r_OWr = Res("OWr")
            op("pool", lambda e: e.memset(PMr[:, :, 0:16], 0.0), writes=[r_PMr])
            op("pool", lambda e: e.memset(PMr[:, :, 16:32], -3.0e38), reads=[r_PMr], writes=[r_PMr])
            op("pool", lambda e: e.memset(OWr[:], 0.0), writes=[r_OWr])
            op("pool", lambda e: e.memset(OWr[:, :, 16:17], 1.0), reads=[r_OWr], writes=[r_OWr])
            Gs = sb("Gs", [128, 2, 8, 128], BF16); r_Gs = Res("Gs")
            dma("pool", lambda e: e.dma_start(out=Gs[:].rearrange("p a h q -> p (a h q)"), in_=ga_raw), writes=[r_Gs])
            op("pool", lambda e: e.affine_select(out=Gs[:, 0, :, :], in_=Gs[:, 0, :, :], pattern=[[0, 8], [1, 128]],
                                                 compare_op=ALU.is_ge, fill=-BIG, base=0, channel_multiplier=-1),
               reads=[r_Gs], writes=[r_Gs])
            op("pool", lambda e: e.affine_select(out=Gs[:, 1, :, :], in_=Gs[:, 1, :, :], pattern=[[0, 8], [-1, 128]],
                                                 compare_op=ALU.is_gt, fill=-BIG, base=0, channel_multiplier=1),
               reads=[r_Gs], writes=[r_Gs])
            Gb = sb("Gb", [128, 8, 640], BF16); r_Gb = Res("Gb")
            xres = [sb(f"xres{i}", [128, D], F32) for i in range(2)]
            r_xres = [Res(f"xres{i}") for i in range(2)]
            gtmp = xres[0]; r_gtmp = r_xres[0]
            for h in range(8):
                dma("sp", lambda e, h=h: e.dma_start(out=gtmp[:, 0:640], in_=gb_raw[:, h * 1024:h * 1024 + 640]), writes=[r_gtmp])
                op("dve", lambda e, h=h: e.tensor_scalar(out=Gb[:, h, :], in0=gtmp[:, 0:640], scalar1=t31_t[:, h:h + 1], scalar2=None,
                                                         op0=ALU.subtract), reads=[r_gtmp, r_t31], writes=[r_Gb])
            op("pool", lambda e: e.affine_select(out=Gb[:], in_=Gb[:], pattern=[[0, 8], [1, 640]],
                                                 compare_op=ALU.is_ge, fill=-BIG, base=-384, channel_multiplier=-1),
               reads=[r_Gb], writes=[r_Gb])
            wa = sb("wa_t", [128, 4, D], BF16); r_wa = Res("wa")
            wb = sb("wb_t", [128, 4, D], BF16); r_wb = Res("wb")
            dma("pool", lambda e: e.dma_start(out=wa[:], in_=wa_d.rearrange("(k p) n -> p k n", p=128)), writes=[r_wa])
            dma("pool", lambda e: e.dma_start(out=wb[:], in_=wb_d.rearrange("(k p) n -> p k n", p=128)), writes=[r_wb])
            KmT = sb("KmT", [128, 4, S], BF16); r_Km = [Res(f"Km{c}") for c in range(NCH)]
            KsT = sb("KsT", [128, 1024], BF16); r_Ks = [Res(f"Ks{c}") for c in range(2)]
            Vm = sb("Vm", [128, NTT, 8, 65], BF16); r_Vm = [Res(f"Vm{c}") for c in range(NCH)]
            Vs = sb("Vs", [128, 8, 2, 65], BF16); r_Vs = [Res(f"Vs{c}") for c in range(2)]
            kmT = sb("kmT", [128, 4, 16], BF16); r_km = Res("kmT")
            kms = sb("kms", [128, 4, 2], F32); r_kms = Res("kms")
            op("pool", lambda e: e.memset(Vm[:, :, :, 64:65], 1.0), writes=r_Vm)
            op("pool", lambda e: e.memset(Vs[:, :, :, 64:65], 1.0), writes=r_Vs)
            op("pool", lambda e: e.memset(kmT[:], 0.0), writes=[r_km])
            zero_q = True
            wbuf = [sb(f"wbuf{i}", [128, 8 * 768], BF16) for i in range(2)]
            wview = lambda i, n: wbuf[i][:, 0:8 * n].rearrange("p (k n) -> p k n", k=8)
            r_wbuf = [Res(f"wbuf{i}") for i in range(2)]
            wctr = [0]
            cbuf = [sb(f"cbuf{i}", [128, 1024], BF16) for i in range(2)]
            r_cbuf = [Res(f"cbuf{i}") for i in range(2)]
            conv_list = []
            for ex in range(NEXP):
                for (src, dst, K_) in ((wg_d, wgb_d, 8), (wu_d, wub_d, 8), (wd_d, wdb_d, 4)):
                    for half in range(4):
                        conv_list.append((src, dst, K_, ex, half))
            conv_state = {"next": 0, "pending": None}
            n_slots = NSEQ * NCH * 8
            conv_per_slot = -(-len(conv_list) // n_slots)

            def conv_flush():
                p_ = conv_state["pending"]
                if p_ is not None:
                    i, dst, ex, half = p_
                    dma("sp", lambda e, i=i, dst=dst, ex=ex, half=half: e.dma_start(out=dst[ex][:, half * 1024:(half + 1) * 1024], in_=cbuf[i][:]),
                        reads=[r_cbuf[i]], writes=[], sem_res=r_cw[i])
                    conv_state["pending"] = None

            def conv_step():
                for _ in range(conv_per_slot):
                    conv_flush()
                    n_ = conv_state["next"]
                    if n_ >= len(conv_list):
                        return
                    src, dst, K_, ex, half = conv_list[n_]
                    conv_state["next"] = n_ + 1
                    i = n_ % 2
                    dma("pool", lambda e, i=i, src=src, ex=ex, K_=K_, half=half: e.dma_start(
                        out=cbuf[i][:].rearrange("p (k n) -> p k n", k=K_ // 4),
                        in_=src[ex].rearrange("(k p) n -> p k n", p=128)[:, half * (K_ // 4):(half + 1) * (K_ // 4), :]),
                        writes=[r_cbuf[i]])
                    conv_state["pending"] = (i, dst, ex, half)
            xTc = [sb(f"xTc{i}", [128, 8, 512], BF16) for i in range(2)]
            r_xTc = [Res(f"xTc{i}") for i in range(2)]
            QsT = sb("QsT", [128, 4, 512], BF16); r_Qs = Res("QsT")
            QmT = sb("QmTz", [128, 8, 512], BF16); r_Qm = Res("QmT")
            op("pool", lambda e: e.memset(QmT[:], 0.0), writes=[r_Qm])
            gm = sb("gm", [128, 8, 16], F32); r_gm = Res("gm")
            mx = sb("mx", [128, 8, 8], F32); r_mx = Res("mx")
            thr = sb("thr", [128, 8], F32); r_thr = Res("thr")
            sel = sb("sel", [128, 8, 16], F32); r_sel = Res("sel")
            madd4 = [sb(f"madd{i}", [128, 128], BF16) for i in range(4)]; r_madd4 = [Res(f"madd{i}") for i in range(4)]
            maddT = sb("maddT", [128, 512], BF16); r_maddT = Res("maddT")
            PT = [sb(f"PT{i}", [128, 512], BF16) for i in range(3)]
            r_PT = [Res(f"PT{i}") for i in range(3)]
            pctr = [0]
            rden = sb("rden", [128, 4], F32); r_rden = Res("rden")
            ytok = sb("ytok", [128, 4, D], BF16); r_ytok = Res("ytok")
            yT = sb("yT", [128, 8, 512], BF16); r_yT = Res("yT")
            mT = ytok[:].rearrange("p a (b c) -> p (a b) c", c=512); r_mT = r_ytok
            g1 = sb("g1", [128, 512], F32); r_g1 = Res("g1")
            g2 = sb("g2", [128, 512], F32); r_g2 = Res("g2")
            t1 = g1; r_t1 = r_g1
            t2 = g2; r_t2 = r_g2
            z = xres
            r_z = r_xres
            stats_l = [sb(f"stats{i}", [128, 2, 6], F32) for i in range(2)]; r_stats_l = [Res(f"stats{i}") for i in range(2)]
            mv_l = [sb(f"mv{i}", [128, 2], F32) for i in range(2)]; r_mv_l = [Res(f"mv{i}") for i in range(2)]
            rstd_l = [sb(f"rstd{i}", [128, 1], F32) for i in range(2)]; r_rstd_l = [Res(f"rstd{i}") for i in range(2)]

            def load_w(c0, ncols, dst0=0, new=True, conv=True):
                if new:
                    wctr[0] += 1
                i = wctr[0] % len(wbuf)
                dma("sp", lambda e: e.dma_start(out=wbuf[i][:, 0:8 * ncols], in_=winb_d[:, 8 * c0:8 * c0 + 8 * ncols]),
                    writes=[r_wbuf[i]])
                return wview(i, ncols), r_wbuf[i]

            def layer_norm(zt, r_zt, gi):
                stats, r_stats, mv, r_mv, rstd, r_rstd = stats_l[gi], r_stats_l[gi], mv_l[gi], r_mv_l[gi], rstd_l[gi], r_rstd_l[gi]
                for hh in range(2):
                    op("dve", lambda e, hh=hh: e.bn_stats(out=stats[:, hh, :], in_=zt[:, hh * 512:(hh + 1) * 512]),
                       reads=[r_zt], writes=[r_stats])
                op("dve", lambda e: e.bn_aggr(out=mv[:], in_=stats[:].rearrange("p a b -> p (a b)")), reads=[r_stats], writes=[r_mv])
                op("act", lambda e: e.activation(out=rstd[:], in_=mv[:, 1:2], func=AF.Sqrt, bias=EPS, scale=1.0),
                   reads=[r_mv], writes=[r_rstd])
                op("dve", lambda e: e.reciprocal(out=rstd[:], in_=rstd[:]), reads=[r_rstd], writes=[r_rstd])
                op("dve", lambda e: e.tensor_scalar(out=mv[:, 1:2], in0=mv[:, 0:1], scalar1=rstd[:, 0:1], scalar2=-1.0,
                                                    op0=ALU.mult, op1=ALU.mult), reads=[r_mv, r_rstd], writes=[r_mv])
                op("act", lambda e: e.activation(out=zt[:], in_=zt[:], func=AF.Identity, bias=mv[:, 1:2], scale=rstd[:, 0:1]),
                   reads=[r_zt, r_mv, r_rstd], writes=[r_zt])
                return gi

            r_winb = Res("winb")
            groups = [([(w_in, 0, 768, 0)], 768, 0), ([(w_in, 768, 512, 0)], 512, 768), ([(w_in, 1280, 512, 0)], 512, 1280),
                      ([(w_in, 1792, 512, 0)], 512, 1792)]
            for jp in range(4):
                groups.append(([(w_in, 2304 + jp * 256, 256, 0), (w_in, 3328 + jp * 256, 256, 256)], 512, 2304 + jp * 512))
            groups += [([(wo_d, 0, 512, 0)], 512, 4352), ([(wo_d, 512, 512, 0)], 512, 4864)]
            for gi_, (pieces, n_, d0_) in enumerate(groups):
                i_ = gi_ % 2
                for (src_, c0_, pn_, po_) in pieces:
                    dma("pool", lambda e, i_=i_, src_=src_, c0_=c0_, pn_=pn_, po_=po_, n_=n_: e.dma_start(
                        out=wview(i_, n_)[:, :, po_:po_ + pn_], in_=src_[:, c0_:c0_ + pn_].rearrange("(k p) n -> p k n", p=128)), writes=[r_wbuf[i_]])
                dma("sp", lambda e, i_=i_, n_=n_, d0_=d0_: e.dma_start(out=winb_d[:, 8 * d0_:8 * d0_ + 8 * n_], in_=wbuf[i_][:, 0:8 * n_]),
                    reads=[r_wbuf[i_]], writes=[], sem_res=r_winb)
            S_.barrier()
            pending_wout = []
            def wout_section(T0):
                woh = [load_w(4352, 512, conv=False), load_w(4864, 512, conv=False)]
                for tt in range(4):
                    zi = tt % 2
                    dma("sp", lambda e, zi=zi, tt=tt, T0=T0: e.dma_start(out=xres[zi][:], in_=x_tok[T0 + tt * 128:T0 + (tt + 1) * 128, :]),
                        writes=[r_xres[zi]])
                    for hh in range(2):
                        ps, rp = nxt("S")
                        for k in range(8):
                            op("pe", lambda e, ps=ps, k=k, tt=tt, hh=hh, woh=woh: e.matmul(ps[:], lhsT=mT[:, k, tt * 128:(tt + 1) * 128],
                                                                                  rhs=woh[hh][0][:, k, 0:512], start=(k == 0), stop=(k == 7)),
                               reads=[r_mT, woh[hh][1]], writes=[rp])
                        op("dve", lambda e, ps=ps, zi=zi, hh=hh: e.scalar_tensor_tensor(
                            out=z[zi][:, hh * 512:(hh + 1) * 512], in0=xres[zi][:, hh * 512:(hh + 1) * 512], scalar=ALPHA, in1=ps[:],
                            op0=ALU.mult, op1=ALU.add), reads=[rp, r_xres[zi]], writes=[r_xres[zi]])
                    layer_norm(z[zi], r_z[zi], zi)
                    op("pool", lambda e, zi=zi: e.tensor_tensor(out=z[zi][:], in0=z[zi][:], in1=ln_t[:, 0, :], op=ALU.mult),
                       reads=[r_z[zi], r_ln], writes=[r_z[zi]])
                    op("pool", lambda e, zi=zi: e.tensor_tensor(out=z[zi][:], in0=z[zi][:], in1=ln_t[:, 1, :], op=ALU.add),
                       reads=[r_z[zi], r_ln], writes=[r_z[zi]])
                    dma("pool", lambda e, zi=zi, tt=tt, T0=T0: e.dma_start(out=x1_d[T0 + tt * 128:T0 + (tt + 1) * 128, :], in_=z[zi][:]),
                        reads=[r_z[zi]], writes=[], sem_res=r_x1s[zi])

            try:
              chk(1)
              for s in range(NSEQ):
                for c in range(NCH):
                    T0 = s * S + c * 512
                    gidx = s * NCH + c
                    xi = gidx % 2
                    xt, r_xt = xTc[xi], r_xTc[xi]

                    def load_xT(g_):
                        b_ = xTc[g_ % 2]
                        t0_ = g_ * 512
                        dma("pool", lambda e: e.dma_start(out=b_[:], in_=x_T[:, t0_:t0_ + 512].rearrange("(k p) n -> p k n", p=128)),
                            writes=[r_xTc[g_ % 2]])

                    if gidx == 0:
                        load_xT(0)
                    wA, r_wA = load_w(0, 768, conv=False)
                    for m in range(5):
                        ps, rp = nxt("S")
                        for k in range(8):
                            op("pe", lambda e, ps=ps, k=k, m=m, wA=wA, xt=xt: e.matmul(
                                ps[:], lhsT=wA[:, k, m * 128:(m + 1) * 128], rhs=xt[:, k, :], start=(k == 0), stop=(k == 7)),
                               reads=[r_wA, r_xt], writes=[rp])
                        if m < 4:
                            op("act", lambda e, ps=ps, m=m: e.activation(out=QsT[:, m, :], in_=ps[:], func=AF.Identity,
                                                                         bias=bqk_t[:, m:m + 1], scale=0.125),
                               reads=[rp, r_bqk], writes=[r_Qs])
                        else:
                            op("act", lambda e, ps=ps, c=c: e.activation(out=KsT[:, (c % 2) * 512:(c % 2 + 1) * 512], in_=ps[:], func=AF.Identity,
                                                                         bias=bqk_t[:, 4:5], scale=1.0),
                               reads=[rp, r_bqk], writes=[r_Ks[c % 2]])
                    for tt in range(4):
                        ps, rp = nxt("S")
                        for k in range(8):
                            op("pe", lambda e, ps=ps, k=k, tt=tt, wA=wA, xt=xt: e.matmul(
                                ps[:, 0:128], lhsT=xt[:, k, tt * 128:(tt + 1) * 128], rhs=wA[:, k, 640:768], start=(k == 0), stop=(k == 7)),
                               reads=[r_wA, r_xt], writes=[rp])
                        op("dve", lambda e, ps=ps, tt=tt, c=c: e.tensor_tensor(
                            out=Vs[:, (c % 2) * 4 + tt, :, 0:64], in0=ps[:, 0:128].rearrange("p (g d) -> p g d", g=2),
                            in1=bv_t[:, 0:128].rearrange("p (g d) -> p g d", g=2), op=ALU.add),
                           reads=[rp, r_bv], writes=[r_Vs[c % 2]])
                    wC, r_wC = load_w(768, 512, conv=False)
                    for m in range(4):
                        ps, rp = nxt("S")
                        for k in range(8):
                            op("pe", lambda e, ps=ps, k=k, m=m, wC=wC, xt=xt: e.matmul(
                                ps[:], lhsT=wC[:, k, m * 128:(m + 1) * 128], rhs=xt[:, k, :], start=(k == 0), stop=(k == 7)),
                               reads=[r_wC, r_xt], writes=[rp])
                        op("act", lambda e, ps=ps, m=m: e.activation(out=QmT[0:64, 2 * m, :], in_=ps[0:64, :], func=AF.Identity,
                                                                     bias=bqk_t[0:64, 5 + m:6 + m], scale=0.125),
                           reads=[rp, r_bqk], writes=[r_Qm])
                        op("act", lambda e, ps=ps, m=m: e.activation(out=QmT[64:128, 2 * m + 1, :], in_=ps[64:128, :], func=AF.Identity,
                                                                     bias=bqk_t[64:128, 5 + m:6 + m], scale=0.125),
                           reads=[rp, r_bqk], writes=[r_Qm])
                    wD, r_wD = load_w(1280, 512)
                    for m in range(4):
                        ps, rp = nxt("S")
                        for k in range(8):
                            op("pe", lambda e, ps=ps, k=k, m=m, wD=wD, xt=xt: e.matmul(
                                ps[:], lhsT=wD[:, k, m * 128:(m + 1) * 128], rhs=xt[:, k, :], start=(k == 0), stop=(k == 7)),
                               reads=[r_wD, r_xt], writes=[rp])
                        op("act", lambda e, ps=ps, m=m, c=c: e.activation(out=KmT[:, m, c * 512:(c + 1) * 512], in_=ps[:], func=AF.Identity,
                                                                          bias=bqk_t[:, 9 + m:10 + m], scale=1.0),
                           reads=[rp, r_bqk], writes=[r_Km[c]])
                    wE, r_wE = load_w(1792, 512)
                    for tt in range(4):
                        ps, rp = nxt("S")
                        for k in range(8):
                            op("pe", lambda e, ps=ps, k=k, tt=tt, wE=wE, xt=xt: e.matmul(
                                ps[:], lhsT=xt[:, k, tt * 128:(tt + 1) * 128], rhs=wE[:, k, 0:512], start=(k == 0), stop=(k == 7)),
                               reads=[r_wE, r_xt], writes=[rp])
                        op("dve", lambda e, ps=ps, tt=tt, c=c: e.tensor_tensor(
                            out=Vm[:, c * 4 + tt, :, 0:64], in0=ps[:].rearrange("p (g d) -> p g d", g=8),
                            in1=bv_t[:, 128:640].rearrange("p (g d) -> p g d", g=8), op=ALU.add),
                           reads=[rp, r_bv], writes=[r_Vm[c]])
                    chk(2)
                    op("dve", lambda e, c=c: e.tensor_reduce(out=kms[:], in_=KmT[:, :, c * 512:(c + 1) * 512].rearrange("p m (b t) -> p m b t", b=2),
                                                             axis=AX.X, op=ALU.add), reads=[r_Km[c]], writes=[r_kms])
                    op("dve", lambda e, c=c: e.tensor_scalar(out=kmT[:, :, 2 * c:2 * c + 2], in0=kms[:], scalar1=1.0 / 256.0, scalar2=None,
                                                             op0=ALU.mult), reads=[r_kms], writes=[r_km])
                    while pending_wout:
                        wout_section(pending_wout.pop(0))
                    if gidx + 1 < NSEQ * NCH:
                        load_xT(gidx + 1)
                    chk(3)
                    for tt in range(4):
                        qb = 2 * c + tt // 2
                        pse, rpe = nxt("S")
                        pso, rpo_ = nxt("S")
                        for h in (0, 2, 4, 6, 1, 3, 5, 7):
                            hb = (h % 2) * 64
                            pp, rpp = (pse, rpe) if h % 2 == 0 else (pso, rpo_)
                            op("pe", lambda e, pp=pp, h=h, hb=hb, tt=tt: e.matmul(
                                pp[:, (h // 2) * 16:(h // 2 + 1) * 16], lhsT=QmT[hb:hb + 64, h, tt * 128:(tt + 1) * 128],
                                rhs=kmT[hb:hb + 64, h // 2, :], start=True, stop=True),
                               reads=[r_Qm, r_km], writes=[rpp])
                        chk(31)
                        gm4 = gm[:].rearrange("p (a b) n -> p a b n", b=2)
                        pm4 = PMr[:, :, 16 - qb:32 - qb].rearrange("p (a b) n -> p a b n", b=2)
                        op("dve", lambda e, pse=pse, gm4=gm4, pm4=pm4: e.tensor_tensor(
                            out=gm4[:, :, 0, :], in0=pse[:, 0:64].rearrange("p (h n) -> p h n", h=4), in1=pm4[:, :, 0, :], op=ALU.add),
                           reads=[rpe, r_PMr], writes=[r_gm])
                        op("dve", lambda e, pso=pso, gm4=gm4, pm4=pm4: e.tensor_tensor(
                            out=gm4[:, :, 1, :], in0=pso[:, 0:64].rearrange("p (h n) -> p h n", h=4), in1=pm4[:, :, 1, :], op=ALU.add),
                           reads=[rpo_, r_PMr, r_gm], writes=[r_gm])
                        chk(32)
                        for h in range(8):
                            op("dve", lambda e, h=h: e.max(out=mx[:, h, :], in_=gm[:, h, :]), reads=[r_gm], writes=[r_mx])
                        chk(33)
                        op("dve", lambda e: e.tensor_scalar(out=thr[:], in0=mx[:, :, 2], scalar1=-1.0e30, scalar2=None, op0=ALU.max),
                           reads=[r_mx], writes=[r_thr])
                        chk(34)
                        op("dve", lambda e: e.tensor_tensor(out=sel[:], in0=gm[:], in1=thr[:].unsqueeze(2).to_broadcast([128, 8, 16]), op=ALU.is_ge),
                           reads=[r_gm, r_thr], writes=[r_sel])
                        op("dve", lambda e, qb=qb: e.tensor_tensor(out=sel[:], in0=sel[:], in1=OWr[:, :, 16 - qb:32 - qb], op=ALU.add),
                           reads=[r_sel, r_OWr], writes=[r_sel])
                        op("dve", lambda e: e.tensor_scalar(out=sel[:], in0=sel[:], scalar1=-1.0, scalar2=BIG, op0=ALU.add, op1=ALU.mult),
                           reads=[r_sel], writes=[r_sel])
                        op("dve", lambda e, tt=tt: e.tensor_tensor(out=madd4[tt][:].rearrange("p (h n) -> p h n", h=8), in0=sel[:],
                                                                   in1=t31_t[:].unsqueeze(2).to_broadcast([128, 8, 16]), op=ALU.add),
                           reads=[r_sel, r_t31], writes=[r_madd4[tt]])
                    chk(5)
                    items = []
                    for tt in range(4):
                        b = c * 4 + tt
                        for g in range(2):
                            whichs = [0] if b == 0 else [0, 1]
                            for wi, which in enumerate(whichs):
                                items.append((tt, g, wi, which, len(whichs), b))
                    obank = {}

                    def swa_S(it):
                        tt, g, wi, which, nw, b = it
                        if wi == 0:
                            obank[(tt, g)] = nxt("O")
                        kt = b - which
                        kc0 = ((kt // 4) % 2) * 512 + (kt % 4) * 128
                        ps, rp = nxt("S")
                        op("pe", lambda e, ps=ps, g=g, kc0=kc0, tt=tt: e.matmul(
                            ps[:], lhsT=KsT[g * 64:(g + 1) * 64, kc0:kc0 + 128],
                            rhs=QsT[g * 64:(g + 1) * 64, :, tt * 128:(tt + 1) * 128], start=True, stop=False),
                           reads=[r_Ks[(kt // 4) % 2], r_Qs], writes=[rp])
                        op("pe", lambda e, ps=ps, g=g, which=which: e.matmul(
                            ps[:], lhsT=ident[:], rhs=Gs[:, which, g * 4:(g + 1) * 4, :], start=False, stop=True),
                           reads=[r_ident, r_Gs], writes=[rp])
                        pi = pctr[0] % 3
                        pctr[0] += 1
                        op("act", lambda e, ps=ps, pi=pi: e.activation(out=PT[pi][:], in_=ps[:], func=AF.Exp),
                           reads=[rp], writes=[r_PT[pi]])
                        return pi

                    def swa_PV(it, pi):
                        tt, g, wi, which, nw, b = it
                        po, rpo = obank[(tt, g)]
                        kt = b - which
                        for j in range(4):
                            first = (wi == 0 and j == 0)
                            op("pe", lambda e, po=po, pi=pi, j=j, kt=kt, g=g, first=first, wi=wi, nw=nw: e.matmul(
                                po[:, j * 65:(j + 1) * 65], lhsT=PT[pi][:, j * 128:(j + 1) * 128], rhs=Vs[:, kt % 8, g, :],
                                start=first, stop=(wi == nw - 1), skip_group_check=True),
                               reads=[r_PT[pi], r_Vs[(kt // 4) % 2]], writes=[rpo])
                        if wi == nw - 1:
                            pov = po[:, 0:260].rearrange("p (t d) -> p t d", t=4)
                            op("dve", lambda e, pov=pov, g=g: e.tensor_tensor(out=rden[:], in0=pov[:, :, 64], in1=es_t[:, g * 4:(g + 1) * 4], op=ALU.add),
                               reads=[rpo, r_es], writes=[r_rden])
                            op("dve", lambda e: e.reciprocal(out=rden[:], in_=rden[:]), reads=[r_rden], writes=[r_rden])
                            op("dve", lambda e, pov=pov, g=g, tt=tt: e.tensor_tensor(
                                out=ytok[:, tt, g * 256:(g + 1) * 256].rearrange("p (j d) -> p j d", j=4), in0=pov[:, :, 0:64],
                                in1=rden[:].unsqueeze(2).to_broadcast([128, 4, 64]), op=ALU.mult),
                               reads=[rpo, r_rden], writes=[r_ytok])

                    prev = None
                    for it in items:
                        pi = swa_S(it)
                        if prev is not None:
                            swa_PV(*prev)
                        prev = (it, pi)
                    swa_PV(*prev)
                    for tt in range(4):
                        pt, rpt = nxt("T")
                        op("pe", lambda e, pt=pt, tt=tt: e.transpose(pt[:, 0:128], madd4[tt][:], ident[:]), reads=[r_madd4[tt], r_ident], writes=[rpt])
                        op("act", lambda e, pt=pt, tt=tt: e.activation(out=maddT[:, tt * 128:(tt + 1) * 128], in_=pt[:, 0:128], func=AF.Copy),
                           reads=[rpt], writes=[r_maddT])
                    chk(4)
                    nkt = 4 * c + 4
                    for h in range(8):
                        hb = (h % 2) * 64
                        po, rpo = nxt("O")

                        def emit_S(kt, h=h, hb=hb):
                            rel = kt - 4 * c
                            n = kt // 2
                            ps, rp = nxt("S")
                            last = rel < -1
                            op("pe", lambda e, ps=ps, kt=kt: e.matmul(
                                ps[:], lhsT=KmT[:, h // 2, kt * 128:(kt + 1) * 128], rhs=QmT[:, h, :],
                                start=True, stop=False), reads=[r_Km[kt // 4], r_Qm], writes=[rp])
                            p = h * 16 + n
                            op("pe", lambda e, ps=ps, p=p, last=last: e.matmul(
                                ps[:], lhsT=ident[:, p:p + 1].to_broadcast([128, 128]), rhs=maddT[:], start=False, stop=last),
                               reads=[r_ident, r_maddT], writes=[rp])
                            if not last:
                                off = 384 - 128 * rel
                                wid = min(512, 640 - off)
                                op("pe", lambda e, ps=ps, off=off, wid=wid: e.matmul(
                                    ps[:, 0:wid], lhsT=ident[:], rhs=Gb[:, h, off:off + wid], start=False, stop=True, skip_group_check=True),
                                   reads=[r_ident, r_Gb], writes=[rp])
                            pi = pctr[0] % 3
                            pctr[0] += 1
                            op("act", lambda e, ps=ps, pi=pi: e.activation(out=PT[pi][:], in_=ps[:], func=AF.Exp),
                               reads=[rp], writes=[r_PT[pi]])
                            return pi

                        def emit_PV(kt, pi, h=h, po=po, rpo=rpo):
                            for tt in range(4):
                                first = (kt == 0 and tt == 0)
                                op("pe", lambda e, pi=pi, tt=tt, kt=kt, first=first, nkt=nkt: e.matmul(
                                    po[:, tt * 65:(tt + 1) * 65], lhsT=PT[pi][:, tt * 128:(tt + 1) * 128], rhs=Vm[:, kt, h, :],
                                    start=first, stop=(kt == nkt - 1), skip_group_check=True),
                                   reads=[r_PT[pi], r_Vm[kt // 4]], writes=[rpo])

                        conv_step()
                        pend = []
                        for kt in range(nkt):
                            pi = emit_S(kt)
                            pend.append((kt, pi))
                            if len(pend) > 2:
                                emit_PV(*pend.pop(0))
                        while pend:
                            emit_PV(*pend.pop(0))
                        pov = po[:, 0:260].rearrange("p (t d) -> p t d", t=4)
                        op("dve", lambda e, pov=pov: e.reciprocal(out=rden[:], in_=pov[:, :, 64]), reads=[rpo], writes=[r_rden])
                        op("dve", lambda e, pov=pov, h=h: e.tensor_tensor(
                            out=ytok[:, :, 512 + h * 64:512 + (h + 1) * 64], in0=pov[:, :, 0:64],
                            in1=rden[:].unsqueeze(2).to_broadcast([128, 4, 64]), op=ALU.mult),
                           reads=[rpo, r_rden], writes=[r_ytok])
                    chk(6)
                    for j in range(8):
                        pt, rpt = nxt("T")
                        for tt in range(4):
                            op("pe", lambda e, pt=pt, tt=tt, j=j: e.transpose(pt[:, tt * 128:(tt + 1) * 128], ytok[:, tt, j * 128:(j + 1) * 128], ident[:]),
                               reads=[r_ytok, r_ident], writes=[rpt])
                        op("act", lambda e, pt=pt, j=j: e.activation(out=yT[:, j, :], in_=pt[:, 0:512], func=AF.Copy),
                           reads=[rpt], writes=[r_yT])
                    chk(7)
                    for j in range(8):
                        if j % 2 == 0:
                            wF1, r_wF1 = load_w(2304 + (j // 2) * 512, 512)
                            wF2, r_wF2 = wF1, r_wF1
                        jo = (j % 2) * 128
                        pa, rpa = nxt("S")
                        for k in range(4):
                            op("pe", lambda e, pa=pa, k=k, j=j: e.matmul(pa[:], lhsT=wa[:, k, j * 128:(j + 1) * 128], rhs=yT[:, k, :],
                                                                         start=(k == 0), stop=(k == 3)), reads=[r_wa, r_yT], writes=[rpa])
                        pb, rpb = nxt("S")
                        for k in range(4):
                            op("pe", lambda e, pb=pb, k=k, j=j: e.matmul(pb[:], lhsT=wb[:, k, j * 128:(j + 1) * 128], rhs=yT[:, 4 + k, :],
                                                                         start=(k == 0), stop=(k == 3)), reads=[r_wb, r_yT], writes=[rpb])
                        pg1, rpg1 = nxt("S")
                        for k in range(8):
                            op("pe", lambda e, pg1=pg1, k=k, jo=jo, wF1=wF1, xt=xt: e.matmul(pg1[:], lhsT=wF1[:, k, jo:jo + 128], rhs=xt[:, k, :],
                                                                                           start=(k == 0), stop=(k == 7)), reads=[r_wF1, r_xt], writes=[rpg1])
                        op("act", lambda e, pg1=pg1, j=j: e.activation(out=g1[:], in_=pg1[:], func=AF.Sigmoid, bias=bg_t[:, j:j + 1], scale=1.0),
                           reads=[rpg1, r_bg], writes=[r_g1])
                        pg2, rpg2 = nxt("S")
                        for k in range(8):
                            op("pe", lambda e, pg2=pg2, k=k, jo=jo, wF2=wF2, xt=xt: e.matmul(pg2[:], lhsT=wF2[:, k, 256 + jo:256 + jo + 128], rhs=xt[:, k, :],
                                                                                           start=(k == 0), stop=(k == 7)), reads=[r_wF2, r_xt], writes=[rpg2])
                        op("act", lambda e, pg2=pg2, j=j: e.activation(out=g2[:], in_=pg2[:], func=AF.Sigmoid, bias=bg_t[:, 8 + j:9 + j], scale=1.0),
                           reads=[rpg2, r_bg], writes=[r_g2])
                        op("dve", lambda e, pa=pa: e.tensor_tensor(out=t1[:], in0=pa[:], in1=g1[:], op=ALU.mult), reads=[rpa, r_g1], writes=[r_t1])
                        op("dve", lambda e, pb=pb: e.tensor_tensor(out=t2[:], in0=pb[:], in1=g2[:], op=ALU.mult), reads=[rpb, r_g2], writes=[r_t2])
                        op("pool", lambda e, j=j: e.tensor_tensor(out=mT[:, j, :], in0=t1[:], in1=t2[:], op=ALU.add), reads=[r_t1, r_t2], writes=[r_mT])
                    chk(8)
                    pending_wout.append(T0)
            except StopBuild:
                pass
            while pending_wout:
                wout_section(pending_wout.pop(0))
            while conv_state["next"] < len(conv_list) or conv_state["pending"] is not None:
                conv_step()
                conv_flush()
            S_.barrier()
            with nc.Block() as blk:
                S_.emit(blk)
            if int(os.environ.get('KSTOP', '99')) < 20:
                return nc

        with ExitStack() as st:
            sb = lambda name, shape, ty: st.enter_context(nc.sbuf_tensor(name, shape, ty))
            CH = 1024
            NMC = NT // CH
            ident = sb("ident2", [128, 128], BF16); r_ident = Res("ident2")
            identf = sb("identf2", [128, 128], F32); r_identf = Res("identf2")
            op("pool", lambda e: e.memset(identf[:], 0.0), writes=[r_identf])
            op("pool", lambda e: e.affine_select(out=identf[:], in_=identf[:], pattern=[[-1, 128]],
                                                 compare_op=ALU.not_equal, fill=1.0, base=0,
                                                 channel_multiplier=1), reads=[r_identf], writes=[r_identf])
            op("dve", lambda e: e.tensor_copy(out=ident[:], in_=identf[:]), reads=[r_identf], writes=[r_ident])
            ln_t = sb("ln2_t", [128, 2, D], F32); r_ln = Res("ln2")
            for i in range(2):
                dma("sp", lambda e, i=i: e.dma_start(out=ln_t[:, i, :], in_=ln_d[2 + i:3 + i, :].partition_broadcast(128)), writes=[r_ln])
            wr = sb("wr_t", [128, 8, 36], BF16); r_wr = Res("wr")
            br = sb("br_t", [128, 36], F32); r_br = Res("br")
            dma("pool", lambda e: e.dma_start(out=wr[:], in_=wr_d.rearrange("(k p) n -> p k n", p=128)), writes=[r_wr])
            dma("sp", lambda e: e.dma_start(out=br[:], in_=br_d.partition_broadcast(128)), writes=[r_br])
            I32 = mybir.dt.int32
            BLK = 256
            NTILE = NT // 128
            NB = (2 * NT) // BLK + NEXP
            CAP = NB * BLK
            wgb2 = wgb_d.rearrange("e p n -> (e p) n")
            wub2 = wub_d.rearrange("e p n -> (e p) n")
            wdb2 = wdb_d.rearrange("e p n -> (e p) n")
            r_xbuf = Res("xbuf"); r_ybuf = Res("ybuf")
            U = sb("U", [128, 128], BF16); r_U = Res("U")
            Uf = identf
            op("pool", lambda e: e.memset(Uf[:], 1.0), reads=[r_identf], writes=[r_identf])
            op("pool", lambda e: e.affine_select(out=Uf[:], in_=Uf[:], pattern=[[1, 128]], compare_op=ALU.is_gt, fill=0.0,
                                                 base=0, channel_multiplier=-1), reads=[r_identf], writes=[r_identf])
            op("dve", lambda e: e.tensor_copy(out=U[:], in_=Uf[:]), reads=[r_identf], writes=[r_U])
            ones = sb("ones", [128, 128], BF16); r_ones = Res("ones")
            op("pool", lambda e: e.memset(ones[:], 1.0), writes=[r_ones])
            pci = sb("pci", [128, 1], I32); r_pci = Res("pci")
            pcf = sb("pcf", [128, 2], F32); r_pcf = Res("pcf")
            op("pool", lambda e: e.iota(pci[:], pattern=[[0, 1]], base=0, channel_multiplier=1), writes=[r_pci])
            op("dve", lambda e: e.tensor_copy(out=pcf[:, 0:1], in_=pci[:]), reads=[r_pci], writes=[r_pcf])
            op("dve", lambda e: e.tensor_scalar(out=pcf[:, 1:2], in0=pcf[:, 0:1], scalar1=float(BLK), scalar2=None, op0=ALU.mult),
               reads=[r_pcf], writes=[r_pcf])
            bvi = sb("bvi", [128, NB], I32); r_bvi = Res("bvi")
            bvf = sb("bvf", [128, NB], F32); r_bvf = Res("bvf")
            op("pool", lambda e: e.iota(bvi[:], pattern=[[1, NB]], base=0, channel_multiplier=0), writes=[r_bvi])
            op("dve", lambda e: e.tensor_copy(out=bvf[:], in_=bvi[:]), reads=[r_bvi], writes=[r_bvf])
            oh1 = sb("oh1", [128, NTILE, 32], BF16); r_oh1 = Res("oh1")
            oh2 = sb("oh2", [128, NTILE, 32], BF16); r_oh2 = Res("oh2")
            posAB = sb("posAB", [128, NTILE, 2], F32); r_pos = Res("posAB")
            wAB = sb("wAB", [128, NTILE, 2], F32); r_wAB = Res("wAB")
            idxAB = sb("idxAB", [128, NTILE, 2], I32); r_idx = Res("idxAB")
            base = sb("base", [128, 32], F32); r_base = Res("base")
            op("pool", lambda e: e.memset(base[:], 0.0), writes=[r_base])
            hT = [sb(f"hT{i}", [128, 8, CH], BF16) for i in range(2)]
            r_hT = [Res(f"hT{i}") for i in range(2)]
            xbr = [sb(f"xbr{i}", [128, D], BF16) for i in range(2)]
            r_xbr = [Res(f"xbr{i}") for i in range(2)]
            lg = sb("lg", [128, 36], F32); r_lg = Res("lg")
            sm = sb("sm", [128, 16], F32); r_sm = Res("sm")
            oh = sb("oh", [128, 4], F32); r_oh = Res("oh")
            em = sb("em", [128, 4, 8], F32); r_em = Res("em")
            m8 = sb("m8", [128, 8], F32); r_m8 = Res("m8")
            s1 = sb("s1", [128, 32], F32); r_s1 = Res("s1")
            s2 = sb("s2", [128, 32], F32); r_s2 = Res("s2")
            sB = sb("sB", [128, 32], F32); r_sB = Res("sB")
            Mb = sb("Mb", [128, 32], BF16); r_Mb = Res("Mb")
            pos = sb("pos", [128, 32], F32); r_posf = Res("posf")
            tmp = sb("tmp", [128, 32], F32); r_tmp = Res("tmp")
            gx = sb("gx", [128, 4], F32); r_gx = Res("gx")
            emf = em[:].rearrange("p g x -> p (g x)")
            for mc in range(NMC):
                T0 = mc * CH
                h_, r_h = hT[mc % 2], r_hT[mc % 2]
                for tt in range(CH // 128):
                    ti = mc * (CH // 128) + tt
                    xi_ = ti % 2
                    dma("pool", lambda e, ti=ti, xi_=xi_: e.dma_start(out=xbr[xi_][:], in_=x1_d[ti * 128:(ti + 1) * 128, :]), writes=[r_xbr[xi_]])
                    pt, rpt = nxt("T")
                    for j in range(8):
                        op("pe", lambda e, pt=pt, j=j, xi_=xi_: e.transpose(pt[:, j * 128:(j + 1) * 128], xbr[xi_][:, j * 128:(j + 1) * 128], ident[:]),
                           reads=[r_xbr[xi_], r_ident], writes=[rpt])
                    op("act", lambda e, pt=pt, tt=tt, h_=h_: e.activation(out=h_[:, :, tt * 128:(tt + 1) * 128], in_=pt[:].rearrange("p (j t) -> p j t", j=8), func=AF.Copy),
                       reads=[rpt], writes=[r_h])
                    ps, rp = nxt("S")
                    for k in range(8):
                        op("pe", lambda e, ps=ps, k=k, tt=tt, h_=h_: e.matmul(ps[:, 0:36], lhsT=h_[:, k, tt * 128:(tt + 1) * 128], rhs=wr[:, k, :],
                                                                              start=(k == 0), stop=(k == 7)), reads=[r_h, r_wr], writes=[rp])
                    op("dve", lambda e, ps=ps: e.tensor_tensor(out=lg[:], in0=ps[:, 0:36], in1=br[:], op=ALU.add), reads=[rp, r_br], writes=[r_lg])
                    op("dve", lambda e: e.tensor_reduce(out=sm[:, 0:1], in_=lg[:, 0:4], axis=AX.X, op=ALU.max), reads=[r_lg], writes=[r_sm])
                    op("dve", lambda e: e.tensor_scalar(out=oh[:], in0=lg[:, 0:4], scalar1=sm[:, 0:1], scalar2=None, op0=ALU.is_ge),
                       reads=[r_lg, r_sm], writes=[r_oh])
                    op("dve", lambda e: e.tensor_scalar(out=sm[:, 1:2], in0=sm[:, 0:1], scalar1=-1.0, scalar2=None, op0=ALU.mult),
                       reads=[r_sm], writes=[r_sm])
                    op("act", lambda e: e.activation(out=gx[:], in_=lg[:, 0:4], func=AF.Exp, bias=sm[:, 1:2], scale=1.0, accum_out=sm[:, 2:3]),
                       reads=[r_lg, r_sm], writes=[r_gx, r_sm])
                    op("dve", lambda e: e.reciprocal(out=sm[:, 3:4], in_=sm[:, 2:3]), reads=[r_sm], writes=[r_sm])
                    op("dve", lambda e: e.tensor_scalar(out=oh[:], in0=oh[:], scalar1=-1.0, scalar2=1.0e30, op0=ALU.add, op1=ALU.mult),
                       reads=[r_oh], writes=[r_oh])
                    op("dve", lambda e: e.tensor_tensor(out=em[:], in0=lg[:, 4:36].rearrange("p (g x) -> p g x", g=4),
                                                        in1=oh[:].unsqueeze(2).to_broadcast([128, 4, 8]), op=ALU.add),
                       reads=[r_lg, r_oh], writes=[r_em])
                    op("dve", lambda e: e.max(out=m8[:], in_=emf), reads=[r_em], writes=[r_m8])
                    op("dve", lambda e: e.tensor_scalar(out=s1[:], in0=emf, scalar1=m8[:, 0:1], scalar2=None, op0=ALU.is_ge),
                       reads=[r_em, r_m8], writes=[r_s1])
                    op("dve", lambda e: e.tensor_scalar(out=s2[:], in0=emf, scalar1=m8[:, 1:2], scalar2=None, op0=ALU.is_ge),
                       reads=[r_em, r_m8], writes=[r_s2])
                    op("dve", lambda e: e.tensor_tensor(out=sB[:], in0=s2[:], in1=s1[:], op=ALU.subtract), reads=[r_s1, r_s2], writes=[r_sB])
                    op("dve", lambda e: e.tensor_copy(out=Mb[:], in_=s2[:]), reads=[r_s2], writes=[r_Mb])
                    op("dve", lambda e, ti=ti: e.tensor_copy(out=oh1[:, ti, :], in_=s1[:]), reads=[r_s1], writes=[r_oh1])
                    op("dve", lambda e, ti=ti: e.tensor_copy(out=oh2[:, ti, :], in_=sB[:]), reads=[r_sB], writes=[r_oh2])
                    op("dve", lambda e: e.tensor_tensor(out=sm[:, 4:5], in0=m8[:, 1:2], in1=m8[:, 0:1], op=ALU.subtract), reads=[r_m8, r_sm], writes=[r_sm])
                    op("act", lambda e: e.activation(out=sm[:, 5:6], in_=sm[:, 4:5], func=AF.Exp), reads=[r_sm], writes=[r_sm])
                    op("dve", lambda e: e.tensor_scalar(out=sm[:, 6:7], in0=sm[:, 5:6], scalar1=1.0, scalar2=None, op0=ALU.add), reads=[r_sm], writes=[r_sm])
                    op("dve", lambda e: e.reciprocal(out=sm[:, 6:7], in_=sm[:, 6:7]), reads=[r_sm], writes=[r_sm])
                    op("dve", lambda e: e.tensor_tensor(out=sm[:, 7:8], in0=sm[:, 5:6], in1=sm[:, 6:7], op=ALU.mult), reads=[r_sm], writes=[r_sm])
                    op("dve", lambda e, ti=ti: e.tensor_scalar(out=wAB[:, ti, :], in0=sm[:, 6:8], scalar1=sm[:, 3:4], scalar2=None, op0=ALU.mult),
                       reads=[r_sm], writes=[r_wAB])
                    pp, rpp = nxt("S")
                    op("pe", lambda e, pp=pp: e.matmul(pp[:, 0:32], lhsT=U[:], rhs=Mb[:], start=True, stop=True), reads=[r_U, r_Mb], writes=[rpp])
                    op("dve", lambda e, pp=pp: e.tensor_tensor(out=pos[:], in0=pp[:, 0:32], in1=base[:], op=ALU.add), reads=[rpp, r_base], writes=[r_posf])
                    pq, rpq = nxt("S")
                    op("pe", lambda e, pq=pq: e.matmul(pq[:, 0:32], lhsT=ones[:], rhs=Mb[:], start=True, stop=True), reads=[r_ones, r_Mb], writes=[rpq])
                    op("dve", lambda e, pq=pq: e.tensor_tensor(out=base[:], in0=pq[:, 0:32], in1=base[:], op=ALU.add), reads=[rpq, r_base], writes=[r_base])
                    op("dve", lambda e: e.tensor_tensor(out=tmp[:], in0=pos[:], in1=s1[:], op=ALU.mult), reads=[r_posf, r_s1], writes=[r_tmp])
                    op("dve", lambda e, ti=ti: e.tensor_reduce(out=posAB[:, ti, 0:1], in_=tmp[:], axis=AX.X, op=ALU.add), reads=[r_tmp], writes=[r_pos])
                    op("dve", lambda e: e.tensor_tensor(out=tmp[:], in0=pos[:], in1=sB[:], op=ALU.mult), reads=[r_posf, r_sB], writes=[r_tmp])
                    op("dve", lambda e, ti=ti: e.tensor_reduce(out=posAB[:, ti, 1:2], in_=tmp[:], axis=AX.X, op=ALU.add), reads=[r_tmp], writes=[r_pos])
            cmpb = sb("cmpb", [128, 32], BF16); r_cmpb = Res("cmpb")
            nblk = sb("nblk", [128, 32], F32); r_nblk = Res("nblk")
            endb = sb("endb", [128, 32], F32); r_endb = Res("endb")
            sbase = sb("sbase", [128, 32], F32); r_sbase = Res("sbase")
            op("dve", lambda e: e.tensor_scalar(out=cmpb[:], in0=base[:], scalar1=pcf[:, 1:2], scalar2=None, op0=ALU.is_gt),
               reads=[r_base, r_pcf], writes=[r_cmpb])
            pn, rpn = nxt("S")
            op("pe", lambda e: e.matmul(pn[:, 0:32], lhsT=ones[:], rhs=cmpb[:], start=True, stop=True), reads=[r_ones, r_cmpb], writes=[rpn])
            op("dve", lambda e: e.tensor_copy(out=nblk[:], in_=pn[:, 0:32]), reads=[rpn], writes=[r_nblk])
            op("dve", lambda e: e.tensor_copy(out=endb[:, 0:1], in_=nblk[:, 0:1]), reads=[r_nblk], writes=[r_endb])
            for ex in range(1, NEXP):
                op("dve", lambda e, ex=ex: e.tensor_tensor(out=endb[:, ex:ex + 1], in0=endb[:, ex - 1:ex], in1=nblk[:, ex:ex + 1], op=ALU.add),
                   reads=[r_endb, r_nblk], writes=[r_endb])
            op("dve", lambda e: e.tensor_tensor(out=sbase[:], in0=endb[:], in1=nblk[:], op=ALU.subtract), reads=[r_endb, r_nblk], writes=[r_sbase])
            op("dve", lambda e: e.tensor_scalar(out=sbase[:], in0=sbase[:], scalar1=float(BLK), scalar2=None, op0=ALU.mult), reads=[r_sbase], writes=[r_sbase])
            cmp3 = sb("cmp3", [128, NB, 32], F32); r_cmp3 = Res("cmp3")
            bex = sb("bex", [128, NB], F32); r_bex = Res("bex")
            idxw = sb("idxw", [128, NB], I32); r_idxw = Res("idxw")
            op("dve", lambda e: e.tensor_tensor(out=cmp3[:], in0=endb[:].unsqueeze(1).to_broadcast([128, NB, 32]),
                                                in1=bvf[:].unsqueeze(2).to_broadcast([128, NB, 32]), op=ALU.is_le),
               reads=[r_endb, r_bvf], writes=[r_cmp3])
            op("dve", lambda e: e.tensor_reduce(out=bex[:], in_=cmp3[:], axis=AX.X, op=ALU.add), reads=[r_cmp3], writes=[r_bex])
            op("dve", lambda e: e.tensor_scalar(out=bex[:], in0=bex[:], scalar1=float(NEXP - 1), scalar2=128.0, op0=ALU.min, op1=ALU.mult),
               reads=[r_bex], writes=[r_bex])
            op("dve", lambda e: e.tensor_scalar(out=bex[:], in0=bex[:], scalar1=pcf[:, 0:1], scalar2=None, op0=ALU.add), reads=[r_bex, r_pcf], writes=[r_bex])
            op("dve", lambda e: e.tensor_copy(out=idxw[:], in_=bex[:]), reads=[r_bex], writes=[r_idxw])
            tmp3 = sb("tmp3", [128, 32], F32); r_tmp3 = Res("tmp3")
            slf = sb("slf", [128, 2], F32); r_slf = Res("slf")
            xb = [sb(f"xb{i}", [128, D], BF16) for i in range(2)]
            r_xbs = [Res("xbs0"), Res("xbs1")]
            r_xb = [Res(f"xb{i}") for i in range(2)]
            big3 = cmp3[:, 0:NTILE, :]; r_big3 = r_cmp3
            slall = sb("slall", [128, NTILE, 2], F32); r_slall = Res("slall")
            for ab, ohx, r_ohx in ((0, oh1, r_oh1), (1, oh2, r_oh2)):
                op("dve", lambda e, ohx=ohx: e.tensor_tensor(out=big3, in0=ohx[:], in1=sbase[:].unsqueeze(1).to_broadcast([128, NTILE, 32]), op=ALU.mult),
                   reads=[r_ohx, r_sbase], writes=[r_big3])
                op("dve", lambda e, ab=ab: e.tensor_reduce(out=slall[:, :, ab], in_=big3, axis=AX.X, op=ALU.add), reads=[r_big3], writes=[r_slall])
            op("dve", lambda e: e.tensor_tensor(out=slall[:], in0=slall[:], in1=posAB[:], op=ALU.add), reads=[r_slall, r_pos], writes=[r_slall])
            op("dve", lambda e: e.tensor_copy(out=idxAB[:], in_=slall[:]), reads=[r_slall], writes=[r_idx])
            for ti in range(NTILE):
                xi = ti % 2
                if ti == 0:
                    dma("pool", lambda e: e.dma_start(out=xb[0][:], in_=x1_d[0:128, :]), reads=[r_x1], writes=[r_xb[0]])
                if ti + 1 < NTILE:
                    dma("pool", lambda e, ti=ti: e.dma_start(out=xb[(ti + 1) % 2][:], in_=x1_d[(ti + 1) * 128:(ti + 2) * 128, :]),
                        reads=[r_x1], writes=[r_xb[(ti + 1) % 2]])
                for ab in range(2):
                    dma("pool", lambda e, ti=ti, xi=xi, ab=ab: e.indirect_dma_start(
                        out=xbuf_d, out_offset=bass.IndirectOffsetOnAxis(ap=idxAB[:, ti, ab:ab + 1], axis=0), in_=xb[xi][:], in_offset=None),
                        reads=[r_xb[xi], r_idx], writes=[], sem_res=r_xbs[xi])
            fence = sb("fence", [128, D], BF16); r_fence = Res("fence")
            dma("pool", lambda e: e.dma_start(out=fence[:], in_=xbuf_d[CAP - 128:CAP, :]), writes=[r_fence])
            dma("pool", lambda e: e.dma_start(out=fence[:], in_=xbuf_d[0:128, :]), writes=[r_fence])
            S_.barrier()
            wgt = [sb(f"wgt{i}", [128, 8, DE], BF16) for i in range(2)]
            wut = [sb(f"wut{i}", [128, 8, DE], BF16) for i in range(2)]
            wdt = [sb(f"wdt{i}", [128, 4, D], BF16) for i in range(2)]
            r_weg = [Res(f"weg{i}") for i in range(2)]
            r_weu = [Res(f"weu{i}") for i in range(2)]
            r_wed = [Res(f"wed{i}") for i in range(2)]
            xs = [sb(f"xs{i}", [128, 2, D], BF16) for i in range(2)]
            r_xs = [Res(f"xs{i}") for i in range(2)]
            xT = [sb(f"xT{i}", [128, 8, BLK], BF16) for i in range(2)]
            r_xT = [Res(f"xT{i}") for i in range(2)]
            actT = sb("actT", [128, 4, BLK], BF16); r_actT = [Res(f"actT{i}") for i in range(4)]
            sg = [sb(f"sg{i}", [128, BLK], F32) for i in range(2)]
            r_sg = [Res(f"sg{i}") for i in range(2)]
            yb = [sb(f"yb{i}", [128, D], F32) for i in range(2)]
            r_yb = [Res(f"yb{i}") for i in range(2)]
            yctr = [0]
            def load_block(b):
                bi = b % 2
                for (wt_, w2, rw) in ((wgt, wgb2, r_weg), (wut, wub2, r_weu), (wdt, wdb2, r_wed)):
                    dma("pool", lambda e, b=b, bi=bi, wt_=wt_, w2=w2: e.indirect_dma_start(
                        out=wt_[bi][:].rearrange("p k n -> p (k n)"), out_offset=None, in_=w2,
                        in_offset=bass.IndirectOffsetOnAxis(ap=idxw[:, b:b + 1], axis=0)), reads=[r_idxw], writes=[rw[bi]])
                dma("sp", lambda e, b=b, bi=bi: e.dma_start(out=xs[bi][:], in_=xbuf_d[b * BLK:(b + 1) * BLK, :].rearrange("(s p) d -> p s d", p=128)),
                    writes=[r_xs[bi]])

            load_block(0)
            for b in range(NB):
                bi = b % 2
                if b + 1 < NB:
                    load_block(b + 1)
                for st_ in range(2):
                    pt, rpt = nxt("T")
                    for j in range(8):
                        op("pe", lambda e, pt=pt, j=j, st_=st_, bi=bi: e.transpose(pt[:, j * 128:(j + 1) * 128], xs[bi][:, st_, j * 128:(j + 1) * 128], ident[:]),
                           reads=[r_xs[bi], r_ident], writes=[rpt])
                    if st_ == 0:
                        op("act", lambda e, pt=pt, bi=bi, st_=st_: e.activation(out=xT[bi][:, :, st_ * 128:(st_ + 1) * 128],
                                                                               in_=pt[:].rearrange("p (j t) -> p j t", j=8), func=AF.Copy),
                           reads=[rpt], writes=[r_xT[bi]])
                    else:
                        op("dve", lambda e, pt=pt, bi=bi, st_=st_: e.tensor_copy(out=xT[bi][:, :, st_ * 128:(st_ + 1) * 128],
                                                                                in_=pt[:].rearrange("p (j t) -> p j t", j=8)),
                           reads=[rpt], writes=[r_xT[bi]])
                for fc in range(4):
                    pg, rpg = nxt("S")
                    for k in range(8):
                        op("pe", lambda e, pg=pg, k=k, fc=fc, bi=bi: e.matmul(pg[:, 0:BLK], lhsT=wgt[bi][:, k, fc * 128:(fc + 1) * 128], rhs=xT[bi][:, k, :],
                                                                              start=(k == 0), stop=(k == 7)), reads=[r_weg[bi], r_xT[bi]], writes=[rpg])
                    pu, rpu = nxt("S")
                    for k in range(8):
                        op("pe", lambda e, pu=pu, k=k, fc=fc, bi=bi: e.matmul(pu[:, 0:BLK], lhsT=wut[bi][:, k, fc * 128:(fc + 1) * 128], rhs=xT[bi][:, k, :],
                                                                              start=(k == 0), stop=(k == 7)), reads=[r_weu[bi], r_xT[bi]], writes=[rpu])
                    si = fc % 2
                    op("act", lambda e, pg=pg, si=si: e.activation(out=sg[si][:], in_=pg[:, 0:BLK], func=AF.Silu), reads=[rpg], writes=[r_sg[si]])
                    op("dve", lambda e, pu=pu, si=si, fc=fc: e.tensor_tensor(out=actT[:, fc, :], in0=pu[:, 0:BLK], in1=sg[si][:], op=ALU.mult),
                       reads=[rpu, r_sg[si]], writes=[r_actT[fc]])
                for st_ in range(2):
                    yi = yctr[0] % 2
                    yctr[0] += 1
                    for hh in range(2):
                        ps, rp = nxt("O")
                        for fc in range(4):
                            op("pe", lambda e, ps=ps, fc=fc, st_=st_, hh=hh, bi=bi: e.matmul(
                                ps[:], lhsT=actT[:, fc, st_ * 128:(st_ + 1) * 128], rhs=wdt[bi][:, fc, hh * 512:(hh + 1) * 512],
                                start=(fc == 0), stop=(fc == 3)), reads=[r_actT[fc], r_wed[bi]], writes=[rp])
                        if hh == 0:
                            op("act", lambda e, ps=ps, yi=yi, hh=hh: e.activation(out=yb[yi][:, hh * 512:(hh + 1) * 512], in_=ps[:], func=AF.Copy),
                               reads=[rp], writes=[r_yb[yi]])
                        else:
                            op("dve", lambda e, ps=ps, yi=yi, hh=hh: e.tensor_copy(out=yb[yi][:, hh * 512:(hh + 1) * 512], in_=ps[:]),
                               reads=[rp], writes=[r_yb[yi]])
                    dma("sp", lambda e, b=b, st_=st_, yi=yi: e.dma_start(out=ybuf_d[b * BLK + st_ * 128:b * BLK + (st_ + 1) * 128, :], in_=yb[yi][:]),
                        reads=[r_yb[yi]], writes=[], sem_res=r_ybuf)
            S_.barrier()
            yA = [sb(f"yA{i}", [128, D], F32) for i in range(4)]
            yB = [sb(f"yB{i}", [128, D], F32) for i in range(4)]
            r_yA = [Res(f"yA{i}") for i in range(4)]
            r_yB = [Res(f"yB{i}") for i in range(4)]
            xr = [sb(f"xr{i}", [128, D], F32) for i in range(4)]
            r_xr = [Res(f"xr{i}") for i in range(4)]
            stats_l = [sb(f"stats2{i}", [128, 2, 6], F32) for i in range(4)]; r_stats_l = [Res(f"stats2{i}") for i in range(4)]
            mv_l = [sb(f"mv2{i}", [128, 2], F32) for i in range(4)]; r_mv_l = [Res(f"mv2{i}") for i in range(4)]
            rstd_l = [sb(f"rstd2{i}", [128, 1], F32) for i in range(4)]; r_rstd_l = [Res(f"rstd2{i}") for i in range(4)]
            def combine_fetch(ti):
                xi = ti % 4
                dma("pool", lambda e, ti=ti, xi=xi: e.indirect_dma_start(out=yA[xi][:], out_offset=None, in_=ybuf_d,
                                                                        in_offset=bass.IndirectOffsetOnAxis(ap=idxAB[:, ti, 0:1], axis=0)),
                    reads=[r_idx], writes=[r_yA[xi]])
                dma("pool", lambda e, ti=ti, xi=xi: e.indirect_dma_start(out=yB[xi][:], out_offset=None, in_=ybuf_d,
                                                                        in_offset=bass.IndirectOffsetOnAxis(ap=idxAB[:, ti, 1:2], axis=0)),
                    reads=[r_idx], writes=[r_yB[xi]])
                dma("sp", lambda e, xi=xi, ti=ti: e.dma_start(out=xr[xi][:], in_=x1_d[ti * 128:(ti + 1) * 128, :]), reads=[r_x1], writes=[r_xr[xi]])

            def cvars(ti):
                xi = ti % 4
                return xi, stats_l[xi], r_stats_l[xi], mv_l[xi], r_mv_l[xi], rstd_l[xi], r_rstd_l[xi]

            def combine_A(ti):
                xi, stats, r_stats, mv, r_mv, rstd, r_rstd = cvars(ti)
                op("act", lambda e, xi=xi: e.activation(out=xr[xi][:], in_=xr[xi][:], func=AF.Copy, scale=ALPHA), reads=[r_xr[xi]], writes=[r_xr[xi]])
                op("act", lambda e, xi=xi, ti=ti: e.activation(out=yA[xi][:], in_=yA[xi][:], func=AF.Copy, scale=wAB[:, ti, 0:1]),
                   reads=[r_yA[xi], r_wAB], writes=[r_yA[xi]])
                op("act", lambda e, xi=xi, ti=ti: e.activation(out=yB[xi][:], in_=yB[xi][:], func=AF.Copy, scale=wAB[:, ti, 1:2]),
                   reads=[r_yB[xi], r_wAB], writes=[r_yB[xi]])
                op("pool", lambda e, xi=xi: e.tensor_tensor(out=yA[xi][:], in0=yA[xi][:], in1=yB[xi][:], op=ALU.add),
                   reads=[r_yA[xi], r_yB[xi]], writes=[r_yA[xi]])
                op("dve", lambda e, xi=xi: e.tensor_tensor(out=xr[xi][:], in0=xr[xi][:], in1=yA[xi][:], op=ALU.add),
                   reads=[r_xr[xi], r_yA[xi]], writes=[r_xr[xi]])
                for hh in range(2):
                    op("dve", lambda e, hh=hh, xi=xi, stats=stats: e.bn_stats(out=stats[:, hh, :], in_=xr[xi][:, hh * 512:(hh + 1) * 512]),
                       reads=[r_xr[xi]], writes=[r_stats])
                op("dve", lambda e, stats=stats, mv=mv: e.bn_aggr(out=mv[:], in_=stats[:].rearrange("p a b -> p (a b)")), reads=[r_stats], writes=[r_mv])
                op("act", lambda e, mv=mv, rstd=rstd: e.activation(out=rstd[:], in_=mv[:, 1:2], func=AF.Sqrt, bias=EPS, scale=1.0), reads=[r_mv], writes=[r_rstd])

            def combine_B(ti):
                xi, stats, r_stats, mv, r_mv, rstd, r_rstd = cvars(ti)
                op("dve", lambda e, rstd=rstd: e.reciprocal(out=rstd[:], in_=rstd[:]), reads=[r_rstd], writes=[r_rstd])
                op("dve", lambda e, mv=mv, rstd=rstd: e.tensor_scalar(out=mv[:, 1:2], in0=mv[:, 0:1], scalar1=rstd[:, 0:1], scalar2=-1.0,
                                                                     op0=ALU.mult, op1=ALU.mult), reads=[r_mv, r_rstd], writes=[r_mv])
                op("act", lambda e, xi=xi, mv=mv, rstd=rstd: e.activation(out=xr[xi][:], in_=xr[xi][:], func=AF.Identity, bias=mv[:, 1:2], scale=rstd[:, 0:1]),
                   reads=[r_xr[xi], r_mv, r_rstd], writes=[r_xr[xi]])

            def combine_C(ti):
                xi, stats, r_stats, mv, r_mv, rstd, r_rstd = cvars(ti)
                op("dve", lambda e, xi=xi: e.tensor_tensor(out=xr[xi][:], in0=xr[xi][:], in1=ln_t[:, 0, :], op=ALU.mult),
                   reads=[r_xr[xi], r_ln], writes=[r_xr[xi]])
                op("dve", lambda e, xi=xi: e.tensor_tensor(out=xr[xi][:], in0=xr[xi][:], in1=ln_t[:, 1, :], op=ALU.add),
                   reads=[r_xr[xi], r_ln], writes=[r_xr[xi]])
                dma("sp", lambda e, xi=xi, ti=ti: e.dma_start(out=out_d[ti * 128:(ti + 1) * 128, :], in_=xr[xi][:]),
                    reads=[r_xr[xi]], writes=[r_out])

            for ti in range(min(2, NTILE)):
                combine_fetch(ti)
            for step in range(NTILE + 2):
                if 0 <= step - 2 < NTILE:
                    combine_C(step - 2)
                if 0 <= step - 1 < NTILE:
                    combine_B(step - 1)
                if step < NTILE:
                    combine_A(step)
                if step + 2 < NTILE:
                    combine_fetch(step + 2)
            S_.barrier()
            with nc.Block() as blk:
                S_.emit(blk)
    return nc


def host_inputs(x2, w_in, b_in, attn_sinks, rel_bias_table, w_branch_swa, w_branch_moba, w_out, ln1_gain, ln1_bias,
                w_group_router, b_group_router, w_expert_router, b_expert_router, w_expert_gate, w_expert_up,
                w_expert_down, ln2_gain, ln2_bias):
    f = lambda a: np.ascontiguousarray(np.asarray(a, dtype=np.float32))
    w = np.asarray(w_in)[0]
    b = np.asarray(b_in)[0]
    perm = np.arange(4352)
    qperm = []
    for c in range(4):
        qperm += list(range(c * 64, c * 64 + 64)) + list(range((4 + c) * 64, (4 + c) * 64 + 64))
    perm[0:512] = np.array(qperm)
    wp = w[:, perm]
    bp = b[perm]
    qk_cols = list(range(0, 640)) + list(range(768, 1792))
    bqk = bp[qk_cols].reshape(13, 128).T
    bgt = bp[2304:4352].reshape(16, 128).T
    bvv = np.concatenate([bp[640:768], bp[1792:2304]])[None, :]
    rel = np.asarray(rel_bias_table)
    k = np.arange(128)[:, None]
    q = np.arange(128)[None, :]
    d_own = rel_bucket_np(q - k)
    d_prev = rel_bucket_np(q + 128 - k)
    ga = np.stack([rel[d_own][:, :, :8].transpose(0, 2, 1), rel[d_prev][:, :, :8].transpose(0, 2, 1)], axis=1)
    j = np.arange(1024)[None, :]
    gbk = rel_bucket_np(j - k - 384)
    gb = rel[gbk][:, :, 8:].transpose(0, 2, 1)
    common = {
        "w_in": f(wp), "bqk": f(bqk), "bg": f(bgt), "bv": f(bvv),
        "sinks": f(np.asarray(attn_sinks)[0][None, :]), "t31": f(rel[31:32, 8:16]),
        "ga_raw": f(ga.reshape(128, -1)), "gb_raw": f(gb.reshape(128, -1)),
        "wa": f(np.asarray(w_branch_swa)[0]), "wb": f(np.asarray(w_branch_moba)[0]), "wo": f(np.asarray(w_out)[0]),
        "ln": f(np.stack([np.asarray(ln1_gain)[0], np.asarray(ln1_bias)[0], np.asarray(ln2_gain)[0], np.asarray(ln2_bias)[0]])),
        "wr": f(np.concatenate([np.asarray(w_group_router)[0], np.asarray(w_expert_router)[0]], axis=1)),
        "br": f(np.concatenate([np.asarray(b_group_router)[0], np.asarray(b_expert_router)[0]])[None, :]),
        "wg": f(np.asarray(w_expert_gate)[0]), "wu": f(np.asarray(w_expert_up)[0]), "wd": f(np.asarray(w_expert_down)[0]),
    }
    return common


def run(x, ncores, nseq, S, **params):
    x = np.asarray(x, dtype=np.float32)
    common = host_inputs(x, **params)
    nc = build(nseq, S)
    in_maps = []
    for i in range(ncores):
        xs = x[i * nseq:(i + 1) * nseq].reshape(nseq * S, D)
        m = dict(common)
        m["x_tok"] = np.ascontiguousarray(xs)
        m["x_T"] = np.ascontiguousarray(xs.T)
        in_maps.append(m)
    res = run_bass_kernel_spmd(nc, in_maps, core_ids=list(range(ncores)))
    outs = [np.asarray(r["out"]).reshape(nseq, S, D) for r in res.results]
    return np.concatenate(outs, axis=0).astype(np.float32)


def kernel(x, **params):
    B, S, _ = np.asarray(x).shape
    return run(x, NCORES, B // NCORES, S, **params)
```

```python
from contextlib import ExitStack
import os


class StopBuild(Exception):
    pass


def chk(n):
    if int(os.environ.get('KSTOP', '99')) == n:
        raise StopBuild()

import numpy as np
import concourse.bass as bass
import concourse.mybir as mybir
from concourse.bass_utils import run_bass_kernel_spmd

F32 = mybir.dt.float32
BF16 = mybir.dt.bfloat16
ALU = mybir.AluOpType
AF = mybir.ActivationFunctionType
AX = mybir.AxisListType

D = 1024
NCORES = 8
BIG = 30000.0
ALPHA = 2.0 ** 0.25
EPS = 1e-5
NEXP = 32
DE = 512


class Res:
    __slots__ = ("name", "w", "r", "dsem", "dcount", "excl")

    def __init__(self, name, excl=False):
        self.name = name
        self.w = None
        self.r = {}
        self.dsem = None
        self.dcount = 0
        self.excl = excl


class Sched:
    ENGS = ("pe", "act", "dve", "pool", "sp")

    def __init__(self, nc, stack):
        self.nc = nc
        self.ops = {e: [] for e in self.ENGS}
        self.cnt = {e: 0 for e in self.ENGS}
        self.seen = {e: {} for e in self.ENGS}
        self.sems = {}
        self.dma_keys = {}
        self._stack = stack

    def _sem(self, key):
        s = self.sems.get(key)
        if s is None:
            s = self._stack.enter_context(self.nc.semaphore("s_" + str(key)))
            self.sems[key] = s
        return s

    def _collect(self, eng, reads, writes):
        need = {}

        def add(tok):
            if tok is None:
                return
            k, v = tok
            if k == eng and eng == "pe":
                return
            if need.get(k, 0) < v:
                need[k] = v
        for r in reads:
            add(r.w)
            if r.excl:
                for k, v in r.r.items():
                    add((k, v))
        for w in writes:
            add(w.w)
            for k, v in w.r.items():
                add((k, v))
        waits = []
        seen = self.seen[eng]
        for k, v in need.items():
            if seen.get(k, 0) < v:
                seen[k] = v
                waits.append((k, v))
        return waits

    def _update(self, tok, reads, writes):
        for r in reads:
            if r.excl:
                r.w = tok
                r.r = {}
            elif r.r.get(tok[0], 0) < tok[1]:
                r.r[tok[0]] = tok[1]
        for w in writes:
            w.w = tok
            w.r = {}

    def op(self, eng, fn, reads=(), writes=()):
        waits = self._collect(eng, reads, writes)
        self.cnt[eng] += 1
        tok = (eng, self.cnt[eng])
        self._sem(eng)
        for k, _ in waits:
            self._sem(k)
        self.ops[eng].append((waits, fn, (eng, 1)))
        self._update(tok, reads, writes)
        return tok

    def raw(self, eng, fn, reads=()):
        waits = self._collect(eng, reads, ())
        for k, _ in waits:
            self._sem(k)
        self.ops[eng].append((waits, fn, "raw"))

    def dma(self, q, fn, reads=(), writes=(), sem_res=None):
        waits = self._collect(q, reads, writes)
        sr = sem_res if sem_res is not None else writes[0]
        if sr.dsem is None:
            sr.dsem = "d_" + sr.name
        sr.dcount += 16
        self.dma_keys[sr.dsem] = sr.dcount
        tok = (sr.dsem, sr.dcount)
        self._sem(sr.dsem)
        for k, _ in waits:
            self._sem(k)
        self.ops[q].append((waits, fn, (sr.dsem, 16)))
        self._update(tok, reads, writes)
        return tok

    def barrier(self):
        for e in self.ENGS:
            waits = []
            seen = self.seen[e]
            for k in self.ENGS:
                v = self.cnt[k]
                if k != e and v > 0 and seen.get(k, 0) < v:
                    seen[k] = v
                    waits.append((k, v))
            for k, v in self.dma_keys.items():
                if seen.get(k, 0) < v:
                    seen[k] = v
                    waits.append((k, v))
            self.ops[e].append((waits, None, None))

    def emit(self, block):
        emap = {"pe": block.tensor, "act": block.scalar, "dve": block.vector,
                "pool": block.gpsimd, "sp": block.sync}
        for ename in self.ENGS:
            ops = self.ops[ename]
            if not ops:
                continue

            def body(e, ops=ops):
                for waits, fn, inc in ops:
                    for k, v in waits:
                        e.wait_ge(self.sems[k], v)
                    if fn is None:
                        continue
                    if inc == "raw":
                        fn(e)
                    else:
                        fn(e).then_inc(self.sems[inc[0]], inc[1])
            emap[ename](body)
            self.ops[ename] = []


def rel_bucket_np(dist):
    n = np.maximum(dist, 0)
    nf = np.maximum(n, 1).astype(np.float32)
    large = 16 + (np.log(nf / np.float32(16)) / np.float32(np.log(8.0)) * 16).astype(np.int32)
    large = np.minimum(large, 31)
    return np.where(n < 16, n, large)


def build(NSEQ, S):
    NT = NSEQ * S
    NCH = S // 512
    NTT = S // 128
    nc = bass.Bass("TRN2", target_bir_lowering=False)
    dt = lambda name, shape, ty, kind="ExternalInput": nc.dram_tensor(name, shape, ty, kind=kind).ap()
    x_tok = dt("x_tok", [NT, D], F32)
    x_T = dt("x_T", [D, NT], F32)
    w_in = dt("w_in", [D, 4352], F32)
    bqk = dt("bqk", [128, 13], F32)
    bg = dt("bg", [128, 16], F32)
    bv = dt("bv", [1, 640], F32)
    sinks = dt("sinks", [1, 8], F32)
    t31 = dt("t31", [1, 8], F32)
    ga_raw = dt("ga_raw", [128, 2 * 8 * 128], F32)
    gb_raw = dt("gb_raw", [128, 8 * 1024], F32)
    wa_d = dt("wa", [512, D], F32)
    wb_d = dt("wb", [512, D], F32)
    wo_d = dt("wo", [D, D], F32)
    ln_d = dt("ln", [4, D], F32)
    wr_d = dt("wr", [D, 36], F32)
    br_d = dt("br", [1, 36], F32)
    wg_d = dt("wg", [NEXP, D, DE], F32)
    wu_d = dt("wu", [NEXP, D, DE], F32)
    wd_d = dt("wd", [NEXP, DE, D], F32)
    out_d = dt("out", [NT, D], F32, kind="ExternalOutput")
    x1_d = dt("x1_scr", [NT, D], F32, kind="Internal")
    winb_d = dt("winb_scr", [128, 8 * (4352 + D)], BF16, kind="Internal")
    x1T_d = dt("x1T_scr", [D, NT], BF16, kind="Internal")
    wgb_d = dt("wgb_scr", [NEXP, 128, 8 * DE], BF16, kind="Internal")
    wub_d = dt("wub_scr", [NEXP, 128, 8 * DE], BF16, kind="Internal")
    wdb_d = dt("wdb_scr", [NEXP, 128, 4 * D], BF16, kind="Internal")
    xbuf_d = dt("xbuf_scr", [(2 * NT) // 256 * 256 + NEXP * 256, D], BF16, kind="Internal")
    ybuf_d = dt("ybuf_scr", [(2 * NT) // 256 * 256 + NEXP * 256, D], F32, kind="Internal")

    with ExitStack() as top:
        S_ = Sched(nc, top)
        op, dma = S_.op, S_.dma
        psS = [top.enter_context(nc.psum_tensor(f"psS{i}", [128, 512], F32)) for i in range(4)]
        rS = [Res(f"psS{i}", excl=True) for i in range(4)]
        psO = [top.enter_context(nc.psum_tensor(f"psO{i}", [128, 512], F32)) for i in range(2)]
        rO = [Res(f"psO{i}", excl=True) for i in range(2)]
        psT = [top.enter_context(nc.psum_tensor(f"psT{i}", [128, 1024], BF16)) for i in range(2)]
        rT = [Res(f"psT{i}", excl=True) for i in range(2)]
        ctr = {"S": 0, "O": 0, "T": 0}

        def nxt(kind):
            lst, rl = {"S": (psS, rS), "O": (psO, rO), "T": (psT, rT)}[kind]
            i = ctr[kind] % len(lst)
            ctr[kind] += 1
            return lst[i], rl[i]

        r_x1 = Res("x1_scr")
        r_x1s = [Res("x1s0"), Res("x1s1")]
        r_x1T = Res("x1T_scr")
        r_cw = [Res("cw0"), Res("cw1")]
        r_out = Res("out")

        with ExitStack() as st:
            sb = lambda name, shape, ty: st.enter_context(nc.sbuf_tensor(name, shape, ty))
            ident = sb("ident", [128, 128], BF16); r_ident = Res("ident")
            identf = sb("identf", [128, 128], F32); r_identf = Res("identf")
            op("pool", lambda e: e.memset(identf[:], 0.0), writes=[r_identf])
            op("pool", lambda e: e.affine_select(out=identf[:], in_=identf[:], pattern=[[-1, 128]],
                                                 compare_op=ALU.not_equal, fill=1.0, base=0,
                                                 channel_multiplier=1), reads=[r_identf], writes=[r_identf])
            op("dve", lambda e: e.tensor_copy(out=ident[:], in_=identf[:]), reads=[r_identf], writes=[r_ident])
            bqk_t = sb("bqk_t", [128, 13], F32); r_bqk = Res("bqk")
            bg_t = sb("bg_t", [128, 16], F32); r_bg = Res("bg")
            bv_t = sb("bv_t", [128, 640], F32); r_bv = Res("bv")
            es_t = sb("es_t", [128, 8], F32); r_es = Res("es")
            t31_t = sb("t31_t", [128, 8], F32); r_t31 = Res("t31")
            ln_t = sb("ln_t", [128, 2, D], F32); r_ln = Res("ln")
            dma("sp", lambda e: e.dma_start(out=bqk_t[:], in_=bqk), writes=[r_bqk])
            dma("sp", lambda e: e.dma_start(out=bg_t[:], in_=bg), writes=[r_bg])
            dma("sp", lambda e: e.dma_start(out=bv_t[:], in_=bv.partition_broadcast(128)), writes=[r_bv])
            dma("sp", lambda e: e.dma_start(out=es_t[:], in_=sinks.partition_broadcast(128)), writes=[r_es])
            dma("sp", lambda e: e.dma_start(out=t31_t[:], in_=t31.partition_broadcast(128)), writes=[r_t31])
            for i in range(2):
                dma("sp", lambda e, i=i: e.dma_start(out=ln_t[:, i, :], in_=ln_d[i:i + 1, :].partition_broadcast(128)), writes=[r_ln])
            op("act", lambda e: e.activation(out=es_t[:], in_=es_t[:], func=AF.Exp), reads=[r_es], writes=[r_es])
            op("dve", lambda e: e.tensor_scalar(out=bqk_t[:, 0:4], in0=bqk_t[:, 0:4], scalar1=0.125, scalar2=None, op0=ALU.mult), reads=[r_bqk], writes=[r_bqk])
            op("dve", lambda e: e.tensor_scalar(out=bqk_t[:, 5:9], in0=bqk_t[:, 5:9], scalar1=0.125, scalar2=None, op0=ALU.mult), reads=[r_bqk], writes=[r_bqk])
            PMr = sb("PMr", [128, 8, 32], F32); r_PMr = Res("PMr")
            OWr = sb("OWr", [128, 8, 32], F32); r_OWr = Res("OWr")
            op("pool", lambda e: e.memset(PMr[:, :, 0:16], 0.0), writes=[r_PMr])
            op("pool", lambda e: e.memset(PMr[:, :, 16:32], -3.0e38), reads=[r_PMr], writes=[r_PMr])
            op("pool", lambda e: e.memset(OWr[:], 0.0), writes=[r_OWr])
            op("pool", lambda e: e.memset(OWr[:, :, 16:17], 1.0), reads=[r_OWr], writes=[r_OWr])
            Gs = sb("Gs", [128, 2, 8, 128], BF16); r_Gs = Res("Gs")
            dma("pool", lambda e: e.dma_start(out=Gs[:].rearrange("p a h q -> p (a h q)"), in_=ga_raw), writes=[r_Gs])
            op("pool", lambda e: e.affine_select(out=Gs[:, 0, :, :], in_=Gs[:, 0, :, :], pattern=[[0, 8], [1, 128]],
                                                 compare_op=ALU.is_ge, fill=-BIG, base=0, channel_multiplier=-1),
               reads=[r_Gs], writes=[r_Gs])
            op("pool", lambda e: e.affine_select(out=Gs[:, 1, :, :], in_=Gs[:, 1, :, :], pattern=[[0, 8], [-1, 128]],
                                                 compare_op=ALU.is_gt, fill=-BIG, base=0, channel_multiplier=1),
               reads=[r_Gs], writes=[r_Gs])
            Gb = sb("Gb", [128, 8, 640], BF16); r_Gb = Res("Gb")
            xres = [sb(f"xres{i}", [128, D], F32) for i in range(2)]
            r_xres = [Res(f"xres{i}") for i in range(2)]
            gtmp = xres[0]; r_gtmp = r_xres[0]
            for h in range(8):
                dma("sp", lambda e, h=h: e.dma_start(out=gtmp[:, 0:640], in_=gb_raw[:, h * 1024:h * 1024 + 640]), writes=[r_gtmp])
                op("dve", lambda e, h=h: e.tensor_scalar(out=Gb[:, h, :], in0=gtmp[:, 0:640], scalar1=t31_t[:, h:h + 1], scalar2=None,
                                                         op0=ALU.subtract), reads=[r_gtmp, r_t31], writes=[r_Gb])
            op("pool", lambda e: e.affine_select(out=Gb[:], in_=Gb[:], pattern=[[0, 8], [1, 640]],
                                                 compare_op=ALU.is_ge, fill=-BIG, base=-384, channel_multiplier=-1),
               reads=[r_Gb], writes=[r_Gb])
            wa = sb("wa_t", [128, 4, D], BF16); r_wa = Res("wa")
            wb = sb("wb_t", [128, 4, D], BF16); r_wb = Res("wb")
            dma("pool", lambda e: e.dma_start(out=wa[:], in_=wa_d.rearrange("(k p) n -> p k n", p=128)), writes=[r_wa])
            dma("pool", lambda e: e.dma_start(out=wb[:], in_=wb_d.rearrange("(k p) n -> p k n", p=128)), writes=[r_wb])
            KmT = sb("KmT", [128, 4, S], BF16); r_Km = [Res(f"Km{c}") for c in range(NCH)]
            KsT = sb("KsT", [128, 1024], BF16); r_Ks = [Res(f"Ks{c}") for c in range(2)]
            Vm = sb("Vm", [128, NTT, 8, 65], BF16); r_Vm = [Res(f"Vm{c}") for c in range(NCH)]
            Vs = sb("Vs", [128, 8, 2, 65], BF16); r_Vs = [Res(f"Vs{c}") for c in range(2)]
            kmT = sb("kmT", [128, 4, 16], BF16); r_km = Res("kmT")
            kms = sb("kms", [128, 4, 2], F32); r_kms = Res("kms")
            op("pool", lambda e: e.memset(Vm[:, :, :, 64:65], 1.0), writes=r_Vm)
            op("pool", lambda e: e.memset(Vs[:, :, :, 64:65], 1.0), writes=r_Vs)
            op("pool", lambda e: e.memset(kmT[:], 0.0), writes=[r_km])
            zero_q = True
            wbuf = [sb(f"wbuf{i}", [128, 8 * 768], BF16) for i in range(2)]
            wview = lambda i, n: wbuf[i][:, 0:8 * n].rearrange("p (k n) -> p k n", k=8)
            r_wbuf = [Res(f"wbuf{i}") for i in range(2)]
            wctr = [0]
            cbuf = [sb(f"cbuf{i}", [128, 1024], BF16) for i in range(2)]
            r_cbuf = [Res(f"cbuf{i}") for i in range(2)]
            conv_list = []
            for ex in range(NEXP):
                for (src, dst, K_) in ((wg_d, wgb_d, 8), (wu_d, wub_d, 8), (wd_d, wdb_d, 4)):
                    for half in range(4):
                        conv_list.append((src, dst, K_, ex, half))
            conv_state = {"next": 0, "pending": None}
            n_slots = NSEQ * NCH * 8
            conv_per_slot = -(-len(conv_list) // n_slots)

            def conv_flush():
                p_ = conv_state["pending"]
                if p_ is not None:
                    i, dst, ex, half = p_
                    dma("sp", lambda e, i=i, dst=dst, ex=ex, half=half: e.dma_start(out=dst[ex][:, half * 1024:(half + 1) * 1024], in_=cbuf[i][:]),
                        reads=[r_cbuf[i]], writes=[], sem_res=r_cw[i])
                    conv_state["pending"] = None

            def conv_step():
                for _ in range(conv_per_slot):
                    conv_flush()
                    n_ = conv_state["next"]
                    if n_ >= len(conv_list):
                        return
                    src, dst, K_, ex, half = conv_list[n_]
                    conv_state["next"] = n_ + 1
                    i = n_ % 2
                    dma("pool", lambda e, i=i, src=src, ex=ex, K_=K_, half=half: e.dma_start(
                        out=cbuf[i][:].rearrange("p (k n) -> p k n", k=K_ // 4),
                        in_=src[ex].rearrange("(k p) n -> p k n", p=128)[:, half * (K_ // 4):(half + 1) * (K_ // 4), :]),
                        writes=[r_cbuf[i]])
                    conv_state["pending"] = (i, dst, ex, half)
            xTc = [sb(f"xTc{i}", [128, 8, 512], BF16) for i in range(2)]
            r_xTc = [Res(f"xTc{i}") for i in range(2)]
            QsT = sb("QsT", [128, 4, 512], BF16); r_Qs = Res("QsT")
            QmT = sb("QmTz", [128, 8, 512], BF16); r_Qm = Res("QmT")
            op("pool", lambda e: e.memset(QmT[:], 0.0), writes=[r_Qm])
            gm = sb("gm", [128, 8, 16], F32); r_gm = Res("gm")
            mx = sb("mx", [128, 8, 8], F32); r_mx = Res("mx")
            thr = sb("thr", [128, 8], F32); r_thr = Res("thr")
            sel = sb("sel", [128, 8, 16], F32); r_sel = Res("sel")
            madd4 = [sb(f"madd{i}", [128, 128], BF16) for i in range(4)]; r_madd4 = [Res(f"madd{i}") for i in range(4)]
            maddT = sb("maddT", [128, 512], BF16); r_maddT = Res("maddT")
            PT = [sb(f"PT{i}", [128, 512], BF16) for i in range(3)]
            r_PT = [Res(f"PT{i}") for i in range(3)]
            pctr = [0]
            rden = sb("rden", [128, 4], F32); r_rden = Res("rden")
            ytok = sb("ytok", [128, 4, D], BF16); r_ytok = Res("ytok")
            yT = sb("yT", [128, 8, 512], BF16); r_yT = Res("yT")
            mT = ytok[:].rearrange("p a (b c) -> p (a b) c", c=512); r_mT = r_ytok
            g1 = sb("g1", [128, 512], F32); r_g1 = Res("g1")
            g2 = sb("g2", [128, 512], F32); r_g2 = Res("g2")
            t1 = g1; r_t1 = r_g1
            t2 = g2; r_t2 = r_g2
            z = xres
            r_z = r_xres
            stats_l = [sb(f"stats{i}", [128, 2, 6], F32) for i in range(2)]; r_stats_l = [Res(f"stats{i}") for i in range(2)]
            mv_l = [sb(f"mv{i}", [128, 2], F32) for i in range(2)]; r_mv_l = [Res(f"mv{i}") for i in range(2)]
            rstd_l = [sb(f"rstd{i}", [128, 1], F32) for i in range(2)]; r_rstd_l = [Res(f"rstd{i}") for i in range(2)]

            def load_w(c0, ncols, dst0=0, new=True, conv=True):
                if new:
                    wctr[0] += 1
                i = wctr[0] % len(wbuf)
                dma("sp", lambda e: e.dma_start(out=wbuf[i][:, 0:8 * ncols], in_=winb_d[:, 8 * c0:8 * c0 + 8 * ncols]),
                    writes=[r_wbuf[i]])
                return wview(i, ncols), r_wbuf[i]

            def layer_norm(zt, r_zt, gi):
                stats, r_stats, mv, r_mv, rstd, r_rstd = stats_l[gi], r_stats_l[gi], mv_l[gi], r_mv_l[gi], rstd_l[gi], r_rstd_l[gi]
                for hh in range(2):
                    op("dve", lambda e, hh=hh: e.bn_stats(out=stats[:, hh, :], in_=zt[:, hh * 512:(hh + 1) * 512]),
                       reads=[r_zt], writes=[r_stats])
                op("dve", lambda e: e.bn_aggr(out=mv[:], in_=stats[:].rearrange("p a b -> p (a b)")), reads=[r_stats], writes=[r_mv])
                op("act", lambda e: e.activation(out=rstd[:], in_=mv[:, 1:2], func=AF.Sqrt, bias=EPS, scale=1.0),
                   reads=[r_mv], writes=[r_rstd])
                op("dve", lambda e: e.reciprocal(out=rstd[:], in_=rstd[:]), reads=[r_rstd], writes=[r_rstd])
                op("dve", lambda e: e.tensor_scalar(out=mv[:, 1:2], in0=mv[:, 0:1], scalar1=rstd[:, 0:1], scalar2=-1.0,
                                                    op0=ALU.mult, op1=ALU.mult), reads=[r_mv, r_rstd], writes=[r_mv])
                op("act", lambda e: e.activation(out=zt[:], in_=zt[:], func=AF.Identity, bias=mv[:, 1:2], scale=rstd[:, 0:1]),
                   reads=[r_zt, r_mv, r_rstd], writes=[r_zt])
                return gi

            r_winb = Res("winb")
            groups = [([(w_in, 0, 768, 0)], 768, 0), ([(w_in, 768, 512, 0)], 512, 768), ([(w_in, 1280, 512, 0)], 512, 1280),
                      ([(w_in, 1792, 512, 0)], 512, 1792)]
            for jp in range(4):
                groups.append(([(w_in, 2304 + jp * 256, 256, 0), (w_in, 3328 + jp * 256, 256, 256)], 512, 2304 + jp * 512))
            groups += [([(wo_d, 0, 512, 0)], 512, 4352), ([(wo_d, 512, 512, 0)], 512, 4864)]
            for gi_, (pieces, n_, d0_) in enumerate(groups):
                i_ = gi_ % 2
                for (src_, c0_, pn_, po_) in pieces:
                    dma("pool", lambda e, i_=i_, src_=src_, c0_=c0_, pn_=pn_, po_=po_, n_=n_: e.dma_start(
                        out=wview(i_, n_)[:, :, po_:po_ + pn_], in_=src_[:, c0_:c0_ + pn_].rearrange("(k p) n -> p k n", p=128)), writes=[r_wbuf[i_]])
                dma("sp", lambda e, i_=i_, n_=n_, d0_=d0_: e.dma_start(out=winb_d[:, 8 * d0_:8 * d0_ + 8 * n_], in_=wbuf[i_][:, 0:8 * n_]),
                    reads=[r_wbuf[i_]], writes=[], sem_res=r_winb)
            S_.barrier()
            pending_wout = []
            def wout_section(T0):
                woh = [load_w(4352, 512, conv=False), load_w(4864, 512, conv=False)]
                for tt in range(4):
                    zi = tt % 2
                    dma("sp", lambda e, zi=zi, tt=tt, T0=T0: e.dma_start(out=xres[zi][:], in_=x_tok[T0 + tt * 128:T0 + (tt + 1) * 128, :]),
                        writes=[r_xres[zi]])
                    for hh in range(2):
                        ps, rp = nxt("S")
                        for k in range(8):
                            op("pe", lambda e, ps=ps, k=k, tt=tt, hh=hh, woh=woh: e.matmul(ps[:], lhsT=mT[:, k, tt * 128:(tt + 1) * 128],
                                                                                  rhs=woh[hh][0][:, k, 0:512], start=(k == 0), stop=(k == 7)),
                               reads=[r_mT, woh[hh][1]], writes=[rp])
                        op("dve", lambda e, ps=ps, zi=zi, hh=hh: e.scalar_tensor_tensor(
                            out=z[zi][:, hh * 512:(hh + 1) * 512], in0=xres[zi][:, hh * 512:(hh + 1) * 512], scalar=ALPHA, in1=ps[:],
                            op0=ALU.mult, op1=ALU.add), reads=[rp, r_xres[zi]], writes=[r_xres[zi]])
                    layer_norm(z[zi], r_z[zi], zi)
                    op("pool", lambda e, zi=zi: e.tensor_tensor(out=z[zi][:], in0=z[zi][:], in1=ln_t[:, 0, :], op=ALU.mult),
                       reads=[r_z[zi], r_ln], writes=[r_z[zi]])
                    op("pool", lambda e, zi=zi: e.tensor_tensor(out=z[zi][:], in0=z[zi][:], in1=ln_t[:, 1, :], op=ALU.add),
                       reads=[r_z[zi], r_ln], writes=[r_z[zi]])
                    dma("pool", lambda e, zi=zi, tt=tt, T0=T0: e.dma_start(out=x1_d[T0 + tt * 128:T0 + (tt + 1) * 128, :], in_=z[zi][:]),
                        reads=[r_z[zi]], writes=[], sem_res=r_x1s[zi])

            try:
              chk(1)
              for s in range(NSEQ):
                for c in range(NCH):
                    T0 = s * S + c * 512
                    gidx = s * NCH + c
                    xi = gidx % 2
                    xt, r_xt = xTc[xi], r_xTc[xi]

                    def load_xT(g_):
                        b_ = xTc[g_ % 2]
                        t0_ = g_ * 512
                        dma("pool", lambda e: e.dma_start(out=b_[:], in_=x_T[:, t0_:t0_ + 512].rearrange("(k p) n -> p k n", p=128)),
                            writes=[r_xTc[g_ % 2]])

                    if gidx == 0:
                        load_xT(0)
                    wA, r_wA = load_w(0, 768, conv=False)
                    for m in range(5):
                        ps, rp = nxt("S")
                        for k in range(8):
                            op("pe", lambda e, ps=ps, k=k, m=m, wA=wA, xt=xt: e.matmul(
                                ps[:], lhsT=wA[:, k, m * 128:(m + 1) * 128], rhs=xt[:, k, :], start=(k == 0), stop=(k == 7)),
                               reads=[r_wA, r_xt], writes=[rp])
                        if m < 4:
                            op("act", lambda e, ps=ps, m=m: e.activation(out=QsT[:, m, :], in_=ps[:], func=AF.Identity,
                                                                         bias=bqk_t[:, m:m + 1], scale=0.125),
                               reads=[rp, r_bqk], writes=[r_Qs])
                        else:
                            op("act", lambda e, ps=ps, c=c: e.activation(out=KsT[:, (c % 2) * 512:(c % 2 + 1) * 512], in_=ps[:], func=AF.Identity,
                                                                         bias=bqk_t[:, 4:5], scale=1.0),
                               reads=[rp, r_bqk], writes=[r_Ks[c % 2]])
                    for tt in range(4):
                        ps, rp = nxt("S")
                        for k in range(8):
                            op("pe", lambda e, ps=ps, k=k, tt=tt, wA=wA, xt=xt: e.matmul(
                                ps[:, 0:128], lhsT=xt[:, k, tt * 128:(tt + 1) * 128], rhs=wA[:, k, 640:768], start=(k == 0), stop=(k == 7)),
                               reads=[r_wA, r_xt], writes=[rp])
                        op("dve", lambda e, ps=ps, tt=tt, c=c: e.tensor_tensor(
                            out=Vs[:, (c % 2) * 4 + tt, :, 0:64], in0=ps[:, 0:128].rearrange("p (g d) -> p g d", g=2),
                            in1=bv_t[:, 0:128].rearrange("p (g d) -> p g d", g=2), op=ALU.add),
                           reads=[rp, r_bv], writes=[r_Vs[c % 2]])
                    wC, r_wC = load_w(768, 512, conv=False)
                    for m in range(4):
                        ps, rp = nxt("S")
                        for k in range(8):
                            op("pe", lambda e, ps=ps, k=k, m=m, wC=wC, xt=xt: e.matmul(
                                ps[:], lhsT=wC[:, k, m * 128:(m + 1) * 128], rhs=xt[:, k, :], start=(k == 0), stop=(k == 7)),
                               reads=[r_wC, r_xt], writes=[rp])
                        op("act", lambda e, ps=ps, m=m: e.activation(out=QmT[0:64, 2 * m, :], in_=ps[0:64, :], func=AF.Identity,
                                                                     bias=bqk_t[0:64, 5 + m:6 + m], scale=0.125),
                           reads=[rp, r_bqk], writes=[r_Qm])
                        op("act", lambda e, ps=ps, m=m: e.activation(out=QmT[64:128, 2 * m + 1, :], in_=ps[64:128, :], func=AF.Identity,
                                                                     bias=bqk_t[64:128, 5 + m:6 + m], scale=0.125),
                           reads=[rp, r_bqk], writes=[r_Qm])
                    wD, r_wD = load_w(1280, 512)
                    for m in range(4):
                        ps, rp = nxt("S")
                        for k in range(8):
                            op("pe", lambda e, ps=ps, k=k, m=m, wD=wD, xt=xt: e.matmul(
                                ps[:], lhsT=wD[:, k, m * 128:(m + 1) * 128], rhs=xt[:, k, :], start=(k == 0), stop=(k == 7)),
                               reads=[r_wD, r_xt], writes=[rp])
                        op("act", lambda e, ps=ps, m=m, c=c: e.activation(out=KmT[:, m, c * 512:(c + 1) * 512], in_=ps[:], func=AF.Identity,
                                                                          bias=bqk_t[:, 9 + m:10 + m], scale=1.0),
                           reads=[rp, r_bqk], writes=[r_Km[c]])
                    wE, r_wE = load_w(1792, 512)
                    for tt in range(4):
                        ps, rp = nxt("S")
                        for k in range(8):
                            op("pe", lambda e, ps=ps, k=k, tt=tt, wE=wE, xt=xt: e.matmul(
                                ps[:], lhsT=xt[:, k, tt * 128:(tt + 1) * 128], rhs=wE[:, k, 0:512], start=(k == 0), stop=(k == 7)),
                               reads=[r_wE, r_xt], writes=[rp])
                        op("dve", lambda e, ps=ps, tt=tt, c=c: e.tensor_tensor(
                            out=Vm[:, c * 4 + tt, :, 0:64], in0=ps[:].rearrange("p (g d) -> p g d", g=8),
                            in1=bv_t[:, 128:640].rearrange("p (g d) -> p g d", g=8), op=ALU.add),
                           reads=[rp, r_bv], writes=[r_Vm[c]])
                    chk(2)
                    op("dve", lambda e, c=c: e.tensor_reduce(out=kms[:], in_=KmT[:, :, c * 512:(c + 1) * 512].rearrange("p m (b t) -> p m b t", b=2),
                                                             axis=AX.X, op=ALU.add), reads=[r_Km[c]], writes=[r_kms])
                    op("dve", lambda e, c=c: e.tensor_scalar(out=kmT[:, :, 2 * c:2 * c + 2], in0=kms[:], scalar1=1.0 / 256.0, scalar2=None,
                                                             op0=ALU.mult), reads=[r_kms], writes=[r_km])
                    while pending_wout:
                        wout_section(pending_wout.pop(0))
                    if gidx + 1 < NSEQ * NCH:
                        load_xT(gidx + 1)
                    chk(3)
                    for tt in range(4):
                        qb = 2 * c + tt // 2
                        pse, rpe = nxt("S")
                        pso, rpo_ = nxt("S")
                        for h in (0, 2, 4, 6, 1, 3, 5, 7):
                            hb = (h % 2) * 64
                            pp, rpp = (pse, rpe) if h % 2 == 0 else (pso, rpo_)
                            op("pe", lambda e, pp=pp, h=h, hb=hb, tt=tt: e.matmul(
                                pp[:, (h // 2) * 16:(h // 2 + 1) * 16], lhsT=QmT[hb:hb + 64, h, tt * 128:(tt + 1) * 128],
                                rhs=kmT[hb:hb + 64, h // 2, :], start=True, stop=True),
                               reads=[r_Qm, r_km], writes=[rpp])
                        chk(31)
                        gm4 = gm[:].rearrange("p (a b) n -> p a b n", b=2)
                        pm4 = PMr[:, :, 16 - qb:32 - qb].rearrange("p (a b) n -> p a b n", b=2)
                        op("dve", lambda e, pse=pse, gm4=gm4, pm4=pm4: e.tensor_tensor(
                            out=gm4[:, :, 0, :], in0=pse[:, 0:64].rearrange("p (h n) -> p h n", h=4), in1=pm4[:, :, 0, :], op=ALU.add),
                           reads=[rpe, r_PMr], writes=[r_gm])
                        op("dve", lambda e, pso=pso, gm4=gm4, pm4=pm4: e.tensor_tensor(
                            out=gm4[:, :, 1, :], in0=pso[:, 0:64].rearrange("p (h n) -> p h n", h=4), in1=pm4[:, :, 1, :], op=ALU.add),
                           reads=[rpo_, r_PMr, r_gm], writes=[r_gm])
                        chk(32)
                        for h in range(8):
                            op("dve", lambda e, h=h: e.max(out=mx[:, h, :], in_=gm[:, h, :]), reads=[r_gm], writes=[r_mx])
                        chk(33)
                        op("dve", lambda e: e.tensor_scalar(out=thr[:], in0=mx[:, :, 2], scalar1=-1.0e30, scalar2=None, op0=ALU.max),
                           reads=[r_mx], writes=[r_thr])
                        chk(34)
                        op("dve", lambda e: e.tensor_tensor(out=sel[:], in0=gm[:], in1=thr[:].unsqueeze(2).to_broadcast([128, 8, 16]), op=ALU.is_ge),
                           reads=[r_gm, r_thr], writes=[r_sel])
                        op("dve", lambda e, qb=qb: e.tensor_tensor(out=sel[:], in0=sel[:], in1=OWr[:, :, 16 - qb:32 - qb], op=ALU.add),
                           reads=[r_sel, r_OWr], writes=[r_sel])
                        op("dve", lambda e: e.tensor_scalar(out=sel[:], in0=sel[:], scalar1=-1.0, scalar2=BIG, op0=ALU.add, op1=ALU.mult),
                           reads=[r_sel], writes=[r_sel])
                        op("dve", lambda e, tt=tt: e.tensor_tensor(out=madd4[tt][:].rearrange("p (h n) -> p h n", h=8), in0=sel[:],
                                                                   in1=t31_t[:].unsqueeze(2).to_broadcast([128, 8, 16]), op=ALU.add),
                           reads=[r_sel, r_t31], writes=[r_madd4[tt]])
                    chk(5)
                    items = []
                    for tt in range(4):
                        b = c * 4 + tt
                        for g in range(2):
                            whichs = [0] if b == 0 else [0, 1]
                            for wi, which in enumerate(whichs):
                                items.append((tt, g, wi, which, len(whichs), b))
                    obank = {}

                    def swa_S(it):
                        tt, g, wi, which, nw, b = it
                        if wi == 0:
                            obank[(tt, g)] = nxt("O")
                        kt = b - which
                        kc0 = ((kt // 4) % 2) * 512 + (kt % 4) * 128
                        ps, rp = nxt("S")
                        op("pe", lambda e, ps=ps, g=g, kc0=kc0, tt=tt: e.matmul(
                            ps[:], lhsT=KsT[g * 64:(g + 1) * 64, kc0:kc0 + 128],
                            rhs=QsT[g * 64:(g + 1) * 64, :, tt * 128:(tt + 1) * 128], start=True, stop=False),
                           reads=[r_Ks[(kt // 4) % 2], r_Qs], writes=[rp])
                        op("pe", lambda e, ps=ps, g=g, which=which: e.matmul(
                            ps[:], lhsT=ident[:], rhs=Gs[:, which, g * 4:(g + 1) * 4, :], start=False, stop=True),
                           reads=[r_ident, r_Gs], writes=[rp])
                        pi = pctr[0] % 3
                        pctr[0] += 1
                        op("act", lambda e, ps=ps, pi=pi: e.activation(out=PT[pi][:], in_=ps[:], func=AF.Exp),
                           reads=[rp], writes=[r_PT[pi]])
                        return pi

                    def swa_PV(it, pi):
                        tt, g, wi, which, nw, b = it
                        po, rpo = obank[(tt, g)]
                        kt = b - which
                        for j in range(4):
                            first = (wi == 0 and j == 0)
                            op("pe", lambda e, po=po, pi=pi, j=j, kt=kt, g=g, first=first, wi=wi, nw=nw: e.matmul(
                                po[:, j * 65:(j + 1) * 65], lhsT=PT[pi][:, j * 128:(j + 1) * 128], rhs=Vs[:, kt % 8, g, :],
                                start=first, stop=(wi == nw - 1), skip_group_check=True),
                               reads=[r_PT[pi], r_Vs[(kt // 4) % 2]], writes=[rpo])
                        if wi == nw - 1:
                            pov = po[:, 0:260].rearrange("p (t d) -> p t d", t=4)
                            op("dve", lambda e, pov=pov, g=g: e.tensor_tensor(out=rden[:], in0=pov[:, :, 64], in1=es_t[:, g * 4:(g + 1) * 4], op=ALU.add),
                               reads=[rpo, r_es], writes=[r_rden])
                            op("dve", lambda e: e.reciprocal(out=rden[:], in_=rden[:]), reads=[r_rden], writes=[r_rden])
                            op("dve", lambda e, pov=pov, g=g, tt=tt: e.tensor_tensor(
                                out=ytok[:, tt, g * 256:(g + 1) * 256].rearrange("p (j d) -> p j d", j=4), in0=pov[:, :, 0:64],
                                in1=rden[:].unsqueeze(2).to_broadcast([128, 4, 64]), op=ALU.mult),
                               reads=[rpo, r_rden], writes=[r_ytok])

                    prev = None
                    for it in items:
                        pi = swa_S(it)
                        if prev is not None:
                            swa_PV(*prev)
                        prev = (it, pi)
                    swa_PV(*prev)
                    for tt in range(4):
                        pt, rpt = nxt("T")
                        op("pe", lambda e, pt=pt, tt=tt: e.transpose(pt[:, 0:128], madd4[tt][:], ident[:]), reads=[r_madd4[tt], r_ident], writes=[rpt])
                        op("act", lambda e, pt=pt, tt=tt: e.activation(out=maddT[:, tt * 128:(tt + 1) * 128], in_=pt[:, 0:128], func=AF.Copy),
                           reads=[rpt], writes=[r_maddT])
                    chk(4)
                    nkt = 4 * c + 4
                    for h in range(8):
                        hb = (h % 2) * 64
                        po, rpo = nxt("O")

                        def emit_S(kt, h=h, hb=hb):
                            rel = kt - 4 * c
                            n = kt // 2
                            ps, rp = nxt("S")
                            last = rel < -1
                            op("pe", lambda e, ps=ps, kt=kt: e.matmul(
                                ps[:], lhsT=KmT[:, h // 2, kt * 128:(kt + 1) * 128], rhs=QmT[:, h, :],
                                start=True, stop=False), reads=[r_Km[kt // 4], r_Qm], writes=[rp])
                            p = h * 16 + n
                            op("pe", lambda e, ps=ps, p=p, last=last: e.matmul(
                                ps[:], lhsT=ident[:, p:p + 1].to_broadcast([128, 128]), rhs=maddT[:], start=False, stop=last),
                               reads=[r_ident, r_maddT], writes=[rp])
                            if not last:
                                off = 384 - 128 * rel
                                wid = min(512, 640 - off)
                                op("pe", lambda e, ps=ps, off=off, wid=wid: e.matmul(
                                    ps[:, 0:wid], lhsT=ident[:], rhs=Gb[:, h, off:off + wid], start=False, stop=True, skip_group_check=True),
                                   reads=[r_ident, r_Gb], writes=[rp])
                            pi = pctr[0] % 3
                            pctr[0] += 1
                            op("act", lambda e, ps=ps, pi=pi: e.activation(out=PT[pi][:], in_=ps[:], func=AF.Exp),
                               reads=[rp], writes=[r_PT[pi]])
                            return pi

                        def emit_PV(kt, pi, h=h, po=po, rpo=rpo):
                            for tt in range(4):
                                first = (kt == 0 and tt == 0)
                                op("pe", lambda e, pi=pi, tt=tt, kt=kt, first=first, nkt=nkt: e.matmul(
                                    po[:, tt * 65:(tt + 1) * 65], lhsT=PT[pi][:, tt * 128:(tt + 1) * 128], rhs=Vm[:, kt, h, :],
                                    start=first, stop=(kt == nkt - 1), skip_group_check=True),
                                   reads=[r_PT[pi], r_Vm[kt // 4]], writes=[rpo])

                        conv_step()
                        pend = []
                        for kt in range(nkt):
                            pi = emit_S(kt)
                            pend.append((kt, pi))
                            if len(pend) > 2:
                                emit_PV(*pend.pop(0))
                        while pend:
                            emit_PV(*pend.pop(0))
                        pov = po[:, 0:260].rearrange("p (t d) -> p t d", t=4)
                        op("dve", lambda e, pov=pov: e.reciprocal(out=rden[:], in_=pov[:, :, 64]), reads=[rpo], writes=[r_rden])
                        op("dve", lambda e, pov=pov, h=h: e.tensor_tensor(
                            out=ytok[:, :, 512 + h * 64:512 + (h + 1) * 64], in0=pov[:, :, 0:64],
                            in1=rden[:].unsqueeze(2).to_broadcast([128, 4, 64]), op=ALU.mult),
                           reads=[rpo, r_rden], writes=[r_ytok])
                    chk(6)
                    for j in range(8):
                        pt, rpt = nxt("T")
                        for tt in range(4):
                            op("pe", lambda e, pt=pt, tt=tt, j=j: e.transpose(pt[:, tt * 128:(tt + 1) * 128], ytok[:, tt, j * 128:(j + 1) * 128], ident[:]),
                               reads=[r_ytok, r_ident], writes=[rpt])
                        op("act", lambda e, pt=pt, j=j: e.activation(out=yT[:, j, :], in_=pt[:, 0:512], func=AF.Copy),
                           reads=[rpt], writes=[r_yT])
                    chk(7)
                    for j in range(8):
                        if j % 2 == 0:
                            wF1, r_wF1 = load_w(2304 + (j // 2) * 512, 512)
                            wF2, r_wF2 = wF1, r_wF1
                        jo = (j % 2) * 128
                        pa, rpa = nxt("S")
                        for k in range(4):
                            op("pe", lambda e, pa=pa, k=k, j=j: e.matmul(pa[:], lhsT=wa[:, k, j * 128:(j + 1) * 128], rhs=yT[:, k, :],
                                                                         start=(k == 0), stop=(k == 3)), reads=[r_wa, r_yT], writes=[rpa])
                        pb, rpb = nxt("S")
                        for k in range(4):
                            op("pe", lambda e, pb=pb, k=k, j=j: e.matmul(pb[:], lhsT=wb[:, k, j * 128:(j + 1) * 128], rhs=yT[:, 4 + k, :],
                                                                         start=(k == 0), stop=(k == 3)), reads=[r_wb, r_yT], writes=[rpb])
                        pg1, rpg1 = nxt("S")
                        for k in range(8):
                            op("pe", lambda e, pg1=pg1, k=k, jo=jo, wF1=wF1, xt=xt: e.matmul(pg1[:], lhsT=wF1[:, k, jo:jo + 128], rhs=xt[:, k, :],
                                                                                           start=(k == 0), stop=(k == 7)), reads=[r_wF1, r_xt], writes=[rpg1])
                        op("act", lambda e, pg1=pg1, j=j: e.activation(out=g1[:], in_=pg1[:], func=AF.Sigmoid, bias=bg_t[:, j:j + 1], scale=1.0),
                           reads=[rpg1, r_bg], writes=[r_g1])
                        pg2, rpg2 = nxt("S")
                        for k in range(8):
                            op("pe", lambda e, pg2=pg2, k=k, jo=jo, wF2=wF2, xt=xt: e.matmul(pg2[:], lhsT=wF2[:, k, 256 + jo:256 + jo + 128], rhs=xt[:, k, :],
                                                                                           start=(k == 0), stop=(k == 7)), reads=[r_wF2, r_xt], writes=[rpg2])
                        op("act", lambda e, pg2=pg2, j=j: e.activation(out=g2[:], in_=pg2[:], func=AF.Sigmoid, bias=bg_t[:, 8 + j:9 + j], scale=1.0),
                           reads=[rpg2, r_bg], writes=[r_g2])
                        op("dve", lambda e, pa=pa: e.tensor_tensor(out=t1[:], in0=pa[:], in1=g1[:], op=ALU.mult), reads=[rpa, r_g1], writes=[r_t1])
                        op("dve", lambda e, pb=pb: e.tensor_tensor(out=t2[:], in0=pb[:], in1=g2[:], op=ALU.mult), reads=[rpb, r_g2], writes=[r_t2])
                        op("pool", lambda e, j=j: e.tensor_tensor(out=mT[:, j, :], in0=t1[:], in1=t2[:], op=ALU.add), reads=[r_t1, r_t2], writes=[r_mT])
                    chk(8)
                    pending_wout.append(T0)
            except StopBuild:
                pass
            while pending_wout:
                wout_section(pending_wout.pop(0))
            while conv_state["next"] < len(conv_list) or conv_state["pending"] is not None:
                conv_step()
                conv_flush()
            S_.barrier()
            with nc.Block() as blk:
                S_.emit(blk)
            if int(os.environ.get('KSTOP', '99')) < 20:
                return nc

        with ExitStack() as st:
            sb = lambda name, shape, ty: st.enter_context(nc.sbuf_tensor(name, shape, ty))
            CH = 1024
            NMC = NT // CH
            ident = sb("ident2", [128, 128], BF16); r_ident = Res("ident2")
            identf = sb("identf2", [128, 128], F32); r_identf = Res("identf2")
            op("pool", lambda e: e.memset(identf[:], 0.0), writes=[r_identf])
            op("pool", lambda e: e.affine_select(out=identf[:], in_=identf[:], pattern=[[-1, 128]],
                                                 compare_op=ALU.not_equal, fill=1.0, base=0,
                                                 channel_multiplier=1), reads=[r_identf], writes=[r_identf])
            op("dve", lambda e: e.tensor_copy(out=ident[:], in_=identf[:]), reads=[r_identf], writes=[r_ident])
            ln_t = sb("ln2_t", [128, 2, D], F32); r_ln = Res("ln2")
            for i in range(2):
                dma("sp", lambda e, i=i: e.dma_start(out=ln_t[:, i, :], in_=ln_d[2 + i:3 + i, :].partition_broadcast(128)), writes=[r_ln])
            wr = sb("wr_t", [128, 8, 36], BF16); r_wr = Res("wr")
            br = sb("br_t", [128, 36], F32); r_br = Res("br")
            dma("pool", lambda e: e.dma_start(out=wr[:], in_=wr_d.rearrange("(k p) n -> p k n", p=128)), writes=[r_wr])
            dma("sp", lambda e: e.dma_start(out=br[:], in_=br_d.partition_broadcast(128)), writes=[r_br])
            I32 = mybir.dt.int32
            BLK = 256
            NTILE = NT // 128
            NB = (2 * NT) // BLK + NEXP
            CAP = NB * BLK
            wgb2 = wgb_d.rearrange("e p n -> (e p) n")
            wub2 = wub_d.rearrange("e p n -> (e p) n")
            wdb2 = wdb_d.rearrange("e p n -> (e p) n")
            r_xbuf = Res("xbuf"); r_ybuf = Res("ybuf")
            U = sb("U", [128, 128], BF16); r_U = Res("U")
            Uf = identf
            op("pool", lambda e: e.memset(Uf[:], 1.0), reads=[r_identf], writes=[r_identf])
            op("pool", lambda e: e.affine_select(out=Uf[:], in_=Uf[:], pattern=[[1, 128]], compare_op=ALU.is_gt, fill=0.0,
                                                 base=0, channel_multiplier=-1), reads=[r_identf], writes=[r_identf])
            op("dve", lambda e: e.tensor_copy(out=U[:], in_=Uf[:]), reads=[r_identf], writes=[r_U])
            ones = sb("ones", [128, 128], BF16); r_ones = Res("ones")
            op("pool", lambda e: e.memset(ones[:], 1.0), writes=[r_ones])
            pci = sb("pci", [128, 1], I32); r_pci = Res("pci")
            pcf = sb("pcf", [128, 2], F32); r_pcf = Res("pcf")
            op("pool", lambda e: e.iota(pci[:], pattern=[[0, 1]], base=0, channel_multiplier=1), writes=[r_pci])
            op("dve", lambda e: e.tensor_copy(out=pcf[:, 0:1], in_=pci[:]), reads=[r_pci], writes=[r_pcf])
            op("dve", lambda e: e.tensor_scalar(out=pcf[:, 1:2], in0=pcf[:, 0:1], scalar1=float(BLK), scalar2=None, op0=ALU.mult),
               reads=[r_pcf], writes=[r_pcf])
            bvi = sb("bvi", [128, NB], I32); r_bvi = Res("bvi")
            bvf = sb("bvf", [128, NB], F32); r_bvf = Res("bvf")
            op("pool", lambda e: e.iota(bvi[:], pattern=[[1, NB]], base=0, channel_multiplier=0), writes=[r_bvi])
            op("dve", lambda e: e.tensor_copy(out=bvf[:], in_=bvi[:]), reads=[r_bvi], writes=[r_bvf])
            oh1 = sb("oh1", [128, NTILE, 32], BF16); r_oh1 = Res("oh1")
            oh2 = sb("oh2", [128, NTILE, 32], BF16); r_oh2 = Res("oh2")
            posAB = sb("posAB", [128, NTILE, 2], F32); r_pos = Res("posAB")
            wAB = sb("wAB", [128, NTILE, 2], F32); r_wAB = Res("wAB")
            idxAB = sb("idxAB", [128, NTILE, 2], I32); r_idx = Res("idxAB")
            base = sb("base", [128, 32], F32); r_base = Res("base")
            op("pool", lambda e: e.memset(base[:], 0.0), writes=[r_base])
            hT = [sb(f"hT{i}", [128, 8, CH], BF16) for i in range(2)]
            r_hT = [Res(f"hT{i}") for i in range(2)]
            xbr = [sb(f"xbr{i}", [128, D], BF16) for i in range(2)]
            r_xbr = [Res(f"xbr{i}") for i in range(2)]
            lg = sb("lg", [128, 36], F32); r_lg = Res("lg")
            sm = sb("sm", [128, 16], F32); r_sm = Res("sm")
            oh = sb("oh", [128, 4], F32); r_oh = Res("oh")
            em = sb("em", [128, 4, 8], F32); r_em = Res("em")
            m8 = sb("m8", [128, 8], F32); r_m8 = Res("m8")
            s1 = sb("s1", [128, 32], F32); r_s1 = Res("s1")
            s2 = sb("s2", [128, 32], F32); r_s2 = Res("s2")
            sB = sb("sB", [128, 32], F32); r_sB = Res("sB")
            Mb = sb("Mb", [128, 32], BF16); r_Mb = Res("Mb")
            pos = sb("pos", [128, 32], F32); r_posf = Res("posf")
            tmp = sb("tmp", [128, 32], F32); r_tmp = Res("tmp")
            gx = sb("gx", [128, 4], F32); r_gx = Res("gx")
            emf = em[:].rearrange("p g x -> p (g x)")
            for mc in range(NMC):
                T0 = mc * CH
                h_, r_h = hT[mc % 2], r_hT[mc % 2]
                for tt in range(CH // 128):
                    ti = mc * (CH // 128) + tt
                    xi_ = ti % 2
                    dma("pool", lambda e, ti=ti, xi_=xi_: e.dma_start(out=xbr[xi_][:], in_=x1_d[ti * 128:(ti + 1) * 128, :]), writes=[r_xbr[xi_]])
                    pt, rpt = nxt("T")
                    for j in range(8):
                        op("pe", lambda e, pt=pt, j=j, xi_=xi_: e.transpose(pt[:, j * 128:(j + 1) * 128], xbr[xi_][:, j * 128:(j + 1) * 128], ident[:]),
                           reads=[r_xbr[xi_], r_ident], writes=[rpt])
                    op("act", lambda e, pt=pt, tt=tt, h_=h_: e.activation(out=h_[:, :, tt * 128:(tt + 1) * 128], in_=pt[:].rearrange("p (j t) -> p j t", j=8), func=AF.Copy),
                       reads=[rpt], writes=[r_h])
                    ps, rp = nxt("S")
                    for k in range(8):
                        op("pe", lambda e, ps=ps, k=k, tt=tt, h_=h_: e.matmul(ps[:, 0:36], lhsT=h_[:, k, tt * 128:(tt + 1) * 128], rhs=wr[:, k, :],
                                                                              start=(k == 0), stop=(k == 7)), reads=[r_h, r_wr], writes=[rp])
                    op("dve", lambda e, ps=ps: e.tensor_tensor(out=lg[:], in0=ps[:, 0:36], in1=br[:], op=ALU.add), reads=[rp, r_br], writes=[r_lg])
                    op("dve", lambda e: e.tensor_reduce(out=sm[:, 0:1], in_=lg[:, 0:4], axis=AX.X, op=ALU.max), reads=[r_lg], writes=[r_sm])
                    op("dve", lambda e: e.tensor_scalar(out=oh[:], in0=lg[:, 0:4], scalar1=sm[:, 0:1], scalar2=None, op0=ALU.is_ge),
                       reads=[r_lg, r_sm], writes=[r_oh])
                    op("dve", lambda e: e.tensor_scalar(out=sm[:, 1:2], in0=sm[:, 0:1], scalar1=-1.0, scalar2=None, op0=ALU.mult),
                       reads=[r_sm], writes=[r_sm])
                    op("act", lambda e: e.activation(out=gx[:], in_=lg[:, 0:4], func=AF.Exp, bias=sm[:, 1:2], scale=1.0, accum_out=sm[:, 2:3]),
                       reads=[r_lg, r_sm], writes=[r_gx, r_sm])
                    op("dve", lambda e: e.reciprocal(out=sm[:, 3:4], in_=sm[:, 2:3]), reads=[r_sm], writes=[r_sm])
                    op("dve", lambda e: e.tensor_scalar(out=oh[:], in0=oh[:], scalar1=-1.0, scalar2=1.0e30, op0=ALU.add, op1=ALU.mult),
                       reads=[r_oh], writes=[r_oh])
                    op("dve", lambda e: e.tensor_tensor(out=em[:], in0=lg[:, 4:36].rearrange("p (g x) -> p g x", g=4),
                                                        in1=oh[:].unsqueeze(2).to_broadcast([128, 4, 8]), op=ALU.add),
                       reads=[r_lg, r_oh], writes=[r_em])
                    op("dve", lambda e: e.max(out=m8[:], in_=emf), reads=[r_em], writes=[r_m8])
                    op("dve", lambda e: e.tensor_scalar(out=s1[:], in0=emf, scalar1=m8[:, 0:1], scalar2=None, op0=ALU.is_ge),
                       reads=[r_em, r_m8], writes=[r_s1])
                    op("dve", lambda e: e.tensor_scalar(out=s2[:], in0=emf, scalar1=m8[:, 1:2], scalar2=None, op0=ALU.is_ge),
                       reads=[r_em, r_m8], writes=[r_s2])
                    op("dve", lambda e: e.tensor_tensor(out=sB[:], in0=s2[:], in1=s1[:], op=ALU.subtract), reads=[r_s1, r_s2], writes=[r_sB])
                    op("dve", lambda e: e.tensor_copy(out=Mb[:], in_=s2[:]), reads=[r_s2], writes=[r_Mb])
                    op("dve", lambda e, ti=ti: e.tensor_copy(out=oh1[:, ti, :], in_=s1[:]), reads=[r_s1], writes=[r_oh1])
                    op("dve", lambda e, ti=ti: e.tensor_copy(out=oh2[:, ti, :], in_=sB[:]), reads=[r_sB], writes=[r_oh2])
                    op("dve", lambda e: e.tensor_tensor(out=sm[:, 4:5], in0=m8[:, 1:2], in1=m8[:, 0:1], op=ALU.subtract), reads=[r_m8, r_sm], writes=[r_sm])
                    op("act", lambda e: e.activation(out=sm[:, 5:6], in_=sm[:, 4:5], func=AF.Exp), reads=[r_sm], writes=[r_sm])
                    op("dve", lambda e: e.tensor_scalar(out=sm[:, 6:7], in0=sm[:, 5:6], scalar1=1.0, scalar2=None, op0=ALU.add), reads=[r_sm], writes=[r_sm])
                    op("dve", lambda e: e.reciprocal(out=sm[:, 6:7], in_=sm[:, 6:7]), reads=[r_sm], writes=[r_sm])
                    op("dve", lambda e: e.tensor_tensor(out=sm[:, 7:8], in0=sm[:, 5:6], in1=sm[:, 6:7], op=ALU.mult), reads=[r_sm], writes=[r_sm])
                    op("dve", lambda e, ti=ti: e.tensor_scalar(out=wAB[:, ti, :], in0=sm[:, 6:8], scalar1=sm[:, 3:4], scalar2=None, op0=ALU.mult),
                       reads=[r_sm], writes=[r_wAB])
                    pp, rpp = nxt("S")
                    op("pe", lambda e, pp=pp: e.matmul(pp[:, 0:32], lhsT=U[:], rhs=Mb[:], start=True, stop=True), reads=[r_U, r_Mb], writes=[rpp])
                    op("dve", lambda e, pp=pp: e.tensor_tensor(out=pos[:], in0=pp[:, 0:32], in1=base[:], op=ALU.add), reads=[rpp, r_base], writes=[r_posf])
                    pq, rpq = nxt("S")
                    op("pe", lambda e, pq=pq: e.matmul(pq[:, 0:32], lhsT=ones[:], rhs=Mb[:], start=True, stop=True), reads=[r_ones, r_Mb], writes=[rpq])
                    op("dve", lambda e, pq=pq: e.tensor_tensor(out=base[:], in0=pq[:, 0:32], in1=base[:], op=ALU.add), reads=[rpq, r_base], writes=[r_base])
                    op("dve", lambda e: e.tensor_tensor(out=tmp[:], in0=pos[:], in1=s1[:], op=ALU.mult), reads=[r_posf, r_s1], writes=[r_tmp])
                    op("dve", lambda e, ti=ti: e.tensor_reduce(out=posAB[:, ti, 0:1], in_=tmp[:], axis=AX.X, op=ALU.add), reads=[r_tmp], writes=[r_pos])
                    op("dve", lambda e: e.tensor_tensor(out=tmp[:], in0=pos[:], in1=sB[:], op=ALU.mult), reads=[r_posf, r_sB], writes=[r_tmp])
                    op("dve", lambda e, ti=ti: e.tensor_reduce(out=posAB[:, ti, 1:2], in_=tmp[:], axis=AX.X, op=ALU.add), reads=[r_tmp], writes=[r_pos])
            cmpb = sb("cmpb", [128, 32], BF16); r_cmpb = Res("cmpb")
            nblk = sb("nblk", [128, 32], F32); r_nblk = Res("nblk")
            endb = sb("endb", [128, 32], F32); r_endb = Res("endb")
            sbase = sb("sbase", [128, 32], F32); r_sbase = Res("sbase")
            op("dve", lambda e: e.tensor_scalar(out=cmpb[:], in0=base[:], scalar1=pcf[:, 1:2], scalar2=None, op0=ALU.is_gt),
               reads=[r_base, r_pcf], writes=[r_cmpb])
            pn, rpn = nxt("S")
            op("pe", lambda e: e.matmul(pn[:, 0:32], lhsT=ones[:], rhs=cmpb[:], start=True, stop=True), reads=[r_ones, r_cmpb], writes=[rpn])
            op("dve", lambda e: e.tensor_copy(out=nblk[:], in_=pn[:, 0:32]), reads=[rpn], writes=[r_nblk])
            op("dve", lambda e: e.tensor_copy(out=endb[:, 0:1], in_=nblk[:, 0:1]), reads=[r_nblk], writes=[r_endb])
            for ex in range(1, NEXP):
                op("dve", lambda e, ex=ex: e.tensor_tensor(out=endb[:, ex:ex + 1], in0=endb[:, ex - 1:ex], in1=nblk[:, ex:ex + 1], op=ALU.add),
                   reads=[r_endb, r_nblk], writes=[r_endb])
            op("dve", lambda e: e.tensor_tensor(out=sbase[:], in0=endb[:], in1=nblk[:], op=ALU.subtract), reads=[r_endb, r_nblk], writes=[r_sbase])
            op("dve", lambda e: e.tensor_scalar(out=sbase[:], in0=sbase[:], scalar1=float(BLK), scalar2=None, op0=ALU.mult), reads=[r_sbase], writes=[r_sbase])
            cmp3 = sb("cmp3", [128, NB, 32], F32); r_cmp3 = Res("cmp3")
            bex = sb("bex", [128, NB], F32); r_bex = Res("bex")
            idxw = sb("idxw", [128, NB], I32); r_idxw = Res("idxw")
            op("dve", lambda e: e.tensor_tensor(out=cmp3[:], in0=endb[:].unsqueeze(1).to_broadcast([128, NB, 32]),
                                                in1=bvf[:].unsqueeze(2).to_broadcast([128, NB, 32]), op=ALU.is_le),
               reads=[r_endb, r_bvf], writes=[r_cmp3])
            op("dve", lambda e: e.tensor_reduce(out=bex[:], in_=cmp3[:], axis=AX.X, op=ALU.add), reads=[r_cmp3], writes=[r_bex])
            op("dve", lambda e: e.tensor_scalar(out=bex[:], in0=bex[:], scalar1=float(NEXP - 1), scalar2=128.0, op0=ALU.min, op1=ALU.mult),
               reads=[r_bex], writes=[r_bex])
            op("dve", lambda e: e.tensor_scalar(out=bex[:], in0=bex[:], scalar1=pcf[:, 0:1], scalar2=None, op0=ALU.add), reads=[r_bex, r_pcf], writes=[r_bex])
            op("dve", lambda e: e.tensor_copy(out=idxw[:], in_=bex[:]), reads=[r_bex], writes=[r_idxw])
            tmp3 = sb("tmp3", [128, 32], F32); r_tmp3 = Res("tmp3")
            slf = sb("slf", [128, 2], F32); r_slf = Res("slf")
            xb = [sb(f"xb{i}", [128, D], BF16) for i in range(2)]
            r_xbs = [Res("xbs0"), Res("xbs1")]
            r_xb = [Res(f"xb{i}") for i in range(2)]
            big3 = cmp3[:, 0:NTILE, :]; r_big3 = r_cmp3
            slall = sb("slall", [128, NTILE, 2], F32); r_slall = Res("slall")
            for ab, ohx, r_ohx in ((0, oh1, r_oh1), (1, oh2, r_oh2)):
                op("dve", lambda e, ohx=ohx: e.tensor_tensor(out=big3, in0=ohx[:], in1=sbase[:].unsqueeze(1).to_broadcast([128, NTILE, 32]), op=ALU.mult),
                   reads=[r_ohx, r_sbase], writes=[r_big3])
                op("dve", lambda e, ab=ab: e.tensor_reduce(out=slall[:, :, ab], in_=big3, axis=AX.X, op=ALU.add), reads=[r_big3], writes=[r_slall])
            op("dve", lambda e: e.tensor_tensor(out=slall[:], in0=slall[:], in1=posAB[:], op=ALU.add), reads=[r_slall, r_pos], writes=[r_slall])
            op("dve", lambda e: e.tensor_copy(out=idxAB[:], in_=slall[:]), reads=[r_slall], writes=[r_idx])
            for ti in range(NTILE):
                xi = ti % 2
                if ti == 0:
                    dma("pool", lambda e: e.dma_start(out=xb[0][:], in_=x1_d[0:128, :]), reads=[r_x1], writes=[r_xb[0]])
                if ti + 1 < NTILE:
                    dma("pool", lambda e, ti=ti: e.dma_start(out=xb[(ti + 1) % 2][:], in_=x1_d[(ti + 1) * 128:(ti + 2) * 128, :]),
                        reads=[r_x1], writes=[r_xb[(ti + 1) % 2]])
                for ab in range(2):
                    dma("pool", lambda e, ti=ti, xi=xi, ab=ab: e.indirect_dma_start(
                        out=xbuf_d, out_offset=bass.IndirectOffsetOnAxis(ap=idxAB[:, ti, ab:ab + 1], axis=0), in_=xb[xi][:], in_offset=None),
                        reads=[r_xb[xi], r_idx], writes=[], sem_res=r_xbs[xi])
            fence = sb("fence", [128, D], BF16); r_fence = Res("fence")
            dma("pool", lambda e: e.dma_start(out=fence[:], in_=xbuf_d[CAP - 128:CAP, :]), writes=[r_fence])
            dma("pool", lambda e: e.dma_start(out=fence[:], in_=xbuf_d[0:128, :]), writes=[r_fence])
            S_.barrier()
            wgt = [sb(f"wgt{i}", [128, 8, DE], BF16) for i in range(2)]
            wut = [sb(f"wut{i}", [128, 8, DE], BF16) for i in range(2)]
            wdt = [sb(f"wdt{i}", [128, 4, D], BF16) for i in range(2)]
            r_weg = [Res(f"weg{i}") for i in range(2)]
            r_weu = [Res(f"weu{i}") for i in range(2)]
            r_wed = [Res(f"wed{i}") for i in range(2)]
            xs = [sb(f"xs{i}", [128, 2, D], BF16) for i in range(2)]
            r_xs = [Res(f"xs{i}") for i in range(2)]
            xT = [sb(f"xT{i}", [128, 8, BLK], BF16) for i in range(2)]
            r_xT = [Res(f"xT{i}") for i in range(2)]
            actT = sb("actT", [128, 4, BLK], BF16); r_actT = [Res(f"actT{i}") for i in range(4)]
            sg = [sb(f"sg{i}", [128, BLK], F32) for i in range(2)]
            r_sg = [Res(f"sg{i}") for i in range(2)]
            yb = [sb(f"yb{i}", [128, D], F32) for i in range(2)]
            r_yb = [Res(f"yb{i}") for i in range(2)]
            yctr = [0]
            def load_block(b):
                bi = b % 2
                for (wt_, w2, rw) in ((wgt, wgb2, r_weg), (wut, wub2, r_weu), (wdt, wdb2, r_wed)):
                    dma("pool", lambda e, b=b, bi=bi, wt_=wt_, w2=w2: e.indirect_dma_start(
                        out=wt_[bi][:].rearrange("p k n -> p (k n)"), out_offset=None, in_=w2,
                        in_offset=bass.IndirectOffsetOnAxis(ap=idxw[:, b:b + 1], axis=0)), reads=[r_idxw], writes=[rw[bi]])
                dma("sp", lambda e, b=b, bi=bi: e.dma_start(out=xs[bi][:], in_=xbuf_d[b * BLK:(b + 1) * BLK, :].rearrange("(s p) d -> p s d", p=128)),
                    writes=[r_xs[bi]])

            load_block(0)
            for b in range(NB):
                bi = b % 2
                if b + 1 < NB:
                    load_block(b + 1)
                for st_ in range(2):
                    pt, rpt = nxt("T")
                    for j in range(8):
                        op("pe", lambda e, pt=pt, j=j, st_=st_, bi=bi: e.transpose(pt[:, j * 128:(j + 1) * 128], xs[bi][:, st_, j * 128:(j + 1) * 128], ident[:]),
                           reads=[r_xs[bi], r_ident], writes=[rpt])
                    if st_ == 0:
                        op("act", lambda e, pt=pt, bi=bi, st_=st_: e.activation(out=xT[bi][:, :, st_ * 128:(st_ + 1) * 128],
                                                                               in_=pt[:].rearrange("p (j t) -> p j t", j=8), func=AF.Copy),
                           reads=[rpt], writes=[r_xT[bi]])
                    else:
                        op("dve", lambda e, pt=pt, bi=bi, st_=st_: e.tensor_copy(out=xT[bi][:, :, st_ * 128:(st_ + 1) * 128],
                                                                                in_=pt[:].rearrange("p (j t) -> p j t", j=8)),
                           reads=[rpt], writes=[r_xT[bi]])
                for fc in range(4):
                    pg, rpg = nxt("S")
                    for k in range(8):
                        op("pe", lambda e, pg=pg, k=k, fc=fc, bi=bi: e.matmul(pg[:, 0:BLK], lhsT=wgt[bi][:, k, fc * 128:(fc + 1) * 128], rhs=xT[bi][:, k, :],
                                                                              start=(k == 0), stop=(k == 7)), reads=[r_weg[bi], r_xT[bi]], writes=[rpg])
                    pu, rpu = nxt("S")
                    for k in range(8):
                        op("pe", lambda e, pu=pu, k=k, fc=fc, bi=bi: e.matmul(pu[:, 0:BLK], lhsT=wut[bi][:, k, fc * 128:(fc + 1) * 128], rhs=xT[bi][:, k, :],
                                                                              start=(k == 0), stop=(k == 7)), reads=[r_weu[bi], r_xT[bi]], writes=[rpu])
                    si = fc % 2
                    op("act", lambda e, pg=pg, si=si: e.activation(out=sg[si][:], in_=pg[:, 0:BLK], func=AF.Silu), reads=[rpg], writes=[r_sg[si]])
                    op("dve", lambda e, pu=pu, si=si, fc=fc: e.tensor_tensor(out=actT[:, fc, :], in0=pu[:, 0:BLK], in1=sg[si][:], op=ALU.mult),
                       reads=[rpu, r_sg[si]], writes=[r_actT[fc]])
                for st_ in range(2):
                    yi = yctr[0] % 2
                    yctr[0] += 1
                    for hh in range(2):
                        ps, rp = nxt("O")
                        for fc in range(4):
                            op("pe", lambda e, ps=ps, fc=fc, st_=st_, hh=hh, bi=bi: e.matmul(
                                ps[:], lhsT=actT[:, fc, st_ * 128:(st_ + 1) * 128], rhs=wdt[bi][:, fc, hh * 512:(hh + 1) * 512],
                                start=(fc == 0), stop=(fc == 3)), reads=[r_actT[fc], r_wed[bi]], writes=[rp])
                        if hh == 0:
                            op("act", lambda e, ps=ps, yi=yi, hh=hh: e.activation(out=yb[yi][:, hh * 512:(hh + 1) * 512], in_=ps[:], func=AF.Copy),
                               reads=[rp], writes=[r_yb[yi]])
                        else:
                            op("dve", lambda e, ps=ps, yi=yi, hh=hh: e.tensor_copy(out=yb[yi][:, hh * 512:(hh + 1) * 512], in_=ps[:]),
                               reads=[rp], writes=[r_yb[yi]])
                    dma("sp", lambda e, b=b, st_=st_, yi=yi: e.dma_start(out=ybuf_d[b * BLK + st_ * 128:b * BLK + (st_ + 1) * 128, :], in_=yb[yi][:]),
                        reads=[r_yb[yi]], writes=[], sem_res=r_ybuf)
            S_.barrier()
            yA = [sb(f"yA{i}", [128, D], F32) for i in range(4)]
            yB = [sb(f"yB{i}", [128, D], F32) for i in range(4)]
            r_yA = [Res(f"yA{i}") for i in range(4)]
            r_yB = [Res(f"yB{i}") for i in range(4)]
            xr = [sb(f"xr{i}", [128, D], F32) for i in range(4)]
            r_xr = [Res(f"xr{i}") for i in range(4)]
            stats_l = [sb(f"stats2{i}", [128, 2, 6], F32) for i in range(4)]; r_stats_l = [Res(f"stats2{i}") for i in range(4)]
            mv_l = [sb(f"mv2{i}", [128, 2], F32) for i in range(4)]; r_mv_l = [Res(f"mv2{i}") for i in range(4)]
            rstd_l = [sb(f"rstd2{i}", [128, 1], F32) for i in range(4)]; r_rstd_l = [Res(f"rstd2{i}") for i in range(4)]
            def combine_fetch(ti):
                xi = ti % 4
                dma("pool", lambda e, ti=ti, xi=xi: e.indirect_dma_start(out=yA[xi][:], out_offset=None, in_=ybuf_d,
                                                                        in_offset=bass.IndirectOffsetOnAxis(ap=idxAB[:, ti, 0:1], axis=0)),
                    reads=[r_idx], writes=[r_yA[xi]])
                dma("pool", lambda e, ti=ti, xi=xi: e.indirect_dma_start(out=yB[xi][:], out_offset=None, in_=ybuf_d,
                                                                        in_offset=bass.IndirectOffsetOnAxis(ap=idxAB[:, ti, 1:2], axis=0)),
                    reads=[r_idx], writes=[r_yB[xi]])
                dma("sp", lambda e, xi=xi, ti=ti: e.dma_start(out=xr[xi][:], in_=x1_d[ti * 128:(ti + 1) * 128, :]), reads=[r_x1], writes=[r_xr[xi]])

            def cvars(ti):
                xi = ti % 4
                return xi, stats_l[xi], r_stats_l[xi], mv_l[xi], r_mv_l[xi], rstd_l[xi], r_rstd_l[xi]

            def combine_A(ti):
                xi, stats, r_stats, mv, r_mv, rstd, r_rstd = cvars(ti)
                op("act", lambda e, xi=xi: e.activation(out=xr[xi][:], in_=xr[xi][:], func=AF.Copy, scale=ALPHA), reads=[r_xr[xi]], writes=[r_xr[xi]])
                op("dve", lambda e, xi=xi, ti=ti: e.scalar_tensor_tensor(out=xr[xi][:], in0=yA[xi][:], scalar=wAB[:, ti, 0:1], in1=xr[xi][:],
                                                                         op0=ALU.mult, op1=ALU.add), reads=[r_yA[xi], r_wAB, r_xr[xi]], writes=[r_xr[xi]])
                op("dve", lambda e, xi=xi, ti=ti: e.scalar_tensor_tensor(out=xr[xi][:], in0=yB[xi][:], scalar=wAB[:, ti, 1:2], in1=xr[xi][:],
                                                                         op0=ALU.mult, op1=ALU.add), reads=[r_yB[xi], r_wAB, r_xr[xi]], writes=[r_xr[xi]])
                for hh in range(2):
                    op("dve", lambda e, hh=hh, xi=xi, stats=stats: e.bn_stats(out=stats[:, hh, :], in_=xr[xi][:, hh * 512:(hh + 1) * 512]),
                       reads=[r_xr[xi]], writes=[r_stats])
                op("dve", lambda e, stats=stats, mv=mv: e.bn_aggr(out=mv[:], in_=stats[:].rearrange("p a b -> p (a b)")), reads=[r_stats], writes=[r_mv])
                op("act", lambda e, mv=mv, rstd=rstd: e.activation(out=rstd[:], in_=mv[:, 1:2], func=AF.Sqrt, bias=EPS, scale=1.0), reads=[r_mv], writes=[r_rstd])

            def combine_B(ti):
                xi, stats, r_stats, mv, r_mv, rstd, r_rstd = cvars(ti)
                op("dve", lambda e, rstd=rstd: e.reciprocal(out=rstd[:], in_=rstd[:]), reads=[r_rstd], writes=[r_rstd])
                op("dve", lambda e, mv=mv, rstd=rstd: e.tensor_scalar(out=mv[:, 1:2], in0=mv[:, 0:1], scalar1=rstd[:, 0:1], scalar2=-1.0,
                                                                     op0=ALU.mult, op1=ALU.mult), reads=[r_mv, r_rstd], writes=[r_mv])
                op("act", lambda e, xi=xi, mv=mv, rstd=rstd: e.activation(out=xr[xi][:], in_=xr[xi][:], func=AF.Identity, bias=mv[:, 1:2], scale=rstd[:, 0:1]),
                   reads=[r_xr[xi], r_mv, r_rstd], writes=[r_xr[xi]])

            def combine_C(ti):
                xi, stats, r_stats, mv, r_mv, rstd, r_rstd = cvars(ti)
                op("dve", lambda e, xi=xi: e.tensor_tensor(out=xr[xi][:], in0=xr[xi][:], in1=ln_t[:, 0, :], op=ALU.mult),
                   reads=[r_xr[xi], r_ln], writes=[r_xr[xi]])
                op("dve", lambda e, xi=xi: e.tensor_tensor(out=xr[xi][:], in0=xr[xi][:], in1=ln_t[:, 1, :], op=ALU.add),
                   reads=[r_xr[xi], r_ln], writes=[r_xr[xi]])
                dma("sp", lambda e, xi=xi, ti=ti: e.dma_start(out=out_d[ti * 128:(ti + 1) * 128, :], in_=xr[xi][:]),
                    reads=[r_xr[xi]], writes=[r_out])

            for ti in range(min(2, NTILE)):
                combine_fetch(ti)
            for step in range(NTILE + 2):
                if 0 <= step - 2 < NTILE:
                    combine_C(step - 2)
                if 0 <= step - 1 < NTILE:
                    combine_B(step - 1)
                if step < NTILE:
                    combine_A(step)
                if step + 2 < NTILE:
                    combine_fetch(step + 2)
            S_.barrier()
            with nc.Block() as blk:
                S_.emit(blk)
    return nc


def host_inputs(x2, w_in, b_in, attn_sinks, rel_bias_table, w_branch_swa, w_branch_moba, w_out, ln1_gain, ln1_bias,
                w_group_router, b_group_router, w_expert_router, b_expert_router, w_expert_gate, w_expert_up,
                w_expert_down, ln2_gain, ln2_bias):
    f = lambda a: np.ascontiguousarray(np.asarray(a, dtype=np.float32))
    w = np.asarray(w_in)[0]
    b = np.asarray(b_in)[0]
    perm = np.arange(4352)
    qperm = []
    for c in range(4):
        qperm += list(range(c * 64, c * 64 + 64)) + list(range((4 + c) * 64, (4 + c) * 64 + 64))
    perm[0:512] = np.array(qperm)
    wp = w[:, perm]
    bp = b[perm]
    qk_cols = list(range(0, 640)) + list(range(768, 1792))
    bqk = bp[qk_cols].reshape(13, 128).T
    bgt = bp[2304:4352].reshape(16, 128).T
    bvv = np.concatenate([bp[640:768], bp[1792:2304]])[None, :]
    rel = np.asarray(rel_bias_table)
    k = np.arange(128)[:, None]
    q = np.arange(128)[None, :]
    d_own = rel_bucket_np(q - k)
    d_prev = rel_bucket_np(q + 128 - k)
    ga = np.stack([rel[d_own][:, :, :8].transpose(0, 2, 1), rel[d_prev][:, :, :8].transpose(0, 2, 1)], axis=1)
    j = np.arange(1024)[None, :]
    gbk = rel_bucket_np(j - k - 384)
    gb = rel[gbk][:, :, 8:].transpose(0, 2, 1)
    common = {
        "w_in": f(wp), "bqk": f(bqk), "bg": f(bgt), "bv": f(bvv),
        "sinks": f(np.asarray(attn_sinks)[0][None, :]), "t31": f(rel[31:32, 8:16]),
        "ga_raw": f(ga.reshape(128, -1)), "gb_raw": f(gb.reshape(128, -1)),
        "wa": f(np.asarray(w_branch_swa)[0]), "wb": f(np.asarray(w_branch_moba)[0]), "wo": f(np.asarray(w_out)[0]),
        "ln": f(np.stack([np.asarray(ln1_gain)[0], np.asarray(ln1_bias)[0], np.asarray(ln2_gain)[0], np.asarray(ln2_bias)[0]])),
        "wr": f(np.concatenate([np.asarray(w_group_router)[0], np.asarray(w_expert_router)[0]], axis=1)),
        "br": f(np.concatenate([np.asarray(b_group_router)[0], np.asarray(b_expert_router)[0]])[None, :]),
        "wg": f(np.asarray(w_expert_gate)[0]), "wu": f(np.asarray(w_expert_up)[0]), "wd": f(np.asarray(w_expert_down)[0]),
    }
    return common


def run(x, ncores, nseq, S, **params):
    x = np.asarray(x, dtype=np.float32)
    common = host_inputs(x, **params)
    nc = build(nseq, S)
    in_maps = []
    for i in range(ncores):
        xs = x[i * nseq:(i + 1) * nseq].reshape(nseq * S, D)
        m = dict(common)
        m["x_tok"] = np.ascontiguousarray(xs)
        m["x_T"] = np.ascontiguousarray(xs.T)
        in_maps.append(m)
    res = run_bass_kernel_spmd(nc, in_maps, core_ids=list(range(ncores)))
    outs = [np.asarray(r["out"]).reshape(nseq, S, D) for r in res.results]
    return np.concatenate(outs, axis=0).astype(np.float32)


def kernel(x, **params):
    B, S, _ = np.asarray(x).shape
    return run(x, NCORES, B // NCORES, S, **params)
```

```python
from contextlib import ExitStack
import os


class StopBuild(Exception):
    pass


def chk(n):
    if int(os.environ.get('KSTOP', '99')) == n:
        raise StopBuild()

import numpy as np
import concourse.bass as bass
import concourse.mybir as mybir
from concourse.bass_utils import run_bass_kernel_spmd

F32 = mybir.dt.float32
BF16 = mybir.dt.bfloat16
ALU = mybir.AluOpType
AF = mybir.ActivationFunctionType
AX = mybir.AxisListType

D = 1024
NCORES = 8
BIG = 30000.0
ALPHA = 2.0 ** 0.25
EPS = 1e-5
NEXP = 32
DE = 512


class Res:
    __slots__ = ("name", "w", "r", "dsem", "dcount", "excl")

    def __init__(self, name, excl=False):
        self.name = name
        self.w = None
        self.r = {}
        self.dsem = None
        self.dcount = 0
        self.excl = excl


class Sched:
    ENGS = ("pe", "act", "dve", "pool", "sp")

    def __init__(self, nc, stack):
        self.nc = nc
        self.ops = {e: [] for e in self.ENGS}
        self.cnt = {e: 0 for e in self.ENGS}
        self.seen = {e: {} for e in self.ENGS}
        self.sems = {}
        self.dma_keys = {}
        self._stack = stack

    def _sem(self, key):
        s = self.sems.get(key)
        if s is None:
            s = self._stack.enter_context(self.nc.semaphore("s_" + str(key)))
            self.sems[key] = s
        return s

    def _collect(self, eng, reads, writes):
        need = {}

        def add(tok):
            if tok is None:
                return
            k, v = tok
            if k == eng and eng == "pe":
                return
            if need.get(k, 0) < v:
                need[k] = v
        for r in reads:
            add(r.w)
            if r.excl:
                for k, v in r.r.items():
                    add((k, v))
        for w in writes:
            add(w.w)
            for k, v in w.r.items():
                add((k, v))
        waits = []
        seen = self.seen[eng]
        for k, v in need.items():
            if seen.get(k, 0) < v:
                seen[k] = v
                waits.append((k, v))
        return waits

    def _update(self, tok, reads, writes):
        for r in reads:
            if r.excl:
                r.w = tok
                r.r = {}
            elif r.r.get(tok[0], 0) < tok[1]:
                r.r[tok[0]] = tok[1]
        for w in writes:
            w.w = tok
            w.r = {}

    def op(self, eng, fn, reads=(), writes=()):
        waits = self._collect(eng, reads, writes)
        self.cnt[eng] += 1
        tok = (eng, self.cnt[eng])
        self._sem(eng)
        for k, _ in waits:
            self._sem(k)
        self.ops[eng].append((waits, fn, (eng, 1)))
        self._update(tok, reads, writes)
        return tok

    def raw(self, eng, fn, reads=()):
        waits = self._collect(eng, reads, ())
        for k, _ in waits:
            self._sem(k)
        self.ops[eng].append((waits, fn, "raw"))

    def dma(self, q, fn, reads=(), writes=(), sem_res=None):
        waits = self._collect(q, reads, writes)
        sr = sem_res if sem_res is not None else writes[0]
        if sr.dsem is None:
            sr.dsem = "d_" + sr.name
        sr.dcount += 16
        self.dma_keys[sr.dsem] = sr.dcount
        tok = (sr.dsem, sr.dcount)
        self._sem(sr.dsem)
        for k, _ in waits:
            self._sem(k)
        self.ops[q].append((waits, fn, (sr.dsem, 16)))
        self._update(tok, reads, writes)
        return tok

    def barrier(self):
        for e in self.ENGS:
            waits = []
            seen = self.seen[e]
            for k in self.ENGS:
                v = self.cnt[k]
                if k != e and v > 0 and seen.get(k, 0) < v:
                    seen[k] = v
                    waits.append((k, v))
            for k, v in self.dma_keys.items():
                if seen.get(k, 0) < v:
                    seen[k] = v
                    waits.append((k, v))
            self.ops[e].append((waits, None, None))

    def emit(self, block):
        emap = {"pe": block.tensor, "act": block.scalar, "dve": block.vector,
                "pool": block.gpsimd, "sp": block.sync}
        for ename in self.ENGS:
            ops = self.ops[ename]
            if not ops:
                continue

            def body(e, ops=ops):
                for waits, fn, inc in ops:
                    for k, v in waits:
                        e.wait_ge(self.sems[k], v)
                    if fn is None:
                        continue
                    if inc == "raw":
                        fn(e)
                    else:
                        fn(e).then_inc(self.sems[inc[0]], inc[1])
            emap[ename](body)
            self.ops[ename] = []


def rel_bucket_np(dist):
    n = np.maximum(dist, 0)
    nf = np.maximum(n, 1).astype(np.float32)
    large = 16 + (np.log(nf / np.float32(16)) / np.float32(np.log(8.0)) * 16).astype(np.int32)
    large = np.minimum(large, 31)
    return np.where(n < 16, n, large)


def build(NSEQ, S):
    NT = NSEQ * S
    NCH = S // 512
    NTT = S // 128
    nc = bass.Bass("TRN2", target_bir_lowering=False)
    dt = lambda name, shape, ty, kind="ExternalInput": nc.dram_tensor(name, shape, ty, kind=kind).ap()
    x_tok = dt("x_tok", [NT, D], F32)
    x_T = dt("x_T", [D, NT], F32)
    w_in = dt("w_in", [D, 4352], F32)
    bqk = dt("bqk", [128, 13], F32)
    bg = dt("bg", [128, 16], F32)
    bv = dt("bv", [1, 640], F32)
    sinks = dt("sinks", [1, 8], F32)
    t31 = dt("t31", [1, 8], F32)
    ga_raw = dt("ga_raw", [128, 2 * 8 * 128], F32)
    gb_raw = dt("gb_raw", [128, 8 * 1024], F32)
    wa_d = dt("wa", [512, D], F32)
    wb_d = dt("wb", [512, D], F32)
    wo_d = dt("wo", [D, D], F32)
    ln_d = dt("ln", [4, D], F32)
    wr_d = dt("wr", [D, 36], F32)
    br_d = dt("br", [1, 36], F32)
    wg_d = dt("wg", [NEXP, D, DE], F32)
    wu_d = dt("wu", [NEXP, D, DE], F32)
    wd_d = dt("wd", [NEXP, DE, D], F32)
    out_d = dt("out", [NT, D], F32, kind="ExternalOutput")
    x1_d = dt("x1_scr", [NT, D], F32, kind="Internal")
    winb_d = dt("winb_scr", [128, 8 * (4352 + D)], BF16, kind="Internal")
    x1T_d = dt("x1T_scr", [D, NT], BF16, kind="Internal")
    wgb_d = dt("wgb_scr", [NEXP, 128, 8 * DE], BF16, kind="Internal")
    wub_d = dt("wub_scr", [NEXP, 128, 8 * DE], BF16, kind="Internal")
    wdb_d = dt("wdb_scr", [NEXP, 128, 4 * D], BF16, kind="Internal")
    xbuf_d = dt("xbuf_scr", [(2 * NT) // 256 * 256 + NEXP * 256, D], BF16, kind="Internal")
    ybuf_d = dt("ybuf_scr", [(2 * NT) // 256 * 256 + NEXP * 256, D], F32, kind="Internal")

    with ExitStack() as top:
        S_ = Sched(nc, top)
        op, dma = S_.op, S_.dma
        psS = [top.enter_context(nc.psum_tensor(f"psS{i}", [128, 512], F32)) for i in range(4)]
        rS = [Res(f"psS{i}", excl=True) for i in range(4)]
        psO = [top.enter_context(nc.psum_tensor(f"psO{i}", [128, 512], F32)) for i in range(2)]
        rO = [Res(f"psO{i}", excl=True) for i in range(2)]
        psT = [top.enter_context(nc.psum_tensor(f"psT{i}", [128, 1024], BF16)) for i in range(2)]
        rT = [Res(f"psT{i}", excl=True) for i in range(2)]
        ctr = {"S": 0, "O": 0, "T": 0}

        def nxt(kind):
            lst, rl = {"S": (psS, rS), "O": (psO, rO), "T": (psT, rT)}[kind]
            i = ctr[kind] % len(lst)
            ctr[kind] += 1
            return lst[i], rl[i]

        r_x1 = Res("x1_scr")
        r_x1s = [Res("x1s0"), Res("x1s1")]
        r_x1T = Res("x1T_scr")
        r_cw = [Res("cw0"), Res("cw1")]
        r_out = Res("out")

        with ExitStack() as st:
            sb = lambda name, shape, ty: st.enter_context(nc.sbuf_tensor(name, shape, ty))
            ident = sb("ident", [128, 128], BF16); r_ident = Res("ident")
            identf = sb("identf", [128, 128], F32); r_identf = Res("identf")
            op("pool", lambda e: e.memset(identf[:], 0.0), writes=[r_identf])
            op("pool", lambda e: e.affine_select(out=identf[:], in_=identf[:], pattern=[[-1, 128]],
                                                 compare_op=ALU.not_equal, fill=1.0, base=0,
                                                 channel_multiplier=1), reads=[r_identf], writes=[r_identf])
            op("dve", lambda e: e.tensor_copy(out=ident[:], in_=identf[:]), reads=[r_identf], writes=[r_ident])
            bqk_t = sb("bqk_t", [128, 13], F32); r_bqk = Res("bqk")
            bg_t = sb("bg_t", [128, 16], F32); r_bg = Res("bg")
            bv_t = sb("bv_t", [128, 640], F32); r_bv = Res("bv")
            es_t = sb("es_t", [128, 8], F32); r_es = Res("es")
            t31_t = sb("t31_t", [128, 8], F32); r_t31 = Res("t31")
            ln_t = sb("ln_t", [128, 2, D], F32); r_ln = Res("ln")
            dma("sp", lambda e: e.dma_start(out=bqk_t[:], in_=bqk), writes=[r_bqk])
            dma("sp", lambda e: e.dma_start(out=bg_t[:], in_=bg), writes=[r_bg])
            dma("sp", lambda e: e.dma_start(out=bv_t[:], in_=bv.partition_broadcast(128)), writes=[r_bv])
            dma("sp", lambda e: e.dma_start(out=es_t[:], in_=sinks.partition_broadcast(128)), writes=[r_es])
            dma("sp", lambda e: e.dma_start(out=t31_t[:], in_=t31.partition_broadcast(128)), writes=[r_t31])
            for i in range(2):
                dma("sp", lambda e, i=i: e.dma_start(out=ln_t[:, i, :], in_=ln_d[i:i + 1, :].partition_broadcast(128)), writes=[r_ln])
            op("act", lambda e: e.activation(out=es_t[:], in_=es_t[:], func=AF.Exp), reads=[r_es], writes=[r_es])
            op("dve", lambda e: e.tensor_scalar(out=bqk_t[:, 0:4], in0=bqk_t[:, 0:4], scalar1=0.125, scalar2=None, op0=ALU.mult), reads=[r_bqk], writes=[r_bqk])
            op("dve", lambda e: e.tensor_scalar(out=bqk_t[:, 5:9], in0=bqk_t[:, 5:9], scalar1=0.125, scalar2=None, op0=ALU.mult), reads=[r_bqk], writes=[r_bqk])
            PMr = sb("PMr", [128, 8, 32], F32); r_PMr = Res("PMr")
            OWr = sb("OWr", [128, 8, 32], F32); r_OWr = Res("OWr")
            op("pool", lambda e: e.memset(PMr[:, :, 0:16], 0.0), writes=[r_PMr])
            op("pool", lambda e: e.memset(PMr[:, :, 16:32], -3.0e38), reads=[r_PMr], writes=[r_PMr])
            op("pool", lambda e: e.memset(OWr[:], 0.0), writes=[r_OWr])
            op("pool", lambda e: e.memset(OWr[:, :, 16:17], 1.0), reads=[r_OWr], writes=[r_OWr])
            Gs = sb("Gs", [128, 2, 8, 128], BF16); r_Gs = Res("Gs")
            dma("pool", lambda e: e.dma_start(out=Gs[:].rearrange("p a h q -> p (a h q)"), in_=ga_raw), writes=[r_Gs])
            op("pool", lambda e: e.affine_select(out=Gs[:, 0, :, :], in_=Gs[:, 0, :, :], pattern=[[0, 8], [1, 128]],
                                                 compare_op=ALU.is_ge, fill=-BIG, base=0, channel_multiplier=-1),
               reads=[r_Gs], writes=[r_Gs])
            op("pool", lambda e: e.affine_select(out=Gs[:, 1, :, :], in_=Gs[:, 1, :, :], pattern=[[0, 8], [-1, 128]],
                                                 compare_op=ALU.is_gt, fill=-BIG, base=0, channel_multiplier=1),
               reads=[r_Gs], writes=[r_Gs])
            Gb = sb("Gb", [128, 8, 640], BF16); r_Gb = Res("Gb")
            xres = [sb(f"xres{i}", [128, D], F32) for i in range(2)]
            r_xres = [Res(f"xres{i}") for i in range(2)]
            gtmp = xres[0]; r_gtmp = r_xres[0]
            for h in range(8):
                dma("sp", lambda e, h=h: e.dma_start(out=gtmp[:, 0:640], in_=gb_raw[:, h * 1024:h * 1024 + 640]), writes=[r_gtmp])
                op("dve", lambda e, h=h: e.tensor_scalar(out=Gb[:, h, :], in0=gtmp[:, 0:640], scalar1=t31_t[:, h:h + 1], scalar2=None,
                                                         op0=ALU.subtract), reads=[r_gtmp, r_t31], writes=[r_Gb])
            op("pool", lambda e: e.affine_select(out=Gb[:], in_=Gb[:], pattern=[[0, 8], [1, 640]],
                                                 compare_op=ALU.is_ge, fill=-BIG, base=-384, channel_multiplier=-1),
               reads=[r_Gb], writes=[r_Gb])
            wa = sb("wa_t", [128, 4, D], BF16); r_wa = Res("wa")
            wb = sb("wb_t", [128, 4, D], BF16); r_wb = Res("wb")
            dma("pool", lambda e: e.dma_start(out=wa[:], in_=wa_d.rearrange("(k p) n -> p k n", p=128)), writes=[r_wa])
            dma("pool", lambda e: e.dma_start(out=wb[:], in_=wb_d.rearrange("(k p) n -> p k n", p=128)), writes=[r_wb])
            KmT = sb("KmT", [128, 4, S], BF16); r_Km = [Res(f"Km{c}") for c in range(NCH)]
            KsT = sb("KsT", [128, 1024], BF16); r_Ks = [Res(f"Ks{c}") for c in range(2)]
            Vm = sb("Vm", [128, NTT, 8, 65], BF16); r_Vm = [Res(f"Vm{c}") for c in range(NCH)]
            Vs = sb("Vs", [128, 8, 2, 65], BF16); r_Vs = [Res(f"Vs{c}") for c in range(2)]
            kmT = sb("kmT", [128, 4, 16], BF16); r_km = Res("kmT")
            kms = sb("kms", [128, 4, 2], F32); r_kms = Res("kms")
            op("pool", lambda e: e.memset(Vm[:, :, :, 64:65], 1.0), writes=r_Vm)
            op("pool", lambda e: e.memset(Vs[:, :, :, 64:65], 1.0), writes=r_Vs)
            op("pool", lambda e: e.memset(kmT[:], 0.0), writes=[r_km])
            zero_q = True
            wbuf = [sb(f"wbuf{i}", [128, 8 * 768], BF16) for i in range(2)]
            wview = lambda i, n: wbuf[i][:, 0:8 * n].rearrange("p (k n) -> p k n", k=8)
            r_wbuf = [Res(f"wbuf{i}") for i in range(2)]
            wctr = [0]
            cbuf = [sb(f"cbuf{i}", [128, 1024], BF16) for i in range(2)]
            r_cbuf = [Res(f"cbuf{i}") for i in range(2)]
            conv_list = []
            for ex in range(NEXP):
                for (src, dst, K_) in ((wg_d, wgb_d, 8), (wu_d, wub_d, 8), (wd_d, wdb_d, 4)):
                    for half in range(4):
                        conv_list.append((src, dst, K_, ex, half))
            conv_state = {"next": 0, "pending": None}
            n_slots = NSEQ * NCH * 8
            conv_per_slot = -(-len(conv_list) // n_slots)

            def conv_flush():
                p_ = conv_state["pending"]
                if p_ is not None:
                    i, dst, ex, half = p_
                    dma("sp", lambda e, i=i, dst=dst, ex=ex, half=half: e.dma_start(out=dst[ex][:, half * 1024:(half + 1) * 1024], in_=cbuf[i][:]),
                        reads=[r_cbuf[i]], writes=[], sem_res=r_cw[i])
                    conv_state["pending"] = None

            def conv_step():
                for _ in range(conv_per_slot):
                    conv_flush()
                    n_ = conv_state["next"]
                    if n_ >= len(conv_list):
                        return
                    src, dst, K_, ex, half = conv_list[n_]
                    conv_state["next"] = n_ + 1
                    i = n_ % 2
                    dma("pool", lambda e, i=i, src=src, ex=ex, K_=K_, half=half: e.dma_start(
                        out=cbuf[i][:].rearrange("p (k n) -> p k n", k=K_ // 4),
                        in_=src[ex].rearrange("(k p) n -> p k n", p=128)[:, half * (K_ // 4):(half + 1) * (K_ // 4), :]),
                        writes=[r_cbuf[i]])
                    conv_state["pending"] = (i, dst, ex, half)
            xTc = [sb(f"xTc{i}", [128, 8, 512], BF16) for i in range(2)]
            r_xTc = [Res(f"xTc{i}") for i in range(2)]
            QsT = sb("QsT", [128, 4, 512], BF16); r_Qs = Res("QsT")
            QmT = sb("QmTz", [128, 8, 512], BF16); r_Qm = Res("QmT")
            op("pool", lambda e: e.memset(QmT[:], 0.0), writes=[r_Qm])
            gm = sb("gm", [128, 8, 16], F32); r_gm = Res("gm")
            mx = sb("mx", [128, 8, 8], F32); r_mx = Res("mx")
            thr = sb("thr", [128, 8], F32); r_thr = Res("thr")
            sel = sb("sel", [128, 8, 16], F32); r_sel = Res("sel")
            madd4 = [sb(f"madd{i}", [128, 128], BF16) for i in range(4)]; r_madd4 = [Res(f"madd{i}") for i in range(4)]
            maddT = sb("maddT", [128, 512], BF16); r_maddT = Res("maddT")
            PT = [sb(f"PT{i}", [128, 512], BF16) for i in range(3)]
            r_PT = [Res(f"PT{i}") for i in range(3)]
            pctr = [0]
            rden = sb("rden", [128, 4], F32); r_rden = Res("rden")
            ytok = sb("ytok", [128, 4, D], BF16); r_ytok = Res("ytok")
            yT = sb("yT", [128, 8, 512], BF16); r_yT = Res("yT")
            mT = ytok[:].rearrange("p a (b c) -> p (a b) c", c=512); r_mT = r_ytok
            g1 = sb("g1", [128, 512], F32); r_g1 = Res("g1")
            g2 = sb("g2", [128, 512], F32); r_g2 = Res("g2")
            t1 = g1; r_t1 = r_g1
            t2 = g2; r_t2 = r_g2
            z = xres
            r_z = r_xres
            stats_l = [sb(f"stats{i}", [128, 2, 6], F32) for i in range(2)]; r_stats_l = [Res(f"stats{i}") for i in range(2)]
            mv_l = [sb(f"mv{i}", [128, 2], F32) for i in range(2)]; r_mv_l = [Res(f"mv{i}") for i in range(2)]
            rstd_l = [sb(f"rstd{i}", [128, 1], F32) for i in range(2)]; r_rstd_l = [Res(f"rstd{i}") for i in range(2)]

            def load_w(c0, ncols, dst0=0, new=True, conv=True):
                if new:
                    wctr[0] += 1
                i = wctr[0] % len(wbuf)
                dma("sp", lambda e: e.dma_start(out=wbuf[i][:, 0:8 * ncols], in_=winb_d[:, 8 * c0:8 * c0 + 8 * ncols]),
                    writes=[r_wbuf[i]])
                return wview(i, ncols), r_wbuf[i]

            def layer_norm(zt, r_zt, gi):
                stats, r_stats, mv, r_mv, rstd, r_rstd = stats_l[gi], r_stats_l[gi], mv_l[gi], r_mv_l[gi], rstd_l[gi], r_rstd_l[gi]
                for hh in range(2):
                    op("dve", lambda e, hh=hh: e.bn_stats(out=stats[:, hh, :], in_=zt[:, hh * 512:(hh + 1) * 512]),
                       reads=[r_zt], writes=[r_stats])
                op("dve", lambda e: e.bn_aggr(out=mv[:], in_=stats[:].rearrange("p a b -> p (a b)")), reads=[r_stats], writes=[r_mv])
                op("act", lambda e: e.activation(out=rstd[:], in_=mv[:, 1:2], func=AF.Sqrt, bias=EPS, scale=1.0),
                   reads=[r_mv], writes=[r_rstd])
                op("dve", lambda e: e.reciprocal(out=rstd[:], in_=rstd[:]), reads=[r_rstd], writes=[r_rstd])
                op("dve", lambda e: e.tensor_scalar(out=mv[:, 1:2], in0=mv[:, 0:1], scalar1=rstd[:, 0:1], scalar2=-1.0,
                                                    op0=ALU.mult, op1=ALU.mult), reads=[r_mv, r_rstd], writes=[r_mv])
                op("act", lambda e: e.activation(out=zt[:], in_=zt[:], func=AF.Identity, bias=mv[:, 1:2], scale=rstd[:, 0:1]),
                   reads=[r_zt, r_mv, r_rstd], writes=[r_zt])
                return gi

            r_winb = Res("winb")
            groups = [([(w_in, 0, 768, 0)], 768, 0), ([(w_in, 768, 512, 0)], 512, 768), ([(w_in, 1280, 512, 0)], 512, 1280),
                      ([(w_in, 1792, 512, 0)], 512, 1792)]
            for jp in range(4):
                groups.append(([(w_in, 2304 + jp * 256, 256, 0), (w_in, 3328 + jp * 256, 256, 256)], 512, 2304 + jp * 512))
            groups += [([(wo_d, 0, 512, 0)], 512, 4352), ([(wo_d, 512, 512, 0)], 512, 4864)]
            for gi_, (pieces, n_, d0_) in enumerate(groups):
                i_ = gi_ % 2
                for (src_, c0_, pn_, po_) in pieces:
                    dma("pool", lambda e, i_=i_, src_=src_, c0_=c0_, pn_=pn_, po_=po_, n_=n_: e.dma_start(
                        out=wview(i_, n_)[:, :, po_:po_ + pn_], in_=src_[:, c0_:c0_ + pn_].rearrange("(k p) n -> p k n", p=128)), writes=[r_wbuf[i_]])
                dma("sp", lambda e, i_=i_, n_=n_, d0_=d0_: e.dma_start(out=winb_d[:, 8 * d0_:8 * d0_ + 8 * n_], in_=wbuf[i_][:, 0:8 * n_]),
                    reads=[r_wbuf[i_]], writes=[], sem_res=r_winb)
            S_.barrier()
            pending_wout = []
            def wout_section(T0):
                woh = [load_w(4352, 512, conv=False), load_w(4864, 512, conv=False)]
                for tt in range(4):
                    zi = tt % 2
                    dma("sp", lambda e, zi=zi, tt=tt, T0=T0: e.dma_start(out=xres[zi][:], in_=x_tok[T0 + tt * 128:T0 + (tt + 1) * 128, :]),
                        writes=[r_xres[zi]])
                    for hh in range(2):
                        ps, rp = nxt("S")
                        for k in range(8):
                            op("pe", lambda e, ps=ps, k=k, tt=tt, hh=hh, woh=woh: e.matmul(ps[:], lhsT=mT[:, k, tt * 128:(tt + 1) * 128],
                                                                                  rhs=woh[hh][0][:, k, 0:512], start=(k == 0), stop=(k == 7)),
                               reads=[r_mT, woh[hh][1]], writes=[rp])
                        op("dve", lambda e, ps=ps, zi=zi, hh=hh: e.scalar_tensor_tensor(
                            out=z[zi][:, hh * 512:(hh + 1) * 512], in0=xres[zi][:, hh * 512:(hh + 1) * 512], scalar=ALPHA, in1=ps[:],
                            op0=ALU.mult, op1=ALU.add), reads=[rp, r_xres[zi]], writes=[r_xres[zi]])
                    layer_norm(z[zi], r_z[zi], zi)
                    op("pool", lambda e, zi=zi: e.tensor_tensor(out=z[zi][:], in0=z[zi][:], in1=ln_t[:, 0, :], op=ALU.mult),
                       reads=[r_z[zi], r_ln], writes=[r_z[zi]])
                    op("pool", lambda e, zi=zi: e.tensor_tensor(out=z[zi][:], in0=z[zi][:], in1=ln_t[:, 1, :], op=ALU.add),
                       reads=[r_z[zi], r_ln], writes=[r_z[zi]])
                    dma("pool", lambda e, zi=zi, tt=tt, T0=T0: e.dma_start(out=x1_d[T0 + tt * 128:T0 + (tt + 1) * 128, :], in_=z[zi][:]),
                        reads=[r_z[zi]], writes=[], sem_res=r_x1s[zi])

            try:
              chk(1)
              for s in range(NSEQ):
                for c in range(NCH):
                    T0 = s * S + c * 512
                    gidx = s * NCH + c
                    xi = gidx % 2
                    xt, r_xt = xTc[xi], r_xTc[xi]

                    def load_xT(g_):
                        b_ = xTc[g_ % 2]
                        t0_ = g_ * 512
                        dma("pool", lambda e: e.dma_start(out=b_[:], in_=x_T[:, t0_:t0_ + 512].rearrange("(k p) n -> p k n", p=128)),
                            writes=[r_xTc[g_ % 2]])

                    if gidx == 0:
                        load_xT(0)
                    wA, r_wA = load_w(0, 768, conv=False)
                    for m in range(5):
                        ps, rp = nxt("S")
                        for k in range(8):
                            op("pe", lambda e, ps=ps, k=k, m=m, wA=wA, xt=xt: e.matmul(
                                ps[:], lhsT=wA[:, k, m * 128:(m + 1) * 128], rhs=xt[:, k, :], start=(k == 0), stop=(k == 7)),
                               reads=[r_wA, r_xt], writes=[rp])
                        if m < 4:
                            op("act", lambda e, ps=ps, m=m: e.activation(out=QsT[:, m, :], in_=ps[:], func=AF.Identity,
                                                                         bias=bqk_t[:, m:m + 1], scale=0.125),
                               reads=[rp, r_bqk], writes=[r_Qs])
                        else:
                            op("act", lambda e, ps=ps, c=c: e.activation(out=KsT[:, (c % 2) * 512:(c % 2 + 1) * 512], in_=ps[:], func=AF.Identity,
                                                                         bias=bqk_t[:, 4:5], scale=1.0),
                               reads=[rp, r_bqk], writes=[r_Ks[c % 2]])
                    for tt in range(4):
                        ps, rp = nxt("S")
                        for k in range(8):
                            op("pe", lambda e, ps=ps, k=k, tt=tt, wA=wA, xt=xt: e.matmul(
                                ps[:, 0:128], lhsT=xt[:, k, tt * 128:(tt + 1) * 128], rhs=wA[:, k, 640:768], start=(k == 0), stop=(k == 7)),
                               reads=[r_wA, r_xt], writes=[rp])
                        op("dve", lambda e, ps=ps, tt=tt, c=c: e.tensor_tensor(
                            out=Vs[:, (c % 2) * 4 + tt, :, 0:64], in0=ps[:, 0:128].rearrange("p (g d) -> p g d", g=2),
                            in1=bv_t[:, 0:128].rearrange("p (g d) -> p g d", g=2), op=ALU.add),
                           reads=[rp, r_bv], writes=[r_Vs[c % 2]])
                    wC, r_wC = load_w(768, 512, conv=False)
                    for m in range(4):
                        ps, rp = nxt("S")
                        for k in range(8):
                            op("pe", lambda e, ps=ps, k=k, m=m, wC=wC, xt=xt: e.matmul(
                                ps[:], lhsT=wC[:, k, m * 128:(m + 1) * 128], rhs=xt[:, k, :], start=(k == 0), stop=(k == 7)),
                               reads=[r_wC, r_xt], writes=[rp])
                        op("act", lambda e, ps=ps, m=m: e.activation(out=QmT[0:64, 2 * m, :], in_=ps[0:64, :], func=AF.Identity,
                                                                     bias=bqk_t[0:64, 5 + m:6 + m], scale=0.125),
                           reads=[rp, r_bqk], writes=[r_Qm])
                        op("act", lambda e, ps=ps, m=m: e.activation(out=QmT[64:128, 2 * m + 1, :], in_=ps[64:128, :], func=AF.Identity,
                                                                     bias=bqk_t[64:128, 5 + m:6 + m], scale=0.125),
                           reads=[rp, r_bqk], writes=[r_Qm])
                    wD, r_wD = load_w(1280, 512)
                    for m in range(4):
                        ps, rp = nxt("S")
                        for k in range(8):
                            op("pe", lambda e, ps=ps, k=k, m=m, wD=wD, xt=xt: e.matmul(
                                ps[:], lhsT=wD[:, k, m * 128:(m + 1) * 128], rhs=xt[:, k, :], start=(k == 0), stop=(k == 7)),
                               reads=[r_wD, r_xt], writes=[rp])
                        op("act", lambda e, ps=ps, m=m, c=c: e.activation(out=KmT[:, m, c * 512:(c + 1) * 512], in_=ps[:], func=AF.Identity,
                                                                          bias=bqk_t[:, 9 + m:10 + m], scale=1.0),
                           reads=[rp, r_bqk], writes=[r_Km[c]])
                    wE, r_wE = load_w(1792, 512)
                    for tt in range(4):
                        ps, rp = nxt("S")
                        for k in range(8):
                            op("pe", lambda e, ps=ps, k=k, tt=tt, wE=wE, xt=xt: e.matmul(
                                ps[:], lhsT=xt[:, k, tt * 128:(tt + 1) * 128], rhs=wE[:, k, 0:512], start=(k == 0), stop=(k == 7)),
                               reads=[r_wE, r_xt], writes=[rp])
                        op("dve", lambda e, ps=ps, tt=tt, c=c: e.tensor_tensor(
                            out=Vm[:, c * 4 + tt, :, 0:64], in0=ps[:].rearrange("p (g d) -> p g d", g=8),
                            in1=bv_t[:, 128:640].rearrange("p (g d) -> p g d", g=8), op=ALU.add),
                           reads=[rp, r_bv], writes=[r_Vm[c]])
                    chk(2)
                    op("dve", lambda e, c=c: e.tensor_reduce(out=kms[:], in_=KmT[:, :, c * 512:(c + 1) * 512].rearrange("p m (b t) -> p m b t", b=2),
                                                             axis=AX.X, op=ALU.add), reads=[r_Km[c]], writes=[r_kms])
                    op("dve", lambda e, c=c: e.tensor_scalar(out=kmT[:, :, 2 * c:2 * c + 2], in0=kms[:], scalar1=1.0 / 256.0, scalar2=None,
                                                             op0=ALU.mult), reads=[r_kms], writes=[r_km])
                    while pending_wout:
                        wout_section(pending_wout.pop(0))
                    if gidx + 1 < NSEQ * NCH:
                        load_xT(gidx + 1)
                    chk(3)
                    for tt in range(4):
                        qb = 2 * c + tt // 2
                        pse, rpe = nxt("S")
                        for h in range(8):
                            op("pe", lambda e, pse=pse, h=h, tt=tt: e.matmul(
                                pse[:, h * 16:(h + 1) * 16], lhsT=QmT[:, h, tt * 128:(tt + 1) * 128],
                                rhs=kmT[:, h // 2, :], start=True, stop=True),
                               reads=[r_Qm, r_km], writes=[rpe])
                        chk(31)
                        op("dve", lambda e, pse=pse, qb=qb: e.tensor_tensor(
                            out=gm[:], in0=pse[:, 0:128].rearrange("p (h n) -> p h n", h=8), in1=PMr[:, :, 16 - qb:32 - qb], op=ALU.add),
                           reads=[rpe, r_PMr], writes=[r_gm])
                        chk(32)
                        for h in range(8):
                            op("dve", lambda e, h=h: e.max(out=mx[:, h, :], in_=gm[:, h, :]), reads=[r_gm], writes=[r_mx])
                        chk(33)
                        op("dve", lambda e: e.tensor_scalar(out=thr[:], in0=mx[:, :, 2], scalar1=-1.0e30, scalar2=None, op0=ALU.max),
                           reads=[r_mx], writes=[r_thr])
                        chk(34)
                        op("dve", lambda e: e.tensor_tensor(out=sel[:], in0=gm[:], in1=thr[:].unsqueeze(2).to_broadcast([128, 8, 16]), op=ALU.is_ge),
                           reads=[r_gm, r_thr], writes=[r_sel])
                        op("dve", lambda e, qb=qb: e.tensor_tensor(out=sel[:], in0=sel[:], in1=OWr[:, :, 16 - qb:32 - qb], op=ALU.add),
                           reads=[r_sel, r_OWr], writes=[r_sel])
                        op("dve", lambda e: e.tensor_scalar(out=sel[:], in0=sel[:], scalar1=-1.0, scalar2=BIG, op0=ALU.add, op1=ALU.mult),
                           reads=[r_sel], writes=[r_sel])
                        op("dve", lambda e, tt=tt: e.tensor_tensor(out=madd4[tt][:].rearrange("p (h n) -> p h n", h=8), in0=sel[:],
                                                                   in1=t31_t[:].unsqueeze(2).to_broadcast([128, 8, 16]), op=ALU.add),
                           reads=[r_sel, r_t31], writes=[r_madd4[tt]])
                    chk(5)
                    items = []
                    for tt in range(4):
                        b = c * 4 + tt
                        for g in range(2):
                            whichs = [0] if b == 0 else [0, 1]
                            for wi, which in enumerate(whichs):
                                items.append((tt, g, wi, which, len(whichs), b))
                    obank = {}

                    def swa_S(it):
                        tt, g, wi, which, nw, b = it
                        if wi == 0:
                            obank[(tt, g)] = nxt("O")
                        kt = b - which
                        kc0 = ((kt // 4) % 2) * 512 + (kt % 4) * 128
                        ps, rp = nxt("S")
                        op("pe", lambda e, ps=ps, g=g, kc0=kc0, tt=tt: e.matmul(
                            ps[:], lhsT=KsT[g * 64:(g + 1) * 64, kc0:kc0 + 128],
                            rhs=QsT[g * 64:(g + 1) * 64, :, tt * 128:(tt + 1) * 128], start=True, stop=False),
                           reads=[r_Ks[(kt // 4) % 2], r_Qs], writes=[rp])
                        op("pe", lambda e, ps=ps, g=g, which=which: e.matmul(
                            ps[:], lhsT=ident[:], rhs=Gs[:, which, g * 4:(g + 1) * 4, :], start=False, stop=True),
                           reads=[r_ident, r_Gs], writes=[rp])
                        pi = pctr[0] % 3
                        pctr[0] += 1
                        op("act", lambda e, ps=ps, pi=pi: e.activation(out=PT[pi][:], in_=ps[:], func=AF.Exp),
                           reads=[rp], writes=[r_PT[pi]])
                        return pi

                    def swa_PV(it, pi):
                        tt, g, wi, which, nw, b = it
                        po, rpo = obank[(tt, g)]
                        kt = b - which
                        for j in range(4):
                            first = (wi == 0 and j == 0)
                            op("pe", lambda e, po=po, pi=pi, j=j, kt=kt, g=g, first=first, wi=wi, nw=nw: e.matmul(
                                po[:, j * 65:(j + 1) * 65], lhsT=PT[pi][:, j * 128:(j + 1) * 128], rhs=Vs[:, kt % 8, g, :],
                                start=first, stop=(wi == nw - 1), skip_group_check=True),
                               reads=[r_PT[pi], r_Vs[(kt // 4) % 2]], writes=[rpo])
                        if wi == nw - 1:
                            pov = po[:, 0:260].rearrange("p (t d) -> p t d", t=4)
                            op("dve", lambda e, pov=pov, g=g: e.tensor_tensor(out=rden[:], in0=pov[:, :, 64], in1=es_t[:, g * 4:(g + 1) * 4], op=ALU.add),
                               reads=[rpo, r_es], writes=[r_rden])
                            op("dve", lambda e: e.reciprocal(out=rden[:], in_=rden[:]), reads=[r_rden], writes=[r_rden])
                            op("dve", lambda e, pov=pov, g=g, tt=tt: e.tensor_tensor(
                                out=ytok[:, tt, g * 256:(g + 1) * 256].rearrange("p (j d) -> p j d", j=4), in0=pov[:, :, 0:64],
                                in1=rden[:].unsqueeze(2).to_broadcast([128, 4, 64]), op=ALU.mult),
                               reads=[rpo, r_rden], writes=[r_ytok])

                    prev = None
                    for it in items:
                        pi = swa_S(it)
                        if prev is not None:
                            swa_PV(*prev)
                        prev = (it, pi)
                    swa_PV(*prev)
                    for tt in range(4):
                        pt, rpt = nxt("T")
                        op("pe", lambda e, pt=pt, tt=tt: e.transpose(pt[:, 0:128], madd4[tt][:], ident[:]), reads=[r_madd4[tt], r_ident], writes=[rpt])
                        op("act", lambda e, pt=pt, tt=tt: e.activation(out=maddT[:, tt * 128:(tt + 1) * 128], in_=pt[:, 0:128], func=AF.Copy),
                           reads=[rpt], writes=[r_maddT])
                    chk(4)
                    nkt = 4 * c + 4
                    for h in range(8):
                        hb = (h % 2) * 64
                        po, rpo = nxt("O")

                        def emit_S(kt, h=h, hb=hb):
                            rel = kt - 4 * c
                            n = kt // 2
                            ps, rp = nxt("S")
                            last = rel < -1
                            op("pe", lambda e, ps=ps, kt=kt: e.matmul(
                                ps[:], lhsT=KmT[:, h // 2, kt * 128:(kt + 1) * 128], rhs=QmT[:, h, :],
                                start=True, stop=False), reads=[r_Km[kt // 4], r_Qm], writes=[rp])
                            p = h * 16 + n
                            op("pe", lambda e, ps=ps, p=p, last=last: e.matmul(
                                ps[:], lhsT=ident[:, p:p + 1].to_broadcast([128, 128]), rhs=maddT[:], start=False, stop=last),
                               reads=[r_ident, r_maddT], writes=[rp])
                            if not last:
                                off = 384 - 128 * rel
                                wid = min(512, 640 - off)
                                op("pe", lambda e, ps=ps, off=off, wid=wid: e.matmul(
                                    ps[:, 0:wid], lhsT=ident[:], rhs=Gb[:, h, off:off + wid], start=False, stop=True, skip_group_check=True),
                                   reads=[r_ident, r_Gb], writes=[rp])
                            pi = pctr[0] % 3
                            pctr[0] += 1
                            op("act", lambda e, ps=ps, pi=pi: e.activation(out=PT[pi][:], in_=ps[:], func=AF.Exp),
                               reads=[rp], writes=[r_PT[pi]])
                            return pi

                        def emit_PV(kt, pi, h=h, po=po, rpo=rpo):
                            for tt in range(4):
                                first = (kt == 0 and tt == 0)
                                op("pe", lambda e, pi=pi, tt=tt, kt=kt, first=first, nkt=nkt: e.matmul(
                                    po[:, tt * 65:(tt + 1) * 65], lhsT=PT[pi][:, tt * 128:(tt + 1) * 128], rhs=Vm[:, kt, h, :],
                                    start=first, stop=(kt == nkt - 1), skip_group_check=True),
                                   reads=[r_PT[pi], r_Vm[kt // 4]], writes=[rpo])

                        conv_step()
                        pend = []
                        for kt in range(nkt):
                            pi = emit_S(kt)
                            pend.append((kt, pi))
                            if len(pend) > 2:
                                emit_PV(*pend.pop(0))
                        while pend:
                            emit_PV(*pend.pop(0))
                        pov = po[:, 0:260].rearrange("p (t d) -> p t d", t=4)
                        op("dve", lambda e, pov=pov: e.reciprocal(out=rden[:], in_=pov[:, :, 64]), reads=[rpo], writes=[r_rden])
                        op("dve", lambda e, pov=pov, h=h: e.tensor_tensor(
                            out=ytok[:, :, 512 + h * 64:512 + (h + 1) * 64], in0=pov[:, :, 0:64],
                            in1=rden[:].unsqueeze(2).to_broadcast([128, 4, 64]), op=ALU.mult),
                           reads=[rpo, r_rden], writes=[r_ytok])
                    chk(6)
                    for j in range(8):
                        pt, rpt = nxt("T")
                        for tt in range(4):
                            op("pe", lambda e, pt=pt, tt=tt, j=j: e.transpose(pt[:, tt * 128:(tt + 1) * 128], ytok[:, tt, j * 128:(j + 1) * 128], ident[:]),
                               reads=[r_ytok, r_ident], writes=[rpt])
                        op("act", lambda e, pt=pt, j=j: e.activation(out=yT[:, j, :], in_=pt[:, 0:512], func=AF.Copy),
                           reads=[rpt], writes=[r_yT])
                    chk(7)
                    for j in range(8):
                        if j % 2 == 0:
                            wF1, r_wF1 = load_w(2304 + (j // 2) * 512, 512)
                            wF2, r_wF2 = wF1, r_wF1
                        jo = (j % 2) * 128
                        pa, rpa = nxt("S")
                        for k in range(4):
                            op("pe", lambda e, pa=pa, k=k, j=j: e.matmul(pa[:], lhsT=wa[:, k, j * 128:(j + 1) * 128], rhs=yT[:, k, :],
                                                                         start=(k == 0), stop=(k == 3)), reads=[r_wa, r_yT], writes=[rpa])
                        pb, rpb = nxt("S")
                        for k in range(4):
                            op("pe", lambda e, pb=pb, k=k, j=j: e.matmul(pb[:], lhsT=wb[:, k, j * 128:(j + 1) * 128], rhs=yT[:, 4 + k, :],
                                                                         start=(k == 0), stop=(k == 3)), reads=[r_wb, r_yT], writes=[rpb])
                        pg1, rpg1 = nxt("S")
                        for k in range(8):
                            op("pe", lambda e, pg1=pg1, k=k, jo=jo, wF1=wF1, xt=xt: e.matmul(pg1[:], lhsT=wF1[:, k, jo:jo + 128], rhs=xt[:, k, :],
                                                                                           start=(k == 0), stop=(k == 7)), reads=[r_wF1, r_xt], writes=[rpg1])
                        op("act", lambda e, pg1=pg1, j=j: e.activation(out=g1[:], in_=pg1[:], func=AF.Sigmoid, bias=bg_t[:, j:j + 1], scale=1.0),
                           reads=[rpg1, r_bg], writes=[r_g1])
                        pg2, rpg2 = nxt("S")
                        for k in range(8):
                            op("pe", lambda e, pg2=pg2, k=k, jo=jo, wF2=wF2, xt=xt: e.matmul(pg2[:], lhsT=wF2[:, k, 256 + jo:256 + jo + 128], rhs=xt[:, k, :],
                                                                                           start=(k == 0), stop=(k == 7)), reads=[r_wF2, r_xt], writes=[rpg2])
                        op("act", lambda e, pg2=pg2, j=j: e.activation(out=g2[:], in_=pg2[:], func=AF.Sigmoid, bias=bg_t[:, 8 + j:9 + j], scale=1.0),
                           reads=[rpg2, r_bg], writes=[r_g2])
                        op("dve", lambda e, pa=pa: e.tensor_tensor(out=t1[:], in0=pa[:], in1=g1[:], op=ALU.mult), reads=[rpa, r_g1], writes=[r_t1])
                        op("dve", lambda e, pb=pb: e.tensor_tensor(out=t2[:], in0=pb[:], in1=g2[:], op=ALU.mult), reads=[rpb, r_g2], writes=[r_t2])
                        op("pool", lambda e, j=j: e.tensor_tensor(out=mT[:, j, :], in0=t1[:], in1=t2[:], op=ALU.add), reads=[r_t1, r_t2], writes=[r_mT])
                    chk(8)
                    pending_wout.append(T0)
            except StopBuild:
                pass
            while pending_wout:
                wout_section(pending_wout.pop(0))
            while conv_state["next"] < len(conv_list) or conv_state["pending"] is not None:
                conv_step()
                conv_flush()
            S_.barrier()
            with nc.Block() as blk:
                S_.emit(blk)
            if int(os.environ.get('KSTOP', '99')) < 20:
                return nc

        with ExitStack() as st:
            sb = lambda name, shape, ty: st.enter_context(nc.sbuf_tensor(name, shape, ty))
            CH = 1024
            NMC = NT // CH
            ident = sb("ident2", [128, 128], BF16); r_ident = Res("ident2")
            identf = sb("identf2", [128, 128], F32); r_identf = Res("identf2")
            op("pool", lambda e: e.memset(identf[:], 0.0), writes=[r_identf])
            op("pool", lambda e: e.affine_select(out=identf[:], in_=identf[:], pattern=[[-1, 128]],
                                                 compare_op=ALU.not_equal, fill=1.0, base=0,
                                                 channel_multiplier=1), reads=[r_identf], writes=[r_identf])
            op("dve", lambda e: e.tensor_copy(out=ident[:], in_=identf[:]), reads=[r_identf], writes=[r_ident])
            ln_t = sb("ln2_t", [128, 2, D], F32); r_ln = Res("ln2")
            for i in range(2):
                dma("sp", lambda e, i=i: e.dma_start(out=ln_t[:, i, :], in_=ln_d[2 + i:3 + i, :].partition_broadcast(128)), writes=[r_ln])
            wr = sb("wr_t", [128, 8, 36], BF16); r_wr = Res("wr")
            br = sb("br_t", [128, 36], F32); r_br = Res("br")
            dma("pool", lambda e: e.dma_start(out=wr[:], in_=wr_d.rearrange("(k p) n -> p k n", p=128)), writes=[r_wr])
            dma("sp", lambda e: e.dma_start(out=br[:], in_=br_d.partition_broadcast(128)), writes=[r_br])
            I32 = mybir.dt.int32
            BLK = 256
            NTILE = NT // 128
            NB = (2 * NT) // BLK + NEXP
            CAP = NB * BLK
            wgb2 = wgb_d.rearrange("e p n -> (e p) n")
            wub2 = wub_d.rearrange("e p n -> (e p) n")
            wdb2 = wdb_d.rearrange("e p n -> (e p) n")
            r_xbuf = Res("xbuf"); r_ybuf = Res("ybuf")
            U = sb("U", [128, 128], BF16); r_U = Res("U")
            Uf = identf
            op("pool", lambda e: e.memset(Uf[:], 1.0), reads=[r_identf], writes=[r_identf])
            op("pool", lambda e: e.affine_select(out=Uf[:], in_=Uf[:], pattern=[[1, 128]], compare_op=ALU.is_gt, fill=0.0,
                                                 base=0, channel_multiplier=-1), reads=[r_identf], writes=[r_identf])
            op("dve", lambda e: e.tensor_copy(out=U[:], in_=Uf[:]), reads=[r_identf], writes=[r_U])
            ones = sb("ones", [128, 128], BF16); r_ones = Res("ones")
            op("pool", lambda e: e.memset(ones[:], 1.0), writes=[r_ones])
            pci = sb("pci", [128, 1], I32); r_pci = Res("pci")
            pcf = sb("pcf", [128, 2], F32); r_pcf = Res("pcf")
            op("pool", lambda e: e.iota(pci[:], pattern=[[0, 1]], base=0, channel_multiplier=1), writes=[r_pci])
            op("dve", lambda e: e.tensor_copy(out=pcf[:, 0:1], in_=pci[:]), reads=[r_pci], writes=[r_pcf])
            op("dve", lambda e: e.tensor_scalar(out=pcf[:, 1:2], in0=pcf[:, 0:1], scalar1=float(BLK), scalar2=None, op0=ALU.mult),
               reads=[r_pcf], writes=[r_pcf])
            bvi = sb("bvi", [128, NB], I32); r_bvi = Res("bvi")
            bvf = sb("bvf", [128, NB], F32); r_bvf = Res("bvf")
            op("pool", lambda e: e.iota(bvi[:], pattern=[[1, NB]], base=0, channel_multiplier=0), writes=[r_bvi])
            op("dve", lambda e: e.tensor_copy(out=bvf[:], in_=bvi[:]), reads=[r_bvi], writes=[r_bvf])
            oh1 = sb("oh1", [128, NTILE, 32], BF16); r_oh1 = Res("oh1")
            oh2 = sb("oh2", [128, NTILE, 32], BF16); r_oh2 = Res("oh2")
            posAB = sb("posAB", [128, NTILE, 2], F32); r_pos = Res("posAB")
            wAB = sb("wAB", [128, NTILE, 2], F32); r_wAB = Res("wAB")
            idxAB = sb("idxAB", [128, NTILE, 2], I32); r_idx = Res("idxAB")
            base = sb("base", [128, 32], F32); r_base = Res("base")
            op("pool", lambda e: e.memset(base[:], 0.0), writes=[r_base])
            hT = [sb(f"hT{i}", [128, 8, 128], BF16) for i in range(2)]
            r_hT = [Res(f"hT{i}") for i in range(2)]
            xbr = [sb(f"xbr{i}", [128, D], BF16) for i in range(2)]
            r_xbr = [Res(f"xbr{i}") for i in range(2)]
            G = min(16, NTILE)
            lgall = sb("lgall", [128, NTILE, 36], F32); r_lgall = [Res(f"lgall{g}") for g in range(NTILE // G)]
            Mball = sb("Mball", [128, NTILE, 32], BF16); r_Mball = [Res(f"Mball{g}") for g in range(NTILE // G)]
            gmx = sb("gmx", [128, G], F32); r_gmx = Res("gmx")
            ohg = sb("ohg", [128, G, 4], F32); r_ohg = Res("ohg")
            gxe = sb("gxe", [128, G, 4], F32); r_gxe = Res("gxe")
            gpr = sb("gpr", [128, G], F32); r_gpr = Res("gpr")
            emb = sb("emb", [128, G, 32], F32); r_emb = Res("emb")
            em2 = sb("em2", [128, G, 32], F32); r_em2 = Res("em2")
            s1b = sb("s1b", [128, G, 32], F32); r_s1b = Res("s1b")
            s2b = sb("s2b", [128, G, 32], F32); r_s2b = Res("s2b")
            m12 = sb("m12", [128, 2, G], F32); r_m12 = Res("m12")
            ww = sb("ww", [128, 3, G], F32); r_ww = Res("ww")
            pos = sb("pos", [128, 32], F32); r_posf = Res("posf")
            tmp = sb("tmp", [128, 32], F32); r_tmp = Res("tmp")
            for ti in range(NTILE):
                xi_ = ti % 2
                h_, r_h = hT[xi_], r_hT[xi_]
                dma("pool", lambda e, ti=ti, xi_=xi_: e.dma_start(out=xbr[xi_][:], in_=x1_d[ti * 128:(ti + 1) * 128, :]), writes=[r_xbr[xi_]])
                pt, rpt = nxt("T")
                for j in range(8):
                    op("pe", lambda e, pt=pt, j=j, xi_=xi_: e.transpose(pt[:, j * 128:(j + 1) * 128], xbr[xi_][:, j * 128:(j + 1) * 128], ident[:]),
                       reads=[r_xbr[xi_], r_ident], writes=[rpt])
                op("act", lambda e, pt=pt, h_=h_: e.activation(out=h_[:], in_=pt[:].rearrange("p (j t) -> p j t", j=8), func=AF.Copy),
                   reads=[rpt], writes=[r_h])
                ps, rp = nxt("S")
                for k in range(8):
                    op("pe", lambda e, ps=ps, k=k, h_=h_: e.matmul(ps[:, 0:36], lhsT=h_[:, k, :], rhs=wr[:, k, :],
                                                                   start=(k == 0), stop=(k == 7)), reads=[r_h, r_wr], writes=[rp])
                op("dve", lambda e, ps=ps, ti=ti: e.tensor_tensor(out=lgall[:, ti, :], in0=ps[:, 0:36], in1=br[:], op=ALU.add),
                   reads=[rp, r_br], writes=[r_lgall[ti // G]])
            for g in range(NTILE // G):
                t0, t1 = g * G, (g + 1) * G
                gl = lgall[:, t0:t1, 0:4]
                el = lgall[:, t0:t1, 4:36].rearrange("p t (g x) -> p t g x", g=4)
                bc3 = lambda ap, n: ap.unsqueeze(2).to_broadcast([128, G, n])
                op("dve", lambda e, gl=gl: e.tensor_reduce(out=gmx[:], in_=gl, axis=AX.X, op=ALU.max), reads=[r_lgall[g]], writes=[r_gmx])
                op("dve", lambda e, gl=gl: e.tensor_tensor(out=ohg[:], in0=gl, in1=bc3(gmx[:], 4), op=ALU.is_ge), reads=[r_lgall[g], r_gmx], writes=[r_ohg])
                op("dve", lambda e, gl=gl: e.tensor_tensor(out=gxe[:], in0=gl, in1=bc3(gmx[:], 4), op=ALU.subtract), reads=[r_lgall[g], r_gmx], writes=[r_gxe])
                op("act", lambda e: e.activation(out=gxe[:], in_=gxe[:], func=AF.Exp), reads=[r_gxe], writes=[r_gxe])
                op("dve", lambda e: e.tensor_scalar(out=ohg[:], in0=ohg[:], scalar1=-1.0, scalar2=1.0e30, op0=ALU.add, op1=ALU.mult), reads=[r_ohg], writes=[r_ohg])
                op("dve", lambda e, el=el: e.tensor_tensor(out=emb[:].rearrange("p t (g x) -> p t g x", g=4), in0=el,
                                                           in1=ohg[:].unsqueeze(3).to_broadcast([128, G, 4, 8]), op=ALU.add),
                   reads=[r_lgall[g], r_ohg], writes=[r_emb])
                op("dve", lambda e: e.tensor_reduce(out=m12[:, 0, :], in_=emb[:], axis=AX.X, op=ALU.max), reads=[r_emb], writes=[r_m12])
                op("dve", lambda e: e.tensor_tensor(out=s1b[:], in0=emb[:], in1=bc3(m12[:, 0, :], 32), op=ALU.is_ge), reads=[r_emb, r_m12], writes=[r_s1b])
                op("dve", lambda e: e.scalar_tensor_tensor(out=em2[:], in0=s1b[:], scalar=-1.0e30, in1=emb[:], op0=ALU.mult, op1=ALU.add),
                   reads=[r_s1b, r_emb], writes=[r_em2])
                op("dve", lambda e: e.tensor_reduce(out=m12[:, 1, :], in_=em2[:], axis=AX.X, op=ALU.max), reads=[r_em2, r_m12], writes=[r_m12])
                op("dve", lambda e: e.tensor_tensor(out=s2b[:], in0=emb[:], in1=bc3(m12[:, 1, :], 32), op=ALU.is_ge), reads=[r_emb, r_m12], writes=[r_s2b])
                op("dve", lambda e, t0=t0, t1=t1: e.tensor_copy(out=Mball[:, t0:t1, :], in_=s2b[:]), reads=[r_s2b], writes=[r_Mball[g]])
                op("dve", lambda e, t0=t0, t1=t1: e.tensor_copy(out=oh1[:, t0:t1, :], in_=s1b[:]), reads=[r_s1b], writes=[r_oh1])
                op("dve", lambda e, t0=t0, t1=t1: e.tensor_tensor(out=oh2[:, t0:t1, :], in0=s2b[:], in1=s1b[:], op=ALU.subtract), reads=[r_s1b, r_s2b], writes=[r_oh2])
                op("dve", lambda e: e.tensor_reduce(out=gpr[:], in_=gxe[:], axis=AX.X, op=ALU.add), reads=[r_gxe], writes=[r_gpr])
                op("dve", lambda e: e.reciprocal(out=gpr[:], in_=gpr[:]), reads=[r_gpr], writes=[r_gpr])
                op("dve", lambda e: e.tensor_tensor(out=ww[:, 0, :], in0=m12[:, 1, :], in1=m12[:, 0, :], op=ALU.subtract), reads=[r_m12], writes=[r_ww])
                op("act", lambda e: e.activation(out=ww[:, 0, :], in_=ww[:, 0, :], func=AF.Exp), reads=[r_ww], writes=[r_ww])
                op("dve", lambda e: e.tensor_scalar(out=ww[:, 1, :], in0=ww[:, 0, :], scalar1=1.0, scalar2=None, op0=ALU.add), reads=[r_ww], writes=[r_ww])
                op("dve", lambda e: e.reciprocal(out=ww[:, 1, :], in_=ww[:, 1, :]), reads=[r_ww], writes=[r_ww])
                op("dve", lambda e: e.tensor_tensor(out=ww[:, 2, :], in0=ww[:, 0, :], in1=ww[:, 1, :], op=ALU.mult), reads=[r_ww], writes=[r_ww])
                op("dve", lambda e, t0=t0, t1=t1: e.tensor_tensor(out=wAB[:, t0:t1, 0], in0=ww[:, 1, :], in1=gpr[:], op=ALU.mult), reads=[r_ww, r_gpr], writes=[r_wAB])
                op("dve", lambda e, t0=t0, t1=t1: e.tensor_tensor(out=wAB[:, t0:t1, 1], in0=ww[:, 2, :], in1=gpr[:], op=ALU.mult), reads=[r_ww, r_gpr], writes=[r_wAB])
            for ti in range(NTILE):
                g = ti // G
                pp, rpp = nxt("S")
                op("pe", lambda e, pp=pp, ti=ti: e.matmul(pp[:, 0:32], lhsT=U[:], rhs=Mball[:, ti, :], start=True, stop=True), reads=[r_U, r_Mball[g]], writes=[rpp])
                op("dve", lambda e, pp=pp: e.tensor_tensor(out=pos[:], in0=pp[:, 0:32], in1=base[:], op=ALU.add), reads=[rpp, r_base], writes=[r_posf])
                pq, rpq = nxt("S")
                op("pe", lambda e, pq=pq, ti=ti: e.matmul(pq[:, 0:32], lhsT=ones[:], rhs=Mball[:, ti, :], start=True, stop=True), reads=[r_ones, r_Mball[g]], writes=[rpq])
                op("dve", lambda e, pq=pq: e.tensor_tensor(out=base[:], in0=pq[:, 0:32], in1=base[:], op=ALU.add), reads=[rpq, r_base], writes=[r_base])
                op("dve", lambda e, ti=ti: e.tensor_tensor(out=tmp[:], in0=pos[:], in1=oh1[:, ti, :], op=ALU.mult), reads=[r_posf, r_oh1], writes=[r_tmp])
                op("dve", lambda e, ti=ti: e.tensor_reduce(out=posAB[:, ti, 0:1], in_=tmp[:], axis=AX.X, op=ALU.add), reads=[r_tmp], writes=[r_pos])
                op("dve", lambda e, ti=ti: e.tensor_tensor(out=tmp[:], in0=pos[:], in1=oh2[:, ti, :], op=ALU.mult), reads=[r_posf, r_oh2], writes=[r_tmp])
                op("dve", lambda e, ti=ti: e.tensor_reduce(out=posAB[:, ti, 1:2], in_=tmp[:], axis=AX.X, op=ALU.add), reads=[r_tmp], writes=[r_pos])
            cmpb = sb("cmpb", [128, 32], BF16); r_cmpb = Res("cmpb")
            nblk = sb("nblk", [128, 32], F32); r_nblk = Res("nblk")
            endb = sb("endb", [128, 32], F32); r_endb = Res("endb")
            sbase = sb("sbase", [128, 32], F32); r_sbase = Res("sbase")
            op("dve", lambda e: e.tensor_scalar(out=cmpb[:], in0=base[:], scalar1=pcf[:, 1:2], scalar2=None, op0=ALU.is_gt),
               reads=[r_base, r_pcf], writes=[r_cmpb])
            pn, rpn = nxt("S")
            op("pe", lambda e: e.matmul(pn[:, 0:32], lhsT=ones[:], rhs=cmpb[:], start=True, stop=True), reads=[r_ones, r_cmpb], writes=[rpn])
            op("dve", lambda e: e.tensor_copy(out=nblk[:], in_=pn[:, 0:32]), reads=[rpn], writes=[r_nblk])
            op("dve", lambda e: e.tensor_copy(out=endb[:, 0:1], in_=nblk[:, 0:1]), reads=[r_nblk], writes=[r_endb])
            for ex in range(1, NEXP):
                op("dve", lambda e, ex=ex: e.tensor_tensor(out=endb[:, ex:ex + 1], in0=endb[:, ex - 1:ex], in1=nblk[:, ex:ex + 1], op=ALU.add),
                   reads=[r_endb, r_nblk], writes=[r_endb])
            op("dve", lambda e: e.tensor_tensor(out=sbase[:], in0=endb[:], in1=nblk[:], op=ALU.subtract), reads=[r_endb, r_nblk], writes=[r_sbase])
            op("dve", lambda e: e.tensor_scalar(out=sbase[:], in0=sbase[:], scalar1=float(BLK), scalar2=None, op0=ALU.mult), reads=[r_sbase], writes=[r_sbase])
            cmp3 = sb("cmp3", [128, NB, 32], F32); r_cmp3 = Res("cmp3")
            bex = sb("bex", [128, NB], F32); r_bex = Res("bex")
            idxw = sb("idxw", [128, NB], I32); r_idxw = Res("idxw")
            op("dve", lambda e: e.tensor_tensor(out=cmp3[:], in0=endb[:].unsqueeze(1).to_broadcast([128, NB, 32]),
                                                in1=bvf[:].unsqueeze(2).to_broadcast([128, NB, 32]), op=ALU.is_le),
               reads=[r_endb, r_bvf], writes=[r_cmp3])
            op("dve", lambda e: e.tensor_reduce(out=bex[:], in_=cmp3[:], axis=AX.X, op=ALU.add), reads=[r_cmp3], writes=[r_bex])
            op("dve", lambda e: e.tensor_scalar(out=bex[:], in0=bex[:], scalar1=float(NEXP - 1), scalar2=128.0, op0=ALU.min, op1=ALU.mult),
               reads=[r_bex], writes=[r_bex])
            op("dve", lambda e: e.tensor_scalar(out=bex[:], in0=bex[:], scalar1=pcf[:, 0:1], scalar2=None, op0=ALU.add), reads=[r_bex, r_pcf], writes=[r_bex])
            op("dve", lambda e: e.tensor_copy(out=idxw[:], in_=bex[:]), reads=[r_bex], writes=[r_idxw])
            tmp3 = sb("tmp3", [128, 32], F32); r_tmp3 = Res("tmp3")
            slf = sb("slf", [128, 2], F32); r_slf = Res("slf")
            xb = [sb(f"xb{i}", [128, D], BF16) for i in range(2)]
            r_xbs = [Res("xbs0"), Res("xbs1")]
            r_xb = [Res(f"xb{i}") for i in range(2)]
            big3 = cmp3[:, 0:NTILE, :]; r_big3 = r_cmp3
            slall = sb("slall", [128, NTILE, 2], F32); r_slall = Res("slall")
            for ab, ohx, r_ohx in ((0, oh1, r_oh1), (1, oh2, r_oh2)):
                op("dve", lambda e, ohx=ohx: e.tensor_tensor(out=big3, in0=ohx[:], in1=sbase[:].unsqueeze(1).to_broadcast([128, NTILE, 32]), op=ALU.mult),
                   reads=[r_ohx, r_sbase], writes=[r_big3])
                op("dve", lambda e, ab=ab: e.tensor_reduce(out=slall[:, :, ab], in_=big3, axis=AX.X, op=ALU.add), reads=[r_big3], writes=[r_slall])
            op("dve", lambda e: e.tensor_tensor(out=slall[:], in0=slall[:], in1=posAB[:], op=ALU.add), reads=[r_slall, r_pos], writes=[r_slall])
            op("dve", lambda e: e.tensor_copy(out=idxAB[:], in_=slall[:]), reads=[r_slall], writes=[r_idx])
            for ti in range(NTILE):
                xi = ti % 2
                if ti == 0:
                    dma("pool", lambda e: e.dma_start(out=xb[0][:], in_=x1_d[0:128, :]), reads=[r_x1], writes=[r_xb[0]])
                if ti + 1 < NTILE:
                    dma("pool", lambda e, ti=ti: e.dma_start(out=xb[(ti + 1) % 2][:], in_=x1_d[(ti + 1) * 128:(ti + 2) * 128, :]),
                        reads=[r_x1], writes=[r_xb[(ti + 1) % 2]])
                for ab in range(2):
                    dma("pool", lambda e, ti=ti, xi=xi, ab=ab: e.indirect_dma_start(
                        out=xbuf_d, out_offset=bass.IndirectOffsetOnAxis(ap=idxAB[:, ti, ab:ab + 1], axis=0), in_=xb[xi][:], in_offset=None),
                        reads=[r_xb[xi], r_idx], writes=[], sem_res=r_xbs[xi])
            fence = sb("fence", [128, D], BF16); r_fence = Res("fence")
            dma("pool", lambda e: e.dma_start(out=fence[:], in_=xbuf_d[CAP - 128:CAP, :]), writes=[r_fence])
            dma("pool", lambda e: e.dma_start(out=fence[:], in_=xbuf_d[0:128, :]), writes=[r_fence])
            S_.barrier()
            wgt = [sb(f"wgt{i}", [128, 8, DE], BF16) for i in range(2)]
            wut = [sb(f"wut{i}", [128, 8, DE], BF16) for i in range(2)]
            wdt = [sb(f"wdt{i}", [128, 4, D], BF16) for i in range(2)]
            r_weg = [Res(f"weg{i}") for i in range(2)]
            r_weu = [Res(f"weu{i}") for i in range(2)]
            r_wed = [Res(f"wed{i}") for i in range(2)]
            xs = [sb(f"xs{i}", [128, 2, D], BF16) for i in range(2)]
            r_xs = [Res(f"xs{i}") for i in range(2)]
            xT = [sb(f"xT{i}", [128, 8, BLK], BF16) for i in range(2)]
            r_xT = [Res(f"xT{i}") for i in range(2)]
            actT = sb("actT", [128, 4, BLK], BF16); r_actT = [Res(f"actT{i}") for i in range(4)]
            sg = [sb(f"sg{i}", [128, BLK], F32) for i in range(2)]
            r_sg = [Res(f"sg{i}") for i in range(2)]
            yb = [sb(f"yb{i}", [128, D], F32) for i in range(2)]
            r_yb = [Res(f"yb{i}") for i in range(2)]
            yctr = [0]
            def load_block(b):
                bi = b % 2
                for (wt_, w2, rw) in ((wgt, wgb2, r_weg), (wut, wub2, r_weu), (wdt, wdb2, r_wed)):
                    dma("pool", lambda e, b=b, bi=bi, wt_=wt_, w2=w2: e.indirect_dma_start(
                        out=wt_[bi][:].rearrange("p k n -> p (k n)"), out_offset=None, in_=w2,
                        in_offset=bass.IndirectOffsetOnAxis(ap=idxw[:, b:b + 1], axis=0)), reads=[r_idxw], writes=[rw[bi]])
                dma("sp", lambda e, b=b, bi=bi: e.dma_start(out=xs[bi][:], in_=xbuf_d[b * BLK:(b + 1) * BLK, :].rearrange("(s p) d -> p s d", p=128)),
                    writes=[r_xs[bi]])

            load_block(0)
            for b in range(NB):
                bi = b % 2
                if b + 1 < NB:
                    load_block(b + 1)
                for st_ in range(2):
                    pt, rpt = nxt("T")
                    for j in range(8):
                        op("pe", lambda e, pt=pt, j=j, st_=st_, bi=bi: e.transpose(pt[:, j * 128:(j + 1) * 128], xs[bi][:, st_, j * 128:(j + 1) * 128], ident[:]),
                           reads=[r_xs[bi], r_ident], writes=[rpt])
                    if st_ == 0:
                        op("act", lambda e, pt=pt, bi=bi, st_=st_: e.activation(out=xT[bi][:, :, st_ * 128:(st_ + 1) * 128],
                                                                               in_=pt[:].rearrange("p (j t) -> p j t", j=8), func=AF.Copy),
                           reads=[rpt], writes=[r_xT[bi]])
                    else:
                        op("dve", lambda e, pt=pt, bi=bi, st_=st_: e.tensor_copy(out=xT[bi][:, :, st_ * 128:(st_ + 1) * 128],
                                                                                in_=pt[:].rearrange("p (j t) -> p j t", j=8)),
                           reads=[rpt], writes=[r_xT[bi]])
                for fc in range(4):
                    pg, rpg = nxt("S")
                    for k in range(8):
                        op("pe", lambda e, pg=pg, k=k, fc=fc, bi=bi: e.matmul(pg[:, 0:BLK], lhsT=wgt[bi][:, k, fc * 128:(fc + 1) * 128], rhs=xT[bi][:, k, :],
                                                                              start=(k == 0), stop=(k == 7)), reads=[r_weg[bi], r_xT[bi]], writes=[rpg])
                    pu, rpu = nxt("S")
                    for k in range(8):
                        op("pe", lambda e, pu=pu, k=k, fc=fc, bi=bi: e.matmul(pu[:, 0:BLK], lhsT=wut[bi][:, k, fc * 128:(fc + 1) * 128], rhs=xT[bi][:, k, :],
                                                                              start=(k == 0), stop=(k == 7)), reads=[r_weu[bi], r_xT[bi]], writes=[rpu])
                    si = fc % 2
                    op("act", lambda e, pg=pg, si=si: e.activation(out=sg[si][:], in_=pg[:, 0:BLK], func=AF.Silu), reads=[rpg], writes=[r_sg[si]])
                    op("dve", lambda e, pu=pu, si=si, fc=fc: e.tensor_tensor(out=actT[:, fc, :], in0=pu[:, 0:BLK], in1=sg[si][:], op=ALU.mult),
                       reads=[rpu, r_sg[si]], writes=[r_actT[fc]])
                for st_ in range(2):
                    yi = yctr[0] % 2
                    yctr[0] += 1
                    for hh in range(2):
                        ps, rp = nxt("O")
                        for fc in range(4):
                            op("pe", lambda e, ps=ps, fc=fc, st_=st_, hh=hh, bi=bi: e.matmul(
                                ps[:], lhsT=actT[:, fc, st_ * 128:(st_ + 1) * 128], rhs=wdt[bi][:, fc, hh * 512:(hh + 1) * 512],
                                start=(fc == 0), stop=(fc == 3)), reads=[r_actT[fc], r_wed[bi]], writes=[rp])
                        if hh == 0:
                            op("act", lambda e, ps=ps, yi=yi, hh=hh: e.activation(out=yb[yi][:, hh * 512:(hh + 1) * 512], in_=ps[:], func=AF.Copy),
                               reads=[rp], writes=[r_yb[yi]])
                        else:
                            op("dve", lambda e, ps=ps, yi=yi, hh=hh: e.tensor_copy(out=yb[yi][:, hh * 512:(hh + 1) * 512], in_=ps[:]),
                               reads=[rp], writes=[r_yb[yi]])
                    dma("sp", lambda e, b=b, st_=st_, yi=yi: e.dma_start(out=ybuf_d[b * BLK + st_ * 128:b * BLK + (st_ + 1) * 128, :], in_=yb[yi][:]),
                        reads=[r_yb[yi]], writes=[], sem_res=r_ybuf)
            S_.barrier()
            yA = [sb(f"yA{i}", [128, D], F32) for i in range(4)]
            yB = [sb(f"yB{i}", [128, D], F32) for i in range(4)]
            r_yA = [Res(f"yA{i}") for i in range(4)]
            r_yB = [Res(f"yB{i}") for i in range(4)]
            xr = [sb(f"xr{i}", [128, D], F32) for i in range(4)]
            r_xr = [Res(f"xr{i}") for i in range(4)]
            stats_l = [sb(f"stats2{i}", [128, 2, 6], F32) for i in range(4)]; r_stats_l = [Res(f"stats2{i}") for i in range(4)]
            mv_l = [sb(f"mv2{i}", [128, 2], F32) for i in range(4)]; r_mv_l = [Res(f"mv2{i}") for i in range(4)]
            rstd_l = [sb(f"rstd2{i}", [128, 1], F32) for i in range(4)]; r_rstd_l = [Res(f"rstd2{i}") for i in range(4)]
            def combine_fetch(ti):
                xi = ti % 4
                dma("pool", lambda e, ti=ti, xi=xi: e.indirect_dma_start(out=yA[xi][:], out_offset=None, in_=ybuf_d,
                                                                        in_offset=bass.IndirectOffsetOnAxis(ap=idxAB[:, ti, 0:1], axis=0)),
                    reads=[r_idx], writes=[r_yA[xi]])
                dma("pool", lambda e, ti=ti, xi=xi: e.indirect_dma_start(out=yB[xi][:], out_offset=None, in_=ybuf_d,
                                                                        in_offset=bass.IndirectOffsetOnAxis(ap=idxAB[:, ti, 1:2], axis=0)),
                    reads=[r_idx], writes=[r_yB[xi]])
                dma("sp", lambda e, xi=xi, ti=ti: e.dma_start(out=xr[xi][:], in_=x1_d[ti * 128:(ti + 1) * 128, :]), reads=[r_x1], writes=[r_xr[xi]])

            def cvars(ti):
                xi = ti % 4
                return xi, stats_l[xi], r_stats_l[xi], mv_l[xi], r_mv_l[xi], rstd_l[xi], r_rstd_l[xi]

            def combine_A(ti):
                xi, stats, r_stats, mv, r_mv, rstd, r_rstd = cvars(ti)
                op("act", lambda e, xi=xi: e.activation(out=xr[xi][:], in_=xr[xi][:], func=AF.Copy, scale=ALPHA), reads=[r_xr[xi]], writes=[r_xr[xi]])
                op("dve", lambda e, xi=xi, ti=ti: e.scalar_tensor_tensor(out=xr[xi][:], in0=yA[xi][:], scalar=wAB[:, ti, 0:1], in1=xr[xi][:],
                                                                         op0=ALU.mult, op1=ALU.add), reads=[r_yA[xi], r_wAB, r_xr[xi]], writes=[r_xr[xi]])
                op("dve", lambda e, xi=xi, ti=ti: e.scalar_tensor_tensor(out=xr[xi][:], in0=yB[xi][:], scalar=wAB[:, ti, 1:2], in1=xr[xi][:],
                                                                         op0=ALU.mult, op1=ALU.add), reads=[r_yB[xi], r_wAB, r_xr[xi]], writes=[r_xr[xi]])
                for hh in range(2):
                    op("dve", lambda e, hh=hh, xi=xi, stats=stats: e.bn_stats(out=stats[:, hh, :], in_=xr[xi][:, hh * 512:(hh + 1) * 512]),
                       reads=[r_xr[xi]], writes=[r_stats])
                op("dve", lambda e, stats=stats, mv=mv: e.bn_aggr(out=mv[:], in_=stats[:].rearrange("p a b -> p (a b)")), reads=[r_stats], writes=[r_mv])
                op("act", lambda e, mv=mv, rstd=rstd: e.activation(out=rstd[:], in_=mv[:, 1:2], func=AF.Sqrt, bias=EPS, scale=1.0), reads=[r_mv], writes=[r_rstd])

            def combine_B(ti):
                xi, stats, r_stats, mv, r_mv, rstd, r_rstd = cvars(ti)
                op("dve", lambda e, rstd=rstd: e.reciprocal(out=rstd[:], in_=rstd[:]), reads=[r_rstd], writes=[r_rstd])
                op("dve", lambda e, mv=mv, rstd=rstd: e.tensor_scalar(out=mv[:, 1:2], in0=mv[:, 0:1], scalar1=rstd[:, 0:1], scalar2=-1.0,
                                                                     op0=ALU.mult, op1=ALU.mult), reads=[r_mv, r_rstd], writes=[r_mv])
                op("act", lambda e, xi=xi, mv=mv, rstd=rstd: e.activation(out=xr[xi][:], in_=xr[xi][:], func=AF.Identity, bias=mv[:, 1:2], scale=rstd[:, 0:1]),
                   reads=[r_xr[xi], r_mv, r_rstd], writes=[r_xr[xi]])

            def combine_C(ti):
                xi, stats, r_stats, mv, r_mv, rstd, r_rstd = cvars(ti)
                op("dve", lambda e, xi=xi: e.tensor_tensor(out=xr[xi][:], in0=xr[xi][:], in1=ln_t[:, 0, :], op=ALU.mult),
                   reads=[r_xr[xi], r_ln], writes=[r_xr[xi]])
                op("dve", lambda e, xi=xi: e.tensor_tensor(out=xr[xi][:], in0=xr[xi][:], in1=ln_t[:, 1, :], op=ALU.add),
                   reads=[r_xr[xi], r_ln], writes=[r_xr[xi]])
                dma("sp", lambda e, xi=xi, ti=ti: e.dma_start(out=out_d[ti * 128:(ti + 1) * 128, :], in_=xr[xi][:]),
                    reads=[r_xr[xi]], writes=[r_out])

            for ti in range(min(2, NTILE)):
                combine_fetch(ti)
            for step in range(NTILE + 2):
                if 0 <= step - 2 < NTILE:
                    combine_C(step - 2)
                if 0 <= step - 1 < NTILE:
                    combine_B(step - 1)
                if step < NTILE:
                    combine_A(step)
                if step + 2 < NTILE:
                    combine_fetch(step + 2)
            S_.barrier()
            with nc.Block() as blk:
                S_.emit(blk)
    return nc


def host_inputs(x2, w_in, b_in, attn_sinks, rel_bias_table, w_branch_swa, w_branch_moba, w_out, ln1_gain, ln1_bias,
                w_group_router, b_group_router, w_expert_router, b_expert_router, w_expert_gate, w_expert_up,
                w_expert_down, ln2_gain, ln2_bias):
    f = lambda a: np.ascontiguousarray(np.asarray(a, dtype=np.float32))
    w = np.asarray(w_in)[0]
    b = np.asarray(b_in)[0]
    perm = np.arange(4352)
    qperm = []
    for c in range(4):
        qperm += list(range(c * 64, c * 64 + 64)) + list(range((4 + c) * 64, (4 + c) * 64 + 64))
    perm[0:512] = np.array(qperm)
    wp = w[:, perm]
    bp = b[perm]
    qk_cols = list(range(0, 640)) + list(range(768, 1792))
    bqk = bp[qk_cols].reshape(13, 128).T
    bgt = bp[2304:4352].reshape(16, 128).T
    bvv = np.concatenate([bp[640:768], bp[1792:2304]])[None, :]
    rel = np.asarray(rel_bias_table)
    k = np.arange(128)[:, None]
    q = np.arange(128)[None, :]
    d_own = rel_bucket_np(q - k)
    d_prev = rel_bucket_np(q + 128 - k)
    ga = np.stack([rel[d_own][:, :, :8].transpose(0, 2, 1), rel[d_prev][:, :, :8].transpose(0, 2, 1)], axis=1)
    j = np.arange(1024)[None, :]
    gbk = rel_bucket_np(j - k - 384)
    gb = rel[gbk][:, :, 8:].transpose(0, 2, 1)
    common = {
        "w_in": f(wp), "bqk": f(bqk), "bg": f(bgt), "bv": f(bvv),
        "sinks": f(np.asarray(attn_sinks)[0][None, :]), "t31": f(rel[31:32, 8:16]),
        "ga_raw": f(ga.reshape(128, -1)), "gb_raw": f(gb.reshape(128, -1)),
        "wa": f(np.asarray(w_branch_swa)[0]), "wb": f(np.asarray(w_branch_moba)[0]), "wo": f(np.asarray(w_out)[0]),
        "ln": f(np.stack([np.asarray(ln1_gain)[0], np.asarray(ln1_bias)[0], np.asarray(ln2_gain)[0], np.asarray(ln2_bias)[0]])),
        "wr": f(np.concatenate([np.asarray(w_group_router)[0], np.asarray(w_expert_router)[0]], axis=1)),
        "br": f(np.concatenate([np.asarray(b_group_router)[0], np.asarray(b_expert_router)[0]])[None, :]),
        "wg": f(np.asarray(w_expert_gate)[0]), "wu": f(np.asarray(w_expert_up)[0]), "wd": f(np.asarray(w_expert_down)[0]),
    }
    return common


def run(x, ncores, nseq, S, **params):
    x = np.asarray(x, dtype=np.float32)
    common = host_inputs(x, **params)
    nc = build(nseq, S)
    in_maps = []
    for i in range(ncores):
        xs = x[i * nseq:(i + 1) * nseq].reshape(nseq * S, D)
        m = dict(common)
        m["x_tok"] = np.ascontiguousarray(xs)
        m["x_T"] = np.ascontiguousarray(xs.T)
        in_maps.append(m)
    res = run_bass_kernel_spmd(nc, in_maps, core_ids=list(range(ncores)))
    outs = [np.asarray(r["out"]).reshape(nseq, S, D) for r in res.results]
    return np.concatenate(outs, axis=0).astype(np.float32)


def kernel(x, **params):
    B, S, _ = np.asarray(x).shape
    return run(x, NCORES, B // NCORES, S, **params)
```

```python
from contextlib import ExitStack
import os


class StopBuild(Exception):
    pass


def chk(n):
    if int(os.environ.get('KSTOP', '99')) == n:
        raise StopBuild()

import numpy as np
import concourse.bass as bass
import concourse.mybir as mybir
from concourse.bass_utils import run_bass_kernel_spmd

F32 = mybir.dt.float32
BF16 = mybir.dt.bfloat16
ALU = mybir.AluOpType
AF = mybir.ActivationFunctionType
AX = mybir.AxisListType

D = 1024
NCORES = 8
BIG = 30000.0
ALPHA = 2.0 ** 0.25
EPS = 1e-5
NEXP = 32
DE = 512


class Res:
    __slots__ = ("name", "w", "r", "dsem", "dcount", "excl")

    def __init__(self, name, excl=False):
        self.name = name
        self.w = None
        self.r = {}
        self.dsem = None
        self.dcount = 0
        self.excl = excl


class Sched:
    ENGS = ("pe", "act", "dve", "pool", "sp")

    def __init__(self, nc, stack):
        self.nc = nc
        self.ops = {e: [] for e in self.ENGS}
        self.cnt = {e: 0 for e in self.ENGS}
        self.seen = {e: {} for e in self.ENGS}
        self.sems = {}
        self.dma_keys = {}
        self._stack = stack

    def _sem(self, key):
        s = self.sems.get(key)
        if s is None:
            s = self._stack.enter_context(self.nc.semaphore("s_" + str(key)))
            self.sems[key] = s
        return s

    def _collect(self, eng, reads, writes):
        need = {}

        def add(tok):
            if tok is None:
                return
            k, v = tok
            if k == eng and eng == "pe":
                return
            if need.get(k, 0) < v:
                need[k] = v
        for r in reads:
            add(r.w)
            if r.excl:
                for k, v in r.r.items():
                    add((k, v))
        for w in writes:
            add(w.w)
            for k, v in w.r.items():
                add((k, v))
        waits = []
        seen = self.seen[eng]
        for k, v in need.items():
            if seen.get(k, 0) < v:
                seen[k] = v
                waits.append((k, v))
        return waits

    def _update(self, tok, reads, writes):
        for r in reads:
            if r.excl:
                r.w = tok
                r.r = {}
            elif r.r.get(tok[0], 0) < tok[1]:
                r.r[tok[0]] = tok[1]
        for w in writes:
            w.w = tok
            w.r = {}

    def op(self, eng, fn, reads=(), writes=()):
        waits = self._collect(eng, reads, writes)
        self.cnt[eng] += 1
        tok = (eng, self.cnt[eng])
        self._sem(eng)
        for k, _ in waits:
            self._sem(k)
        self.ops[eng].append((waits, fn, (eng, 1)))
        self._update(tok, reads, writes)
        return tok

    def raw(self, eng, fn, reads=()):
        waits = self._collect(eng, reads, ())
        for k, _ in waits:
            self._sem(k)
        self.ops[eng].append((waits, fn, "raw"))

    def dma(self, q, fn, reads=(), writes=(), sem_res=None):
        waits = self._collect(q, reads, writes)
        sr = sem_res if sem_res is not None else writes[0]
        if sr.dsem is None:
            sr.dsem = "d_" + sr.name
        sr.dcount += 16
        self.dma_keys[sr.dsem] = sr.dcount
        tok = (sr.dsem, sr.dcount)
        self._sem(sr.dsem)
        for k, _ in waits:
            self._sem(k)
        self.ops[q].append((waits, fn, (sr.dsem, 16)))
        self._update(tok, reads, writes)
        return tok

    def barrier(self):
        for e in self.ENGS:
            waits = []
            seen = self.seen[e]
            for k in self.ENGS:
                v = self.cnt[k]
                if k != e and v > 0 and seen.get(k, 0) < v:
                    seen[k] = v
                    waits.append((k, v))
            for k, v in self.dma_keys.items():
                if seen.get(k, 0) < v:
                    seen[k] = v
                    waits.append((k, v))
            self.ops[e].append((waits, None, None))

    def emit(self, block):
        emap = {"pe": block.tensor, "act": block.scalar, "dve": block.vector,
                "pool": block.gpsimd, "sp": block.sync}
        for ename in self.ENGS:
            ops = self.ops[ename]
            if not ops:
                continue

            def body(e, ops=ops):
                for waits, fn, inc in ops:
                    for k, v in waits:
                        e.wait_ge(self.sems[k], v)
                    if fn is None:
                        continue
                    if inc == "raw":
                        fn(e)
                    else:
                        fn(e).then_inc(self.sems[inc[0]], inc[1])
            emap[ename](body)
            self.ops[ename] = []


def rel_bucket_np(dist):
    n = np.maximum(dist, 0)
    nf = np.maximum(n, 1).astype(np.float32)
    large = 16 + (np.log(nf / np.float32(16)) / np.float32(np.log(8.0)) * 16).astype(np.int32)
    large = np.minimum(large, 31)
    return np.where(n < 16, n, large)


def build(NSEQ, S):
    NT = NSEQ * S
    NCH = S // 512
    NTT = S // 128
    nc = bass.Bass("TRN2", target_bir_lowering=False)
    dt = lambda name, shape, ty, kind="ExternalInput": nc.dram_tensor(name, shape, ty, kind=kind).ap()
    x_tok = dt("x_tok", [NT, D], F32)
    x_T = dt("x_T", [D, NT], F32)
    w_in = dt("w_in", [D, 4352], F32)
    bqk = dt("bqk", [128, 13], F32)
    bg = dt("bg", [128, 16], F32)
    bv = dt("bv", [1, 640], F32)
    sinks = dt("sinks", [1, 8], F32)
    t31 = dt("t31", [1, 8], F32)
    ga_raw = dt("ga_raw", [128, 2 * 8 * 128], F32)
    gb_raw = dt("gb_raw", [128, 8 * 1024], F32)
    wa_d = dt("wa", [512, D], F32)
    wb_d = dt("wb", [512, D], F32)
    wo_d = dt("wo", [D, D], F32)
    ln_d = dt("ln", [4, D], F32)
    wr_d = dt("wr", [D, 36], F32)
    br_d = dt("br", [1, 36], F32)
    wg_d = dt("wg", [NEXP, D, DE], F32)
    wu_d = dt("wu", [NEXP, D, DE], F32)
    wd_d = dt("wd", [NEXP, DE, D], F32)
    out_d = dt("out", [NT, D], F32, kind="ExternalOutput")
    x1_d = dt("x1_scr", [NT, D], F32, kind="Internal")
    winb_d = dt("winb_scr", [128, 8 * (4352 + D)], BF16, kind="Internal")
    x1T_d = dt("x1T_scr", [D, NT], BF16, kind="Internal")
    wgb_d = dt("wgb_scr", [NEXP, 128, 8 * DE], BF16, kind="Internal")
    wub_d = dt("wub_scr", [NEXP, 128, 8 * DE], BF16, kind="Internal")
    wdb_d = dt("wdb_scr", [NEXP, 128, 4 * D], BF16, kind="Internal")
    xbuf_d = dt("xbuf_scr", [(2 * NT) // 256 * 256 + NEXP * 256, D], BF16, kind="Internal")
    ybuf_d = dt("ybuf_scr", [(2 * NT) // 256 * 256 + NEXP * 256, D], F32, kind="Internal")

    with ExitStack() as top:
        S_ = Sched(nc, top)
        op, dma = S_.op, S_.dma
        psS = [top.enter_context(nc.psum_tensor(f"psS{i}", [128, 512], F32)) for i in range(4)]
        rS = [Res(f"psS{i}", excl=True) for i in range(4)]
        psO = [top.enter_context(nc.psum_tensor(f"psO{i}", [128, 512], F32)) for i in range(2)]
        rO = [Res(f"psO{i}", excl=True) for i in range(2)]
        psT = [top.enter_context(nc.psum_tensor(f"psT{i}", [128, 1024], BF16)) for i in range(2)]
        rT = [Res(f"psT{i}", excl=True) for i in range(2)]
        ctr = {"S": 0, "O": 0, "T": 0}

        def nxt(kind):
            lst, rl = {"S": (psS, rS), "O": (psO, rO), "T": (psT, rT)}[kind]
            i = ctr[kind] % len(lst)
            ctr[kind] += 1
            return lst[i], rl[i]

        r_x1 = Res("x1_scr")
        r_x1s = [Res("x1s0"), Res("x1s1")]
        r_x1T = Res("x1T_scr")
        r_cw = [Res("cw0"), Res("cw1")]
        r_out = Res("out")

        with ExitStack() as st:
            sb = lambda name, shape, ty: st.enter_context(nc.sbuf_tensor(name, shape, ty))
            ident = sb("ident", [128, 128], BF16); r_ident = Res("ident")
            identf = sb("identf", [128, 128], F32); r_identf = Res("identf")
            op("pool", lambda e: e.memset(identf[:], 0.0), writes=[r_identf])
            op("pool", lambda e: e.affine_select(out=identf[:], in_=identf[:], pattern=[[-1, 128]],
                                                 compare_op=ALU.not_equal, fill=1.0, base=0,
                                                 channel_multiplier=1), reads=[r_identf], writes=[r_identf])
            op("dve", lambda e: e.tensor_copy(out=ident[:], in_=identf[:]), reads=[r_identf], writes=[r_ident])
            bqk_t = sb("bqk_t", [128, 13], F32); r_bqk = Res("bqk")
            bg_t = sb("bg_t", [128, 16], F32); r_bg = Res("bg")
            bv_t = sb("bv_t", [128, 640], F32); r_bv = Res("bv")
            es_t = sb("es_t", [128, 8], F32); r_es = Res("es")
            t31_t = sb("t31_t", [128, 8], F32); r_t31 = Res("t31")
            ln_t = sb("ln_t", [128, 2, D], F32); r_ln = Res("ln")
            dma("sp", lambda e: e.dma_start(out=bqk_t[:], in_=bqk), writes=[r_bqk])
            dma("sp", lambda e: e.dma_start(out=bg_t[:], in_=bg), writes=[r_bg])
            dma("sp", lambda e: e.dma_start(out=bv_t[:], in_=bv.partition_broadcast(128)), writes=[r_bv])
            dma("sp", lambda e: e.dma_start(out=es_t[:], in_=sinks.partition_broadcast(128)), writes=[r_es])
            dma("sp", lambda e: e.dma_start(out=t31_t[:], in_=t31.partition_broadcast(128)), writes=[r_t31])
            for i in range(2):
                dma("sp", lambda e, i=i: e.dma_start(out=ln_t[:, i, :], in_=ln_d[i:i + 1, :].partition_broadcast(128)), writes=[r_ln])
            op("act", lambda e: e.activation(out=es_t[:], in_=es_t[:], func=AF.Exp), reads=[r_es], writes=[r_es])
            op("dve", lambda e: e.tensor_scalar(out=bqk_t[:, 0:4], in0=bqk_t[:, 0:4], scalar1=0.125, scalar2=None, op0=ALU.mult), reads=[r_bqk], writes=[r_bqk])
            op("dve", lambda e: e.tensor_scalar(out=bqk_t[:, 5:9], in0=bqk_t[:, 5:9], scalar1=0.125, scalar2=None, op0=ALU.mult), reads=[r_bqk], writes=[r_bqk])
            PMr = sb("PMr", [128, 8, 32], F32); r_PMr = Res("PMr")
            OWr = sb("OWr", [128, 8, 32], F32); r_OWr = Res("OWr")
            op("pool", lambda e: e.memset(PMr[:, :, 0:16], 0.0), writes=[r_PMr])
            op("pool", lambda e: e.memset(PMr[:, :, 16:32], -3.0e38), reads=[r_PMr], writes=[r_PMr])
            op("pool", lambda e: e.memset(OWr[:], 0.0), writes=[r_OWr])
            op("pool", lambda e: e.memset(OWr[:, :, 16:17], 1.0), reads=[r_OWr], writes=[r_OWr])
            Gs = sb("Gs", [128, 2, 8, 128], BF16); r_Gs = Res("Gs")
            dma("pool", lambda e: e.dma_start(out=Gs[:].rearrange("p a h q -> p (a h q)"), in_=ga_raw), writes=[r_Gs])
            op("pool", lambda e: e.affine_select(out=Gs[:, 0, :, :], in_=Gs[:, 0, :, :], pattern=[[0, 8], [1, 128]],
                                                 compare_op=ALU.is_ge, fill=-BIG, base=0, channel_multiplier=-1),
               reads=[r_Gs], writes=[r_Gs])
            op("pool", lambda e: e.affine_select(out=Gs[:, 1, :, :], in_=Gs[:, 1, :, :], pattern=[[0, 8], [-1, 128]],
                                                 compare_op=ALU.is_gt, fill=-BIG, base=0, channel_multiplier=1),
               reads=[r_Gs], writes=[r_Gs])
            Gb = sb("Gb", [128, 8, 640], BF16); r_Gb = Res("Gb")
            xres = [sb(f"xres{i}", [128, D], F32) for i in range(2)]
            r_xres = [Res(f"xres{i}") for i in range(2)]
            gtmp = xres[0]; r_gtmp = r_xres[0]
            for h in range(8):
                dma("sp", lambda e, h=h: e.dma_start(out=gtmp[:, 0:640], in_=gb_raw[:, h * 1024:h * 1024 + 640]), writes=[r_gtmp])
                op("dve", lambda e, h=h: e.tensor_scalar(out=Gb[:, h, :], in0=gtmp[:, 0:640], scalar1=t31_t[:, h:h + 1], scalar2=None,
                                                         op0=ALU.subtract), reads=[r_gtmp, r_t31], writes=[r_Gb])
            op("pool", lambda e: e.affine_select(out=Gb[:], in_=Gb[:], pattern=[[0, 8], [1, 640]],
                                                 compare_op=ALU.is_ge, fill=-BIG, base=-384, channel_multiplier=-1),
               reads=[r_Gb], writes=[r_Gb])
            wa = sb("wa_t", [128, 4, D], BF16); r_wa = Res("wa")
            wb = sb("wb_t", [128, 4, D], BF16); r_wb = Res("wb")
            dma("pool", lambda e: e.dma_start(out=wa[:], in_=wa_d.rearrange("(k p) n -> p k n", p=128)), writes=[r_wa])
            dma("pool", lambda e: e.dma_start(out=wb[:], in_=wb_d.rearrange("(k p) n -> p k n", p=128)), writes=[r_wb])
            KmT = sb("KmT", [128, 4, S], BF16); r_Km = [Res(f"Km{c}") for c in range(NCH)]
            KsT = sb("KsT", [128, 1024], BF16); r_Ks = [Res(f"Ks{c}") for c in range(2)]
            Vm = sb("Vm", [128, NTT, 8, 65], BF16); r_Vm = [Res(f"Vm{c}") for c in range(NCH)]
            Vs = sb("Vs", [128, 8, 2, 65], BF16); r_Vs = [Res(f"Vs{c}") for c in range(2)]
            kmT = sb("kmT", [128, 4, 16], BF16); r_km = Res("kmT")
            kms = sb("kms", [128, 4, 2], F32); r_kms = Res("kms")
            op("pool", lambda e: e.memset(Vm[:, :, :, 64:65], 1.0), writes=r_Vm)
            op("pool", lambda e: e.memset(Vs[:, :, :, 64:65], 1.0), writes=r_Vs)
            op("pool", lambda e: e.memset(kmT[:], 0.0), writes=[r_km])
            zero_q = True
            wbuf = [sb(f"wbuf{i}", [128, 8 * 768], BF16) for i in range(2)]
            wview = lambda i, n: wbuf[i][:, 0:8 * n].rearrange("p (k n) -> p k n", k=8)
            r_wbuf = [Res(f"wbuf{i}") for i in range(2)]
            wctr = [0]
            cbuf = [sb(f"cbuf{i}", [128, 1024], BF16) for i in range(2)]
            r_cbuf = [Res(f"cbuf{i}") for i in range(2)]
            conv_list = []
            for ex in range(NEXP):
                for (src, dst, K_) in ((wg_d, wgb_d, 8), (wu_d, wub_d, 8), (wd_d, wdb_d, 4)):
                    for half in range(4):
                        conv_list.append((src, dst, K_, ex, half))
            conv_state = {"next": 0, "pending": None}
            n_slots = NSEQ * NCH * 8
            conv_per_slot = -(-len(conv_list) // n_slots)

            def conv_flush():
                p_ = conv_state["pending"]
                if p_ is not None:
                    i, dst, ex, half = p_
                    dma("sp", lambda e, i=i, dst=dst, ex=ex, half=half: e.dma_start(out=dst[ex][:, half * 1024:(half + 1) * 1024], in_=cbuf[i][:]),
                        reads=[r_cbuf[i]], writes=[], sem_res=r_cw[i])
                    conv_state["pending"] = None

            def conv_step():
                for _ in range(conv_per_slot):
                    conv_flush()
                    n_ = conv_state["next"]
                    if n_ >= len(conv_list):
                        return
                    src, dst, K_, ex, half = conv_list[n_]
                    conv_state["next"] = n_ + 1
                    i = n_ % 2
                    dma("pool", lambda e, i=i, src=src, ex=ex, K_=K_, half=half: e.dma_start(
                        out=cbuf[i][:].rearrange("p (k n) -> p k n", k=K_ // 4),
                        in_=src[ex].rearrange("(k p) n -> p k n", p=128)[:, half * (K_ // 4):(half + 1) * (K_ // 4), :]),
                        writes=[r_cbuf[i]])
                    conv_state["pending"] = (i, dst, ex, half)
            xTc = [sb(f"xTc{i}", [128, 8, 512], BF16) for i in range(2)]
            r_xTc = [Res(f"xTc{i}") for i in range(2)]
            QsT = sb("QsT", [128, 4, 512], BF16); r_Qs = Res("QsT")
            QmT = sb("QmTz", [128, 8, 512], BF16); r_Qm = Res("QmT")
            op("pool", lambda e: e.memset(QmT[:], 0.0), writes=[r_Qm])
            gm = sb("gm", [128, 8, 16], F32); r_gm = Res("gm")
            mx = sb("mx", [128, 8, 8], F32); r_mx = Res("mx")
            thr = sb("thr", [128, 8], F32); r_thr = Res("thr")
            sel = sb("sel", [128, 8, 16], F32); r_sel = Res("sel")
            madd4 = [sb(f"madd{i}", [128, 128], BF16) for i in range(4)]; r_madd4 = [Res(f"madd{i}") for i in range(4)]
            maddT = sb("maddT", [128, 512], BF16); r_maddT = Res("maddT")
            PT = [sb(f"PT{i}", [128, 512], BF16) for i in range(3)]
            r_PT = [Res(f"PT{i}") for i in range(3)]
            pctr = [0]
            rden = sb("rden", [128, 4], F32); r_rden = Res("rden")
            ytok = sb("ytok", [128, 4, D], BF16); r_ytok = Res("ytok")
            yT = sb("yT", [128, 8, 512], BF16); r_yT = Res("yT")
            mT = ytok[:].rearrange("p a (b c) -> p (a b) c", c=512); r_mT = r_ytok
            g1 = sb("g1", [128, 512], F32); r_g1 = Res("g1")
            g2 = sb("g2", [128, 512], F32); r_g2 = Res("g2")
            t1 = g1; r_t1 = r_g1
            t2 = g2; r_t2 = r_g2
            z = xres
            r_z = r_xres
            stats_l = [sb(f"stats{i}", [128, 2, 6], F32) for i in range(2)]; r_stats_l = [Res(f"stats{i}") for i in range(2)]
            mv_l = [sb(f"mv{i}", [128, 2], F32) for i in range(2)]; r_mv_l = [Res(f"mv{i}") for i in range(2)]
            rstd_l = [sb(f"rstd{i}", [128, 1], F32) for i in range(2)]; r_rstd_l = [Res(f"rstd{i}") for i in range(2)]

            def load_w(c0, ncols, dst0=0, new=True, conv=True):
                if new:
                    wctr[0] += 1
                i = wctr[0] % len(wbuf)
                dma("sp", lambda e: e.dma_start(out=wbuf[i][:, 0:8 * ncols], in_=winb_d[:, 8 * c0:8 * c0 + 8 * ncols]),
                    reads=[r_winb_g[c0]], writes=[r_wbuf[i]])
                return wview(i, ncols), r_wbuf[i]

            def layer_norm(zt, r_zt, gi):
                stats, r_stats, mv, r_mv, rstd, r_rstd = stats_l[gi], r_stats_l[gi], mv_l[gi], r_mv_l[gi], rstd_l[gi], r_rstd_l[gi]
                for hh in range(2):
                    op("dve", lambda e, hh=hh: e.bn_stats(out=stats[:, hh, :], in_=zt[:, hh * 512:(hh + 1) * 512]),
                       reads=[r_zt], writes=[r_stats])
                op("dve", lambda e: e.bn_aggr(out=mv[:], in_=stats[:].rearrange("p a b -> p (a b)")), reads=[r_stats], writes=[r_mv])
                op("act", lambda e: e.activation(out=rstd[:], in_=mv[:, 1:2], func=AF.Sqrt, bias=EPS, scale=1.0),
                   reads=[r_mv], writes=[r_rstd])
                op("dve", lambda e: e.reciprocal(out=rstd[:], in_=rstd[:]), reads=[r_rstd], writes=[r_rstd])
                op("dve", lambda e: e.tensor_scalar(out=mv[:, 1:2], in0=mv[:, 0:1], scalar1=rstd[:, 0:1], scalar2=-1.0,
                                                    op0=ALU.mult, op1=ALU.mult), reads=[r_mv, r_rstd], writes=[r_mv])
                op("act", lambda e: e.activation(out=zt[:], in_=zt[:], func=AF.Identity, bias=mv[:, 1:2], scale=rstd[:, 0:1]),
                   reads=[r_zt, r_mv, r_rstd], writes=[r_zt])
                return gi

            r_winb_g = {}
            groups = [([(w_in, 0, 768, 0)], 768, 0), ([(w_in, 768, 512, 0)], 512, 768), ([(w_in, 1280, 512, 0)], 512, 1280),
                      ([(w_in, 1792, 512, 0)], 512, 1792)]
            for jp in range(4):
                groups.append(([(w_in, 2304 + jp * 256, 256, 0), (w_in, 3328 + jp * 256, 256, 256)], 512, 2304 + jp * 512))
            groups += [([(wo_d, 0, 512, 0)], 512, 4352), ([(wo_d, 512, 512, 0)], 512, 4864)]
            for gi_, (pieces, n_, d0_) in enumerate(groups):
                i_ = gi_ % 2
                for (src_, c0_, pn_, po_) in pieces:
                    dma("pool", lambda e, i_=i_, src_=src_, c0_=c0_, pn_=pn_, po_=po_, n_=n_: e.dma_start(
                        out=wview(i_, n_)[:, :, po_:po_ + pn_], in_=src_[:, c0_:c0_ + pn_].rearrange("(k p) n -> p k n", p=128)), writes=[r_wbuf[i_]])
                dma("sp", lambda e, i_=i_, n_=n_, d0_=d0_: e.dma_start(out=winb_d[:, 8 * d0_:8 * d0_ + 8 * n_], in_=wbuf[i_][:, 0:8 * n_]),
                    reads=[r_wbuf[i_]], writes=[r_winb_g.setdefault(d0_, Res(f"winb{d0_}"))])
            pending_wout = []
            def wout_section(T0):
                woh = [load_w(4352, 512, conv=False), load_w(4864, 512, conv=False)]
                for tt in range(4):
                    zi = tt % 2
                    dma("sp", lambda e, zi=zi, tt=tt, T0=T0: e.dma_start(out=xres[zi][:], in_=x_tok[T0 + tt * 128:T0 + (tt + 1) * 128, :]),
                        writes=[r_xres[zi]])
                    for hh in range(2):
                        ps, rp = nxt("S")
                        for k in range(8):
                            op("pe", lambda e, ps=ps, k=k, tt=tt, hh=hh, woh=woh: e.matmul(ps[:], lhsT=mT[:, k, tt * 128:(tt + 1) * 128],
                                                                                  rhs=woh[hh][0][:, k, 0:512], start=(k == 0), stop=(k == 7)),
                               reads=[r_mT, woh[hh][1]], writes=[rp])
                        op("dve", lambda e, ps=ps, zi=zi, hh=hh: e.scalar_tensor_tensor(
                            out=z[zi][:, hh * 512:(hh + 1) * 512], in0=xres[zi][:, hh * 512:(hh + 1) * 512], scalar=ALPHA, in1=ps[:],
                            op0=ALU.mult, op1=ALU.add), reads=[rp, r_xres[zi]], writes=[r_xres[zi]])
                    layer_norm(z[zi], r_z[zi], zi)
                    op("pool", lambda e, zi=zi: e.tensor_tensor(out=z[zi][:], in0=z[zi][:], in1=ln_t[:, 0, :], op=ALU.mult),
                       reads=[r_z[zi], r_ln], writes=[r_z[zi]])
                    op("pool", lambda e, zi=zi: e.tensor_tensor(out=z[zi][:], in0=z[zi][:], in1=ln_t[:, 1, :], op=ALU.add),
                       reads=[r_z[zi], r_ln], writes=[r_z[zi]])
                    dma("pool", lambda e, zi=zi, tt=tt, T0=T0: e.dma_start(out=x1_d[T0 + tt * 128:T0 + (tt + 1) * 128, :], in_=z[zi][:]),
                        reads=[r_z[zi]], writes=[], sem_res=r_x1s[zi])

            try:
              chk(1)
              for s in range(NSEQ):
                for c in range(NCH):
                    T0 = s * S + c * 512
                    gidx = s * NCH + c
                    xi = gidx % 2
                    xt, r_xt = xTc[xi], r_xTc[xi]

                    def load_xT(g_):
                        b_ = xTc[g_ % 2]
                        t0_ = g_ * 512
                        dma("pool", lambda e: e.dma_start(out=b_[:], in_=x_T[:, t0_:t0_ + 512].rearrange("(k p) n -> p k n", p=128)),
                            writes=[r_xTc[g_ % 2]])

                    if gidx == 0:
                        load_xT(0)
                    wA, r_wA = load_w(0, 768, conv=False)
                    for m in range(5):
                        ps, rp = nxt("S")
                        for k in range(8):
                            op("pe", lambda e, ps=ps, k=k, m=m, wA=wA, xt=xt: e.matmul(
                                ps[:], lhsT=wA[:, k, m * 128:(m + 1) * 128], rhs=xt[:, k, :], start=(k == 0), stop=(k == 7)),
                               reads=[r_wA, r_xt], writes=[rp])
                        if m < 4:
                            op("act", lambda e, ps=ps, m=m: e.activation(out=QsT[:, m, :], in_=ps[:], func=AF.Identity,
                                                                         bias=bqk_t[:, m:m + 1], scale=0.125),
                               reads=[rp, r_bqk], writes=[r_Qs])
                        else:
                            op("act", lambda e, ps=ps, c=c: e.activation(out=KsT[:, (c % 2) * 512:(c % 2 + 1) * 512], in_=ps[:], func=AF.Identity,
                                                                         bias=bqk_t[:, 4:5], scale=1.0),
                               reads=[rp, r_bqk], writes=[r_Ks[c % 2]])
                    for tt in range(4):
                        ps, rp = nxt("S")
                        for k in range(8):
                            op("pe", lambda e, ps=ps, k=k, tt=tt, wA=wA, xt=xt: e.matmul(
                                ps[:, 0:128], lhsT=xt[:, k, tt * 128:(tt + 1) * 128], rhs=wA[:, k, 640:768], start=(k == 0), stop=(k == 7)),
                               reads=[r_wA, r_xt], writes=[rp])
                        op("dve", lambda e, ps=ps, tt=tt, c=c: e.tensor_tensor(
                            out=Vs[:, (c % 2) * 4 + tt, :, 0:64], in0=ps[:, 0:128].rearrange("p (g d) -> p g d", g=2),
                            in1=bv_t[:, 0:128].rearrange("p (g d) -> p g d", g=2), op=ALU.add),
                           reads=[rp, r_bv], writes=[r_Vs[c % 2]])
                    wC, r_wC = load_w(768, 512, conv=False)
                    for m in range(4):
                        ps, rp = nxt("S")
                        for k in range(8):
                            op("pe", lambda e, ps=ps, k=k, m=m, wC=wC, xt=xt: e.matmul(
                                ps[:], lhsT=wC[:, k, m * 128:(m + 1) * 128], rhs=xt[:, k, :], start=(k == 0), stop=(k == 7)),
                               reads=[r_wC, r_xt], writes=[rp])
                        op("act", lambda e, ps=ps, m=m: e.activation(out=QmT[0:64, 2 * m, :], in_=ps[0:64, :], func=AF.Identity,
                                                                     bias=bqk_t[0:64, 5 + m:6 + m], scale=0.125),
                           reads=[rp, r_bqk], writes=[r_Qm])
                        op("act", lambda e, ps=ps, m=m: e.activation(out=QmT[64:128, 2 * m + 1, :], in_=ps[64:128, :], func=AF.Identity,
                                                                     bias=bqk_t[64:128, 5 + m:6 + m], scale=0.125),
                           reads=[rp, r_bqk], writes=[r_Qm])
                    wD, r_wD = load_w(1280, 512)
                    for m in range(4):
                        ps, rp = nxt("S")
                        for k in range(8):
                            op("pe", lambda e, ps=ps, k=k, m=m, wD=wD, xt=xt: e.matmul(
                                ps[:], lhsT=wD[:, k, m * 128:(m + 1) * 128], rhs=xt[:, k, :], start=(k == 0), stop=(k == 7)),
                               reads=[r_wD, r_xt], writes=[rp])
                        op("act", lambda e, ps=ps, m=m, c=c: e.activation(out=KmT[:, m, c * 512:(c + 1) * 512], in_=ps[:], func=AF.Identity,
                                                                          bias=bqk_t[:, 9 + m:10 + m], scale=1.0),
                           reads=[rp, r_bqk], writes=[r_Km[c]])
                    wE, r_wE = load_w(1792, 512)
                    for tt in range(4):
                        ps, rp = nxt("S")
                        for k in range(8):
                            op("pe", lambda e, ps=ps, k=k, tt=tt, wE=wE, xt=xt: e.matmul(
                                ps[:], lhsT=xt[:, k, tt * 128:(tt + 1) * 128], rhs=wE[:, k, 0:512], start=(k == 0), stop=(k == 7)),
                               reads=[r_wE, r_xt], writes=[rp])
                        op("dve", lambda e, ps=ps, tt=tt, c=c: e.tensor_tensor(
                            out=Vm[:, c * 4 + tt, :, 0:64], in0=ps[:].rearrange("p (g d) -> p g d", g=8),
                            in1=bv_t[:, 128:640].rearrange("p (g d) -> p g d", g=8), op=ALU.add),
                           reads=[rp, r_bv], writes=[r_Vm[c]])
                    chk(2)
                    op("dve", lambda e, c=c: e.tensor_reduce(out=kms[:], in_=KmT[:, :, c * 512:(c + 1) * 512].rearrange("p m (b t) -> p m b t", b=2),
                                                             axis=AX.X, op=ALU.add), reads=[r_Km[c]], writes=[r_kms])
                    op("dve", lambda e, c=c: e.tensor_scalar(out=kmT[:, :, 2 * c:2 * c + 2], in0=kms[:], scalar1=1.0 / 256.0, scalar2=None,
                                                             op0=ALU.mult), reads=[r_kms], writes=[r_km])
                    while pending_wout:
                        wout_section(pending_wout.pop(0))
                    if gidx + 1 < NSEQ * NCH:
                        load_xT(gidx + 1)
                    chk(3)
                    for tt in range(4):
                        qb = 2 * c + tt // 2
                        pse, rpe = nxt("S")
                        for h in range(8):
                            op("pe", lambda e, pse=pse, h=h, tt=tt: e.matmul(
                                pse[:, h * 16:(h + 1) * 16], lhsT=QmT[:, h, tt * 128:(tt + 1) * 128],
                                rhs=kmT[:, h // 2, :], start=True, stop=True),
                               reads=[r_Qm, r_km], writes=[rpe])
                        chk(31)
                        op("dve", lambda e, pse=pse, qb=qb: e.tensor_tensor(
                            out=gm[:], in0=pse[:, 0:128].rearrange("p (h n) -> p h n", h=8), in1=PMr[:, :, 16 - qb:32 - qb], op=ALU.add),
                           reads=[rpe, r_PMr], writes=[r_gm])
                        chk(32)
                        for h in range(8):
                            op("dve", lambda e, h=h: e.max(out=mx[:, h, :], in_=gm[:, h, :]), reads=[r_gm], writes=[r_mx])
                        chk(33)
                        op("dve", lambda e: e.tensor_scalar(out=thr[:], in0=mx[:, :, 2], scalar1=-1.0e30, scalar2=None, op0=ALU.max),
                           reads=[r_mx], writes=[r_thr])
                        chk(34)
                        op("dve", lambda e: e.tensor_tensor(out=sel[:], in0=gm[:], in1=thr[:].unsqueeze(2).to_broadcast([128, 8, 16]), op=ALU.is_ge),
                           reads=[r_gm, r_thr], writes=[r_sel])
                        op("dve", lambda e, qb=qb: e.tensor_tensor(out=sel[:], in0=sel[:], in1=OWr[:, :, 16 - qb:32 - qb], op=ALU.add),
                           reads=[r_sel, r_OWr], writes=[r_sel])
                        op("dve", lambda e: e.tensor_scalar(out=sel[:], in0=sel[:], scalar1=-1.0, scalar2=BIG, op0=ALU.add, op1=ALU.mult),
                           reads=[r_sel], writes=[r_sel])
                        op("dve", lambda e, tt=tt: e.tensor_tensor(out=madd4[tt][:].rearrange("p (h n) -> p h n", h=8), in0=sel[:],
                                                                   in1=t31_t[:].unsqueeze(2).to_broadcast([128, 8, 16]), op=ALU.add),
                           reads=[r_sel, r_t31], writes=[r_madd4[tt]])
                    chk(5)
                    items = []
                    for tt in range(4):
                        b = c * 4 + tt
                        for g in range(2):
                            whichs = [0] if b == 0 else [0, 1]
                            for wi, which in enumerate(whichs):
                                items.append((tt, g, wi, which, len(whichs), b))
                    obank = {}

                    def swa_S(it):
                        tt, g, wi, which, nw, b = it
                        if wi == 0:
                            obank[(tt, g)] = nxt("O")
                        kt = b - which
                        kc0 = ((kt // 4) % 2) * 512 + (kt % 4) * 128
                        ps, rp = nxt("S")
                        op("pe", lambda e, ps=ps, g=g, kc0=kc0, tt=tt: e.matmul(
                            ps[:], lhsT=KsT[g * 64:(g + 1) * 64, kc0:kc0 + 128],
                            rhs=QsT[g * 64:(g + 1) * 64, :, tt * 128:(tt + 1) * 128], start=True, stop=False),
                           reads=[r_Ks[(kt // 4) % 2], r_Qs], writes=[rp])
                        op("pe", lambda e, ps=ps, g=g, which=which: e.matmul(
                            ps[:], lhsT=ident[:], rhs=Gs[:, which, g * 4:(g + 1) * 4, :], start=False, stop=True),
                           reads=[r_ident, r_Gs], writes=[rp])
                        pi = pctr[0] % 3
                        pctr[0] += 1
                        op("act", lambda e, ps=ps, pi=pi: e.activation(out=PT[pi][:], in_=ps[:], func=AF.Exp),
                           reads=[rp], writes=[r_PT[pi]])
                        return pi

                    def swa_PV(it, pi):
                        tt, g, wi, which, nw, b = it
                        po, rpo = obank[(tt, g)]
                        kt = b - which
                        for j in range(4):
                            first = (wi == 0 and j == 0)
                            op("pe", lambda e, po=po, pi=pi, j=j, kt=kt, g=g, first=first, wi=wi, nw=nw: e.matmul(
                                po[:, j * 65:(j + 1) * 65], lhsT=PT[pi][:, j * 128:(j + 1) * 128], rhs=Vs[:, kt % 8, g, :],
                                start=first, stop=(wi == nw - 1), skip_group_check=True),
                               reads=[r_PT[pi], r_Vs[(kt // 4) % 2]], writes=[rpo])
                        if wi == nw - 1:
                            pov = po[:, 0:260].rearrange("p (t d) -> p t d", t=4)
                            op("dve", lambda e, pov=pov, g=g: e.tensor_tensor(out=rden[:], in0=pov[:, :, 64], in1=es_t[:, g * 4:(g + 1) * 4], op=ALU.add),
                               reads=[rpo, r_es], writes=[r_rden])
                            op("dve", lambda e: e.reciprocal(out=rden[:], in_=rden[:]), reads=[r_rden], writes=[r_rden])
                            op("dve", lambda e, pov=pov, g=g, tt=tt: e.tensor_tensor(
                                out=ytok[:, tt, g * 256:(g + 1) * 256].rearrange("p (j d) -> p j d", j=4), in0=pov[:, :, 0:64],
                                in1=rden[:].unsqueeze(2).to_broadcast([128, 4, 64]), op=ALU.mult),
                               reads=[rpo, r_rden], writes=[r_ytok])

                    prev = None
                    for it in items:
                        pi = swa_S(it)
                        if prev is not None:
                            swa_PV(*prev)
                        prev = (it, pi)
                    swa_PV(*prev)
                    for tt in range(4):
                        pt, rpt = nxt("T")
                        op("pe", lambda e, pt=pt, tt=tt: e.transpose(pt[:, 0:128], madd4[tt][:], ident[:]), reads=[r_madd4[tt], r_ident], writes=[rpt])
                        op("act", lambda e, pt=pt, tt=tt: e.activation(out=maddT[:, tt * 128:(tt + 1) * 128], in_=pt[:, 0:128], func=AF.Copy),
                           reads=[rpt], writes=[r_maddT])
                    chk(4)
                    nkt = 4 * c + 4
                    for h in range(8):
                        hb = (h % 2) * 64
                        po, rpo = nxt("O")

                        def emit_S(kt, h=h, hb=hb):
                            rel = kt - 4 * c
                            n = kt // 2
                            ps, rp = nxt("S")
                            last = rel < -1
                            op("pe", lambda e, ps=ps, kt=kt: e.matmul(
                                ps[:], lhsT=KmT[:, h // 2, kt * 128:(kt + 1) * 128], rhs=QmT[:, h, :],
                                start=True, stop=False), reads=[r_Km[kt // 4], r_Qm], writes=[rp])
                            p = h * 16 + n
                            op("pe", lambda e, ps=ps, p=p, last=last: e.matmul(
                                ps[:], lhsT=ident[:, p:p + 1].to_broadcast([128, 128]), rhs=maddT[:], start=False, stop=last),
                               reads=[r_ident, r_maddT], writes=[rp])
                            if not last:
                                off = 384 - 128 * rel
                                wid = min(512, 640 - off)
                                op("pe", lambda e, ps=ps, off=off, wid=wid: e.matmul(
                                    ps[:, 0:wid], lhsT=ident[:], rhs=Gb[:, h, off:off + wid], start=False, stop=True, skip_group_check=True),
                                   reads=[r_ident, r_Gb], writes=[rp])
                            pi = pctr[0] % 3
                            pctr[0] += 1
                            op("act", lambda e, ps=ps, pi=pi: e.activation(out=PT[pi][:], in_=ps[:], func=AF.Exp),
                               reads=[rp], writes=[r_PT[pi]])
                            return pi

                        def emit_PV(kt, pi, h=h, po=po, rpo=rpo):
                            for tt in range(4):
                                first = (kt == 0 and tt == 0)
                                op("pe", lambda e, pi=pi, tt=tt, kt=kt, first=first, nkt=nkt: e.matmul(
                                    po[:, tt * 65:(tt + 1) * 65], lhsT=PT[pi][:, tt * 128:(tt + 1) * 128], rhs=Vm[:, kt, h, :],
                                    start=first, stop=(kt == nkt - 1), skip_group_check=True),
                                   reads=[r_PT[pi], r_Vm[kt // 4]], writes=[rpo])

                        conv_step()
                        pend = []
                        for kt in range(nkt):
                            pi = emit_S(kt)
                            pend.append((kt, pi))
                            if len(pend) > 2:
                                emit_PV(*pend.pop(0))
                        while pend:
                            emit_PV(*pend.pop(0))
                        pov = po[:, 0:260].rearrange("p (t d) -> p t d", t=4)
                        op("dve", lambda e, pov=pov: e.reciprocal(out=rden[:], in_=pov[:, :, 64]), reads=[rpo], writes=[r_rden])
                        op("dve", lambda e, pov=pov, h=h: e.tensor_tensor(
                            out=ytok[:, :, 512 + h * 64:512 + (h + 1) * 64], in0=pov[:, :, 0:64],
                            in1=rden[:].unsqueeze(2).to_broadcast([128, 4, 64]), op=ALU.mult),
                           reads=[rpo, r_rden], writes=[r_ytok])
                    chk(6)
                    for j in range(8):
                        pt, rpt = nxt("T")
                        for tt in range(4):
                            op("pe", lambda e, pt=pt, tt=tt, j=j: e.transpose(pt[:, tt * 128:(tt + 1) * 128], ytok[:, tt, j * 128:(j + 1) * 128], ident[:]),
                               reads=[r_ytok, r_ident], writes=[rpt])
                        op("act", lambda e, pt=pt, j=j: e.activation(out=yT[:, j, :], in_=pt[:, 0:512], func=AF.Copy),
                           reads=[rpt], writes=[r_yT])
                    chk(7)
                    for j in range(8):
                        if j % 2 == 0:
                            wF1, r_wF1 = load_w(2304 + (j // 2) * 512, 512)
                            wF2, r_wF2 = wF1, r_wF1
                        jo = (j % 2) * 128
                        pa, rpa = nxt("S")
                        for k in range(4):
                            op("pe", lambda e, pa=pa, k=k, j=j: e.matmul(pa[:], lhsT=wa[:, k, j * 128:(j + 1) * 128], rhs=yT[:, k, :],
                                                                         start=(k == 0), stop=(k == 3)), reads=[r_wa, r_yT], writes=[rpa])
                        pb, rpb = nxt("S")
                        for k in range(4):
                            op("pe", lambda e, pb=pb, k=k, j=j: e.matmul(pb[:], lhsT=wb[:, k, j * 128:(j + 1) * 128], rhs=yT[:, 4 + k, :],
                                                                         start=(k == 0), stop=(k == 3)), reads=[r_wb, r_yT], writes=[rpb])
                        pg1, rpg1 = nxt("S")
                        for k in range(8):
                            op("pe", lambda e, pg1=pg1, k=k, jo=jo, wF1=wF1, xt=xt: e.matmul(pg1[:], lhsT=wF1[:, k, jo:jo + 128], rhs=xt[:, k, :],
                                                                                           start=(k == 0), stop=(k == 7)), reads=[r_wF1, r_xt], writes=[rpg1])
                        op("act", lambda e, pg1=pg1, j=j: e.activation(out=g1[:], in_=pg1[:], func=AF.Sigmoid, bias=bg_t[:, j:j + 1], scale=1.0),
                           reads=[rpg1, r_bg], writes=[r_g1])
                        pg2, rpg2 = nxt("S")
                        for k in range(8):
                            op("pe", lambda e, pg2=pg2, k=k, jo=jo, wF2=wF2, xt=xt: e.matmul(pg2[:], lhsT=wF2[:, k, 256 + jo:256 + jo + 128], rhs=xt[:, k, :],
                                                                                           start=(k == 0), stop=(k == 7)), reads=[r_wF2, r_xt], writes=[rpg2])
                        op("act", lambda e, pg2=pg2, j=j: e.activation(out=g2[:], in_=pg2[:], func=AF.Sigmoid, bias=bg_t[:, 8 + j:9 + j], scale=1.0),
                           reads=[rpg2, r_bg], writes=[r_g2])
                        op("dve", lambda e, pa=pa: e.tensor_tensor(out=t1[:], in0=pa[:], in1=g1[:], op=ALU.mult), reads=[rpa, r_g1], writes=[r_t1])
                        op("dve", lambda e, pb=pb: e.tensor_tensor(out=t2[:], in0=pb[:], in1=g2[:], op=ALU.mult), reads=[rpb, r_g2], writes=[r_t2])
                        op("pool", lambda e, j=j: e.tensor_tensor(out=mT[:, j, :], in0=t1[:], in1=t2[:], op=ALU.add), reads=[r_t1, r_t2], writes=[r_mT])
                    chk(8)
                    pending_wout.append(T0)
            except StopBuild:
                pass
            while pending_wout:
                wout_section(pending_wout.pop(0))
            while conv_state["next"] < len(conv_list) or conv_state["pending"] is not None:
                conv_step()
                conv_flush()
            S_.barrier()
            with nc.Block() as blk:
                S_.emit(blk)
            if int(os.environ.get('KSTOP', '99')) < 20:
                return nc

        with ExitStack() as st:
            sb = lambda name, shape, ty: st.enter_context(nc.sbuf_tensor(name, shape, ty))
            CH = 1024
            NMC = NT // CH
            ident = sb("ident2", [128, 128], BF16); r_ident = Res("ident2")
            identf = sb("identf2", [128, 128], F32); r_identf = Res("identf2")
            op("pool", lambda e: e.memset(identf[:], 0.0), writes=[r_identf])
            op("pool", lambda e: e.affine_select(out=identf[:], in_=identf[:], pattern=[[-1, 128]],
                                                 compare_op=ALU.not_equal, fill=1.0, base=0,
                                                 channel_multiplier=1), reads=[r_identf], writes=[r_identf])
            op("dve", lambda e: e.tensor_copy(out=ident[:], in_=identf[:]), reads=[r_identf], writes=[r_ident])
            ln_t = sb("ln2_t", [128, 2, D], F32); r_ln = Res("ln2")
            for i in range(2):
                dma("sp", lambda e, i=i: e.dma_start(out=ln_t[:, i, :], in_=ln_d[2 + i:3 + i, :].partition_broadcast(128)), writes=[r_ln])
            wr = sb("wr_t", [128, 8, 36], BF16); r_wr = Res("wr")
            br = sb("br_t", [128, 36], F32); r_br = Res("br")
            dma("pool", lambda e: e.dma_start(out=wr[:], in_=wr_d.rearrange("(k p) n -> p k n", p=128)), writes=[r_wr])
            dma("sp", lambda e: e.dma_start(out=br[:], in_=br_d.partition_broadcast(128)), writes=[r_br])
            I32 = mybir.dt.int32
            BLK = 256
            NTILE = NT // 128
            NB = (2 * NT) // BLK + NEXP
            CAP = NB * BLK
            wgb2 = wgb_d.rearrange("e p n -> (e p) n")
            wub2 = wub_d.rearrange("e p n -> (e p) n")
            wdb2 = wdb_d.rearrange("e p n -> (e p) n")
            r_xbuf = Res("xbuf"); r_ybuf = Res("ybuf")
            U = sb("U", [128, 128], BF16); r_U = Res("U")
            Uf = identf
            op("pool", lambda e: e.memset(Uf[:], 1.0), reads=[r_identf], writes=[r_identf])
            op("pool", lambda e: e.affine_select(out=Uf[:], in_=Uf[:], pattern=[[1, 128]], compare_op=ALU.is_gt, fill=0.0,
                                                 base=0, channel_multiplier=-1), reads=[r_identf], writes=[r_identf])
            op("dve", lambda e: e.tensor_copy(out=U[:], in_=Uf[:]), reads=[r_identf], writes=[r_U])
            ones = sb("ones", [128, 128], BF16); r_ones = Res("ones")
            op("pool", lambda e: e.memset(ones[:], 1.0), writes=[r_ones])
            pci = sb("pci", [128, 1], I32); r_pci = Res("pci")
            pcf = sb("pcf", [128, 2], F32); r_pcf = Res("pcf")
            op("pool", lambda e: e.iota(pci[:], pattern=[[0, 1]], base=0, channel_multiplier=1), writes=[r_pci])
            op("dve", lambda e: e.tensor_copy(out=pcf[:, 0:1], in_=pci[:]), reads=[r_pci], writes=[r_pcf])
            op("dve", lambda e: e.tensor_scalar(out=pcf[:, 1:2], in0=pcf[:, 0:1], scalar1=float(BLK), scalar2=None, op0=ALU.mult),
               reads=[r_pcf], writes=[r_pcf])
            bvi = sb("bvi", [128, NB], I32); r_bvi = Res("bvi")
            bvf = sb("bvf", [128, NB], F32); r_bvf = Res("bvf")
            op("pool", lambda e: e.iota(bvi[:], pattern=[[1, NB]], base=0, channel_multiplier=0), writes=[r_bvi])
            op("dve", lambda e: e.tensor_copy(out=bvf[:], in_=bvi[:]), reads=[r_bvi], writes=[r_bvf])
            oh1 = sb("oh1", [128, NTILE, 32], BF16); r_oh1 = Res("oh1")
            oh2 = sb("oh2", [128, NTILE, 32], BF16); r_oh2 = Res("oh2")
            posAB = sb("posAB", [128, NTILE, 2], F32); r_pos = Res("posAB")
            wAB = sb("wAB", [128, NTILE, 2], F32); r_wAB = Res("wAB")
            idxAB = sb("idxAB", [128, NTILE, 2], I32); r_idx = Res("idxAB")
            base = sb("base", [128, 32], F32); r_base = Res("base")
            op("pool", lambda e: e.memset(base[:], 0.0), writes=[r_base])
            hT = [sb(f"hT{i}", [128, 8, 128], BF16) for i in range(2)]
            r_hT = [Res(f"hT{i}") for i in range(2)]
            xbr = [sb(f"xbr{i}", [128, D], BF16) for i in range(2)]
            r_xbr = [Res(f"xbr{i}") for i in range(2)]
            G = min(16, NTILE)
            lgall = sb("lgall", [128, NTILE, 36], F32); r_lgall = [Res(f"lgall{g}") for g in range(NTILE // G)]
            Mball = sb("Mball", [128, NTILE, 32], BF16); r_Mball = [Res(f"Mball{g}") for g in range(NTILE // G)]
            gmx = sb("gmx", [128, G], F32); r_gmx = Res("gmx")
            ohg = sb("ohg", [128, G, 4], F32); r_ohg = Res("ohg")
            gxe = sb("gxe", [128, G, 4], F32); r_gxe = Res("gxe")
            gpr = sb("gpr", [128, G], F32); r_gpr = Res("gpr")
            emb = sb("emb", [128, G, 32], F32); r_emb = Res("emb")
            em2 = sb("em2", [128, G, 32], F32); r_em2 = Res("em2")
            s1b = sb("s1b", [128, G, 32], F32); r_s1b = Res("s1b")
            s2b = sb("s2b", [128, G, 32], F32); r_s2b = Res("s2b")
            m12 = sb("m12", [128, 2, G], F32); r_m12 = Res("m12")
            ww = sb("ww", [128, 3, G], F32); r_ww = Res("ww")
            pos = sb("pos", [128, 32], F32); r_posf = Res("posf")
            tmp = sb("tmp", [128, 32], F32); r_tmp = Res("tmp")
            for ti in range(NTILE):
                xi_ = ti % 2
                h_, r_h = hT[xi_], r_hT[xi_]
                dma("pool", lambda e, ti=ti, xi_=xi_: e.dma_start(out=xbr[xi_][:], in_=x1_d[ti * 128:(ti + 1) * 128, :]), writes=[r_xbr[xi_]])
                pt, rpt = nxt("T")
                for j in range(8):
                    op("pe", lambda e, pt=pt, j=j, xi_=xi_: e.transpose(pt[:, j * 128:(j + 1) * 128], xbr[xi_][:, j * 128:(j + 1) * 128], ident[:]),
                       reads=[r_xbr[xi_], r_ident], writes=[rpt])
                op("act", lambda e, pt=pt, h_=h_: e.activation(out=h_[:], in_=pt[:].rearrange("p (j t) -> p j t", j=8), func=AF.Copy),
                   reads=[rpt], writes=[r_h])
                ps, rp = nxt("S")
                for k in range(8):
                    op("pe", lambda e, ps=ps, k=k, h_=h_: e.matmul(ps[:, 0:36], lhsT=h_[:, k, :], rhs=wr[:, k, :],
                                                                   start=(k == 0), stop=(k == 7)), reads=[r_h, r_wr], writes=[rp])
                op("dve", lambda e, ps=ps, ti=ti: e.tensor_tensor(out=lgall[:, ti, :], in0=ps[:, 0:36], in1=br[:], op=ALU.add),
                   reads=[rp, r_br], writes=[r_lgall[ti // G]])
            for g in range(NTILE // G):
                t0, t1 = g * G, (g + 1) * G
                gl = lgall[:, t0:t1, 0:4]
                el = lgall[:, t0:t1, 4:36].rearrange("p t (g x) -> p t g x", g=4)
                bc3 = lambda ap, n: ap.unsqueeze(2).to_broadcast([128, G, n])
                op("dve", lambda e, gl=gl: e.tensor_reduce(out=gmx[:], in_=gl, axis=AX.X, op=ALU.max), reads=[r_lgall[g]], writes=[r_gmx])
                op("dve", lambda e, gl=gl: e.tensor_tensor(out=ohg[:], in0=gl, in1=bc3(gmx[:], 4), op=ALU.is_ge), reads=[r_lgall[g], r_gmx], writes=[r_ohg])
                op("dve", lambda e, gl=gl: e.tensor_tensor(out=gxe[:], in0=gl, in1=bc3(gmx[:], 4), op=ALU.subtract), reads=[r_lgall[g], r_gmx], writes=[r_gxe])
                op("act", lambda e: e.activation(out=gxe[:], in_=gxe[:], func=AF.Exp), reads=[r_gxe], writes=[r_gxe])
                op("dve", lambda e: e.tensor_scalar(out=ohg[:], in0=ohg[:], scalar1=-1.0, scalar2=1.0e30, op0=ALU.add, op1=ALU.mult), reads=[r_ohg], writes=[r_ohg])
                op("dve", lambda e, el=el: e.tensor_tensor(out=emb[:].rearrange("p t (g x) -> p t g x", g=4), in0=el,
                                                           in1=ohg[:].unsqueeze(3).to_broadcast([128, G, 4, 8]), op=ALU.add),
                   reads=[r_lgall[g], r_ohg], writes=[r_emb])
                op("dve", lambda e: e.tensor_reduce(out=m12[:, 0, :], in_=emb[:], axis=AX.X, op=ALU.max), reads=[r_emb], writes=[r_m12])
                op("dve", lambda e: e.tensor_tensor(out=s1b[:], in0=emb[:], in1=bc3(m12[:, 0, :], 32), op=ALU.is_ge), reads=[r_emb, r_m12], writes=[r_s1b])
                op("dve", lambda e: e.scalar_tensor_tensor(out=em2[:], in0=s1b[:], scalar=-1.0e30, in1=emb[:], op0=ALU.mult, op1=ALU.add),
                   reads=[r_s1b, r_emb], writes=[r_em2])
                op("dve", lambda e: e.tensor_reduce(out=m12[:, 1, :], in_=em2[:], axis=AX.X, op=ALU.max), reads=[r_em2, r_m12], writes=[r_m12])
                op("dve", lambda e: e.tensor_tensor(out=s2b[:], in0=emb[:], in1=bc3(m12[:, 1, :], 32), op=ALU.is_ge), reads=[r_emb, r_m12], writes=[r_s2b])
                op("dve", lambda e, t0=t0, t1=t1: e.tensor_copy(out=Mball[:, t0:t1, :], in_=s2b[:]), reads=[r_s2b], writes=[r_Mball[g]])
                op("dve", lambda e, t0=t0, t1=t1: e.tensor_copy(out=oh1[:, t0:t1, :], in_=s1b[:]), reads=[r_s1b], writes=[r_oh1])
                op("dve", lambda e, t0=t0, t1=t1: e.tensor_tensor(out=oh2[:, t0:t1, :], in0=s2b[:], in1=s1b[:], op=ALU.subtract), reads=[r_s1b, r_s2b], writes=[r_oh2])
                op("dve", lambda e: e.tensor_reduce(out=gpr[:], in_=gxe[:], axis=AX.X, op=ALU.add), reads=[r_gxe], writes=[r_gpr])
                op("dve", lambda e: e.reciprocal(out=gpr[:], in_=gpr[:]), reads=[r_gpr], writes=[r_gpr])
                op("dve", lambda e: e.tensor_tensor(out=ww[:, 0, :], in0=m12[:, 1, :], in1=m12[:, 0, :], op=ALU.subtract), reads=[r_m12], writes=[r_ww])
                op("act", lambda e: e.activation(out=ww[:, 0, :], in_=ww[:, 0, :], func=AF.Exp), reads=[r_ww], writes=[r_ww])
                op("dve", lambda e: e.tensor_scalar(out=ww[:, 1, :], in0=ww[:, 0, :], scalar1=1.0, scalar2=None, op0=ALU.add), reads=[r_ww], writes=[r_ww])
                op("dve", lambda e: e.reciprocal(out=ww[:, 1, :], in_=ww[:, 1, :]), reads=[r_ww], writes=[r_ww])
                op("dve", lambda e: e.tensor_tensor(out=ww[:, 2, :], in0=ww[:, 0, :], in1=ww[:, 1, :], op=ALU.mult), reads=[r_ww], writes=[r_ww])
                op("dve", lambda e, t0=t0, t1=t1: e.tensor_tensor(out=wAB[:, t0:t1, 0], in0=ww[:, 1, :], in1=gpr[:], op=ALU.mult), reads=[r_ww, r_gpr], writes=[r_wAB])
                op("dve", lambda e, t0=t0, t1=t1: e.tensor_tensor(out=wAB[:, t0:t1, 1], in0=ww[:, 2, :], in1=gpr[:], op=ALU.mult), reads=[r_ww, r_gpr], writes=[r_wAB])
            for ti in range(NTILE):
                g = ti // G
                pp, rpp = nxt("S")
                op("pe", lambda e, pp=pp, ti=ti: e.matmul(pp[:, 0:32], lhsT=U[:], rhs=Mball[:, ti, :], start=True, stop=True), reads=[r_U, r_Mball[g]], writes=[rpp])
                op("dve", lambda e, pp=pp: e.tensor_tensor(out=pos[:], in0=pp[:, 0:32], in1=base[:], op=ALU.add), reads=[rpp, r_base], writes=[r_posf])
                pq, rpq = nxt("S")
                op("pe", lambda e, pq=pq, ti=ti: e.matmul(pq[:, 0:32], lhsT=ones[:], rhs=Mball[:, ti, :], start=True, stop=True), reads=[r_ones, r_Mball[g]], writes=[rpq])
                op("dve", lambda e, pq=pq: e.tensor_tensor(out=base[:], in0=pq[:, 0:32], in1=base[:], op=ALU.add), reads=[rpq, r_base], writes=[r_base])
                op("dve", lambda e, ti=ti: e.tensor_tensor(out=tmp[:], in0=pos[:], in1=oh1[:, ti, :], op=ALU.mult), reads=[r_posf, r_oh1], writes=[r_tmp])
                op("dve", lambda e, ti=ti: e.tensor_reduce(out=posAB[:, ti, 0:1], in_=tmp[:], axis=AX.X, op=ALU.add), reads=[r_tmp], writes=[r_pos])
                op("dve", lambda e, ti=ti: e.tensor_tensor(out=tmp[:], in0=pos[:], in1=oh2[:, ti, :], op=ALU.mult), reads=[r_posf, r_oh2], writes=[r_tmp])
                op("dve", lambda e, ti=ti: e.tensor_reduce(out=posAB[:, ti, 1:2], in_=tmp[:], axis=AX.X, op=ALU.add), reads=[r_tmp], writes=[r_pos])
            cmpb = sb("cmpb", [128, 32], BF16); r_cmpb = Res("cmpb")
            nblk = sb("nblk", [128, 32], F32); r_nblk = Res("nblk")
            endb = sb("endb", [128, 32], F32); r_endb = Res("endb")
            sbase = sb("sbase", [128, 32], F32); r_sbase = Res("sbase")
            op("dve", lambda e: e.tensor_scalar(out=cmpb[:], in0=base[:], scalar1=pcf[:, 1:2], scalar2=None, op0=ALU.is_gt),
               reads=[r_base, r_pcf], writes=[r_cmpb])
            pn, rpn = nxt("S")
            op("pe", lambda e: e.matmul(pn[:, 0:32], lhsT=ones[:], rhs=cmpb[:], start=True, stop=True), reads=[r_ones, r_cmpb], writes=[rpn])
            op("dve", lambda e: e.tensor_copy(out=nblk[:], in_=pn[:, 0:32]), reads=[rpn], writes=[r_nblk])
            op("dve", lambda e: e.tensor_copy(out=endb[:, 0:1], in_=nblk[:, 0:1]), reads=[r_nblk], writes=[r_endb])
            for ex in range(1, NEXP):
                op("dve", lambda e, ex=ex: e.tensor_tensor(out=endb[:, ex:ex + 1], in0=endb[:, ex - 1:ex], in1=nblk[:, ex:ex + 1], op=ALU.add),
                   reads=[r_endb, r_nblk], writes=[r_endb])
            op("dve", lambda e: e.tensor_tensor(out=sbase[:], in0=endb[:], in1=nblk[:], op=ALU.subtract), reads=[r_endb, r_nblk], writes=[r_sbase])
            op("dve", lambda e: e.tensor_scalar(out=sbase[:], in0=sbase[:], scalar1=float(BLK), scalar2=None, op0=ALU.mult), reads=[r_sbase], writes=[r_sbase])
            cmp3 = sb("cmp3", [128, NB, 32], F32); r_cmp3 = Res("cmp3")
            bex = sb("bex", [128, NB], F32); r_bex = Res("bex")
            idxw = sb("idxw", [128, NB], I32); r_idxw = Res("idxw")
            op("dve", lambda e: e.tensor_tensor(out=cmp3[:], in0=endb[:].unsqueeze(1).to_broadcast([128, NB, 32]),
                                                in1=bvf[:].unsqueeze(2).to_broadcast([128, NB, 32]), op=ALU.is_le),
               reads=[r_endb, r_bvf], writes=[r_cmp3])
            op("dve", lambda e: e.tensor_reduce(out=bex[:], in_=cmp3[:], axis=AX.X, op=ALU.add), reads=[r_cmp3], writes=[r_bex])
            op("dve", lambda e: e.tensor_scalar(out=bex[:], in0=bex[:], scalar1=float(NEXP - 1), scalar2=128.0, op0=ALU.min, op1=ALU.mult),
               reads=[r_bex], writes=[r_bex])
            op("dve", lambda e: e.tensor_scalar(out=bex[:], in0=bex[:], scalar1=pcf[:, 0:1], scalar2=None, op0=ALU.add), reads=[r_bex, r_pcf], writes=[r_bex])
            op("dve", lambda e: e.tensor_copy(out=idxw[:], in_=bex[:]), reads=[r_bex], writes=[r_idxw])
            tmp3 = sb("tmp3", [128, 32], F32); r_tmp3 = Res("tmp3")
            slf = sb("slf", [128, 2], F32); r_slf = Res("slf")
            xb = [sb(f"xb{i}", [128, D], BF16) for i in range(2)]
            r_xbs = [Res("xbs0"), Res("xbs1")]
            r_xb = [Res(f"xb{i}") for i in range(2)]
            big3 = cmp3[:, 0:NTILE, :]; r_big3 = r_cmp3
            slall = sb("slall", [128, NTILE, 2], F32); r_slall = Res("slall")
            for ab, ohx, r_ohx in ((0, oh1, r_oh1), (1, oh2, r_oh2)):
                op("dve", lambda e, ohx=ohx: e.tensor_tensor(out=big3, in0=ohx[:], in1=sbase[:].unsqueeze(1).to_broadcast([128, NTILE, 32]), op=ALU.mult),
                   reads=[r_ohx, r_sbase], writes=[r_big3])
                op("dve", lambda e, ab=ab: e.tensor_reduce(out=slall[:, :, ab], in_=big3, axis=AX.X, op=ALU.add), reads=[r_big3], writes=[r_slall])
            op("dve", lambda e: e.tensor_tensor(out=slall[:], in0=slall[:], in1=posAB[:], op=ALU.add), reads=[r_slall, r_pos], writes=[r_slall])
            op("dve", lambda e: e.tensor_copy(out=idxAB[:], in_=slall[:]), reads=[r_slall], writes=[r_idx])
            for ti in range(NTILE):
                xi = ti % 2
                if ti == 0:
                    dma("pool", lambda e: e.dma_start(out=xb[0][:], in_=x1_d[0:128, :]), reads=[r_x1], writes=[r_xb[0]])
                if ti + 1 < NTILE:
                    dma("pool", lambda e, ti=ti: e.dma_start(out=xb[(ti + 1) % 2][:], in_=x1_d[(ti + 1) * 128:(ti + 2) * 128, :]),
                        reads=[r_x1], writes=[r_xb[(ti + 1) % 2]])
                for ab in range(2):
                    dma("pool", lambda e, ti=ti, xi=xi, ab=ab: e.indirect_dma_start(
                        out=xbuf_d, out_offset=bass.IndirectOffsetOnAxis(ap=idxAB[:, ti, ab:ab + 1], axis=0), in_=xb[xi][:], in_offset=None),
                        reads=[r_xb[xi], r_idx], writes=[], sem_res=r_xbs[xi])
            fence = sb("fence", [128, D], BF16); r_fence = Res("fence")
            dma("pool", lambda e: e.dma_start(out=fence[:], in_=xbuf_d[CAP - 128:CAP, :]), writes=[r_fence])
            dma("pool", lambda e: e.dma_start(out=fence[:], in_=xbuf_d[0:128, :]), writes=[r_fence])
            S_.barrier()
            wgt = [sb(f"wgt{i}", [128, 8, DE], BF16) for i in range(2)]
            wut = [sb(f"wut{i}", [128, 8, DE], BF16) for i in range(2)]
            wdt = [sb(f"wdt{i}", [128, 4, D], BF16) for i in range(2)]
            r_weg = [Res(f"weg{i}") for i in range(2)]
            r_weu = [Res(f"weu{i}") for i in range(2)]
            r_wed = [Res(f"wed{i}") for i in range(2)]
            xs = [sb(f"xs{i}", [128, 2, D], BF16) for i in range(2)]
            r_xs = [Res(f"xs{i}") for i in range(2)]
            xT = [sb(f"xT{i}", [128, 8, BLK], BF16) for i in range(2)]
            r_xT = [Res(f"xT{i}") for i in range(2)]
            actT = sb("actT", [128, 4, BLK], BF16); r_actT = [Res(f"actT{i}") for i in range(4)]
            sg = [sb(f"sg{i}", [128, BLK], F32) for i in range(2)]
            r_sg = [Res(f"sg{i}") for i in range(2)]
            yb = [sb(f"yb{i}", [128, D], F32) for i in range(2)]
            r_yb = [Res(f"yb{i}") for i in range(2)]
            yctr = [0]
            def load_block(b):
                bi = b % 2
                for (wt_, w2, rw) in ((wgt, wgb2, r_weg), (wut, wub2, r_weu), (wdt, wdb2, r_wed)):
                    dma("pool", lambda e, b=b, bi=bi, wt_=wt_, w2=w2: e.indirect_dma_start(
                        out=wt_[bi][:].rearrange("p k n -> p (k n)"), out_offset=None, in_=w2,
                        in_offset=bass.IndirectOffsetOnAxis(ap=idxw[:, b:b + 1], axis=0)), reads=[r_idxw], writes=[rw[bi]])
                dma("sp", lambda e, b=b, bi=bi: e.dma_start(out=xs[bi][:], in_=xbuf_d[b * BLK:(b + 1) * BLK, :].rearrange("(s p) d -> p s d", p=128)),
                    writes=[r_xs[bi]])

            load_block(0)
            for b in range(NB):
                bi = b % 2
                if b + 1 < NB:
                    load_block(b + 1)
                for st_ in range(2):
                    pt, rpt = nxt("T")
                    for j in range(8):
                        op("pe", lambda e, pt=pt, j=j, st_=st_, bi=bi: e.transpose(pt[:, j * 128:(j + 1) * 128], xs[bi][:, st_, j * 128:(j + 1) * 128], ident[:]),
                           reads=[r_xs[bi], r_ident], writes=[rpt])
                    if st_ == 0:
                        op("act", lambda e, pt=pt, bi=bi, st_=st_: e.activation(out=xT[bi][:, :, st_ * 128:(st_ + 1) * 128],
                                                                               in_=pt[:].rearrange("p (j t) -> p j t", j=8), func=AF.Copy),
                           reads=[rpt], writes=[r_xT[bi]])
                    else:
                        op("dve", lambda e, pt=pt, bi=bi, st_=st_: e.tensor_copy(out=xT[bi][:, :, st_ * 128:(st_ + 1) * 128],
                                                                                in_=pt[:].rearrange("p (j t) -> p j t", j=8)),
                           reads=[rpt], writes=[r_xT[bi]])
                for fc in range(4):
                    pg, rpg = nxt("S")
                    for k in range(8):
                        op("pe", lambda e, pg=pg, k=k, fc=fc, bi=bi: e.matmul(pg[:, 0:BLK], lhsT=wgt[bi][:, k, fc * 128:(fc + 1) * 128], rhs=xT[bi][:, k, :],
                                                                              start=(k == 0), stop=(k == 7)), reads=[r_weg[bi], r_xT[bi]], writes=[rpg])
                    pu, rpu = nxt("S")
                    for k in range(8):
                        op("pe", lambda e, pu=pu, k=k, fc=fc, bi=bi: e.matmul(pu[:, 0:BLK], lhsT=wut[bi][:, k, fc * 128:(fc + 1) * 128], rhs=xT[bi][:, k, :],
                                                                              start=(k == 0), stop=(k == 7)), reads=[r_weu[bi], r_xT[bi]], writes=[rpu])
                    si = fc % 2
                    op("act", lambda e, pg=pg, si=si: e.activation(out=sg[si][:], in_=pg[:, 0:BLK], func=AF.Silu), reads=[rpg], writes=[r_sg[si]])
                    op("dve", lambda e, pu=pu, si=si, fc=fc: e.tensor_tensor(out=actT[:, fc, :], in0=pu[:, 0:BLK], in1=sg[si][:], op=ALU.mult),
                       reads=[rpu, r_sg[si]], writes=[r_actT[fc]])
                for st_ in range(2):
                    yi = yctr[0] % 2
                    yctr[0] += 1
                    for hh in range(2):
                        ps, rp = nxt("O")
                        for fc in range(4):
                            op("pe", lambda e, ps=ps, fc=fc, st_=st_, hh=hh, bi=bi: e.matmul(
                                ps[:], lhsT=actT[:, fc, st_ * 128:(st_ + 1) * 128], rhs=wdt[bi][:, fc, hh * 512:(hh + 1) * 512],
                                start=(fc == 0), stop=(fc == 3)), reads=[r_actT[fc], r_wed[bi]], writes=[rp])
                        if hh == 0:
                            op("act", lambda e, ps=ps, yi=yi, hh=hh: e.activation(out=yb[yi][:, hh * 512:(hh + 1) * 512], in_=ps[:], func=AF.Copy),
                               reads=[rp], writes=[r_yb[yi]])
                        else:
                            op("dve", lambda e, ps=ps, yi=yi, hh=hh: e.tensor_copy(out=yb[yi][:, hh * 512:(hh + 1) * 512], in_=ps[:]),
                               reads=[rp], writes=[r_yb[yi]])
                    dma("sp", lambda e, b=b, st_=st_, yi=yi: e.dma_start(out=ybuf_d[b * BLK + st_ * 128:b * BLK + (st_ + 1) * 128, :], in_=yb[yi][:]),
                        reads=[r_yb[yi]], writes=[], sem_res=r_ybuf)
            S_.barrier()
            yA = [sb(f"yA{i}", [128, D], F32) for i in range(4)]
            yB = [sb(f"yB{i}", [128, D], F32) for i in range(4)]
            r_yA = [Res(f"yA{i}") for i in range(4)]
            r_yB = [Res(f"yB{i}") for i in range(4)]
            xr = [sb(f"xr{i}", [128, D], F32) for i in range(4)]
            r_xr = [Res(f"xr{i}") for i in range(4)]
            stats_l = [sb(f"stats2{i}", [128, 2, 6], F32) for i in range(4)]; r_stats_l = [Res(f"stats2{i}") for i in range(4)]
            mv_l = [sb(f"mv2{i}", [128, 2], F32) for i in range(4)]; r_mv_l = [Res(f"mv2{i}") for i in range(4)]
            rstd_l = [sb(f"rstd2{i}", [128, 1], F32) for i in range(4)]; r_rstd_l = [Res(f"rstd2{i}") for i in range(4)]
            def combine_fetch(ti):
                xi = ti % 4
                dma("pool", lambda e, ti=ti, xi=xi: e.indirect_dma_start(out=yA[xi][:], out_offset=None, in_=ybuf_d,
                                                                        in_offset=bass.IndirectOffsetOnAxis(ap=idxAB[:, ti, 0:1], axis=0)),
                    reads=[r_idx], writes=[r_yA[xi]])
                dma("pool", lambda e, ti=ti, xi=xi: e.indirect_dma_start(out=yB[xi][:], out_offset=None, in_=ybuf_d,
                                                                        in_offset=bass.IndirectOffsetOnAxis(ap=idxAB[:, ti, 1:2], axis=0)),
                    reads=[r_idx], writes=[r_yB[xi]])
                dma("sp", lambda e, xi=xi, ti=ti: e.dma_start(out=xr[xi][:], in_=x1_d[ti * 128:(ti + 1) * 128, :]), reads=[r_x1], writes=[r_xr[xi]])

            def cvars(ti):
                xi = ti % 4
                return xi, stats_l[xi], r_stats_l[xi], mv_l[xi], r_mv_l[xi], rstd_l[xi], r_rstd_l[xi]

            def combine_A(ti):
                xi, stats, r_stats, mv, r_mv, rstd, r_rstd = cvars(ti)
                op("act", lambda e, xi=xi: e.activation(out=xr[xi][:], in_=xr[xi][:], func=AF.Copy, scale=ALPHA), reads=[r_xr[xi]], writes=[r_xr[xi]])
                op("dve", lambda e, xi=xi, ti=ti: e.scalar_tensor_tensor(out=xr[xi][:], in0=yA[xi][:], scalar=wAB[:, ti, 0:1], in1=xr[xi][:],
                                                                         op0=ALU.mult, op1=ALU.add), reads=[r_yA[xi], r_wAB, r_xr[xi]], writes=[r_xr[xi]])
                op("dve", lambda e, xi=xi, ti=ti: e.scalar_tensor_tensor(out=xr[xi][:], in0=yB[xi][:], scalar=wAB[:, ti, 1:2], in1=xr[xi][:],
                                                                         op0=ALU.mult, op1=ALU.add), reads=[r_yB[xi], r_wAB, r_xr[xi]], writes=[r_xr[xi]])
                for hh in range(2):
                    op("dve", lambda e, hh=hh, xi=xi, stats=stats: e.bn_stats(out=stats[:, hh, :], in_=xr[xi][:, hh * 512:(hh + 1) * 512]),
                       reads=[r_xr[xi]], writes=[r_stats])
                op("dve", lambda e, stats=stats, mv=mv: e.bn_aggr(out=mv[:], in_=stats[:].rearrange("p a b -> p (a b)")), reads=[r_stats], writes=[r_mv])
                op("act", lambda e, mv=mv, rstd=rstd: e.activation(out=rstd[:], in_=mv[:, 1:2], func=AF.Sqrt, bias=EPS, scale=1.0), reads=[r_mv], writes=[r_rstd])

            def combine_B(ti):
                xi, stats, r_stats, mv, r_mv, rstd, r_rstd = cvars(ti)
                op("dve", lambda e, rstd=rstd: e.reciprocal(out=rstd[:], in_=rstd[:]), reads=[r_rstd], writes=[r_rstd])
                op("dve", lambda e, mv=mv, rstd=rstd: e.tensor_scalar(out=mv[:, 1:2], in0=mv[:, 0:1], scalar1=rstd[:, 0:1], scalar2=-1.0,
                                                                     op0=ALU.mult, op1=ALU.mult), reads=[r_mv, r_rstd], writes=[r_mv])
                op("act", lambda e, xi=xi, mv=mv, rstd=rstd: e.activation(out=xr[xi][:], in_=xr[xi][:], func=AF.Identity, bias=mv[:, 1:2], scale=rstd[:, 0:1]),
                   reads=[r_xr[xi], r_mv, r_rstd], writes=[r_xr[xi]])

            def combine_C(ti):
                xi, stats, r_stats, mv, r_mv, rstd, r_rstd = cvars(ti)
                op("dve", lambda e, xi=xi: e.tensor_tensor(out=xr[xi][:], in0=xr[xi][:], in1=ln_t[:, 0, :], op=ALU.mult),
                   reads=[r_xr[xi], r_ln], writes=[r_xr[xi]])
                op("dve", lambda e, xi=xi: e.tensor_tensor(out=xr[xi][:], in0=xr[xi][:], in1=ln_t[:, 1, :], op=ALU.add),
                   reads=[r_xr[xi], r_ln], writes=[r_xr[xi]])
                dma("sp", lambda e, xi=xi, ti=ti: e.dma_start(out=out_d[ti * 128:(ti + 1) * 128, :], in_=xr[xi][:]),
                    reads=[r_xr[xi]], writes=[r_out])

            for ti in range(min(2, NTILE)):
                combine_fetch(ti)
            for step in range(NTILE + 2):
                if 0 <= step - 2 < NTILE:
                    combine_C(step - 2)
                if 0 <= step - 1 < NTILE:
                    combine_B(step - 1)
                if step < NTILE:
                    combine_A(step)
                if step + 2 < NTILE:
                    combine_fetch(step + 2)
            S_.barrier()
            with nc.Block() as blk:
                S_.emit(blk)
    return nc


def host_inputs(x2, w_in, b_in, attn_sinks, rel_bias_table, w_branch_swa, w_branch_moba, w_out, ln1_gain, ln1_bias,
                w_group_router, b_group_router, w_expert_router, b_expert_router, w_expert_gate, w_expert_up,
                w_expert_down, ln2_gain, ln2_bias):
    f = lambda a: np.ascontiguousarray(np.asarray(a, dtype=np.float32))
    w = np.asarray(w_in)[0]
    b = np.asarray(b_in)[0]
    perm = np.arange(4352)
    qperm = []
    for c in range(4):
        qperm += list(range(c * 64, c * 64 + 64)) + list(range((4 + c) * 64, (4 + c) * 64 + 64))
    perm[0:512] = np.array(qperm)
    wp = w[:, perm]
    bp = b[perm]
    qk_cols = list(range(0, 640)) + list(range(768, 1792))
    bqk = bp[qk_cols].reshape(13, 128).T
    bgt = bp[2304:4352].reshape(16, 128).T
    bvv = np.concatenate([bp[640:768], bp[1792:2304]])[None, :]
    rel = np.asarray(rel_bias_table)
    k = np.arange(128)[:, None]
    q = np.arange(128)[None, :]
    d_own = rel_bucket_np(q - k)
    d_prev = rel_bucket_np(q + 128 - k)
    ga = np.stack([rel[d_own][:, :, :8].transpose(0, 2, 1), rel[d_prev][:, :, :8].transpose(0, 2, 1)], axis=1)
    j = np.arange(1024)[None, :]
    gbk = rel_bucket_np(j - k - 384)
    gb = rel[gbk][:, :, 8:].transpose(0, 2, 1)
    common = {
        "w_in": f(wp), "bqk": f(bqk), "bg": f(bgt), "bv": f(bvv),
        "sinks": f(np.asarray(attn_sinks)[0][None, :]), "t31": f(rel[31:32, 8:16]),
        "ga_raw": f(ga.reshape(128, -1)), "gb_raw": f(gb.reshape(128, -1)),
        "wa": f(np.asarray(w_branch_swa)[0]), "wb": f(np.asarray(w_branch_moba)[0]), "wo": f(np.asarray(w_out)[0]),
        "ln": f(np.stack([np.asarray(ln1_gain)[0], np.asarray(ln1_bias)[0], np.asarray(ln2_gain)[0], np.asarray(ln2_bias)[0]])),
        "wr": f(np.concatenate([np.asarray(w_group_router)[0], np.asarray(w_expert_router)[0]], axis=1)),
        "br": f(np.concatenate([np.asarray(b_group_router)[0], np.asarray(b_expert_router)[0]])[None, :]),
        "wg": f(np.asarray(w_expert_gate)[0]), "wu": f(np.asarray(w_expert_up)[0]), "wd": f(np.asarray(w_expert_down)[0]),
    }
    return common


def run(x, ncores, nseq, S, **params):
    x = np.asarray(x, dtype=np.float32)
    common = host_inputs(x, **params)
    nc = build(nseq, S)
    in_maps = []
    for i in range(ncores):
        xs = x[i * nseq:(i + 1) * nseq].reshape(nseq * S, D)
        m = dict(common)
        m["x_tok"] = np.ascontiguousarray(xs)
        m["x_T"] = np.ascontiguousarray(xs.T)
        in_maps.append(m)
    res = run_bass_kernel_spmd(nc, in_maps, core_ids=list(range(ncores)))
    outs = [np.asarray(r["out"]).reshape(nseq, S, D) for r in res.results]
    return np.concatenate(outs, axis=0).astype(np.float32)


def kernel(x, **params):
    B, S, _ = np.asarray(x).shape
    return run(x, NCORES, B // NCORES, S, **params)
```
